# Optimizing a Trainium2 kernel written in Bass

```python
import math
import jax
import jax.numpy as jnp
from jax import lax
import numpy as np

D_MODEL = 1024
BATCH = 2
SEQ = 8192
DEPTH = 2

GRID_W = 64
CTX_LEN = 256
N_MOD = 6
DA_HEADS = 4
DA_QK = 64
DA_V = 2 * DA_QK
DA_QK_W = DA_HEADS * 2 * DA_QK
DA_V_W = DA_HEADS * DA_V
MLA_HEADS = 4
MLA_NOPE = 128
MLA_ROPE = 64
MLA_V = 128
MLA_Q_RANK = 384
MLA_KV_RANK = 256
MLA_V_W = MLA_HEADS * MLA_V
POOL_GROUPS = 4
POOL_WINDOWS = (2, 4, 8, 16)
POOL_GROUP_W = 128
POOL_W = POOL_GROUPS * POOL_GROUP_W
N_BRANCH = 3
BRANCH_W = 512
D_FF = 2816
N_EXPERTS = 8
TOP_K = 2
D_FF_EXPERT = 3584
ROPE_BASE = 10000.0
Q_BLOCK = 128
EPS = 1e-6
IN_SPLITS = (DA_QK_W, DA_QK_W, DA_V_W, MLA_Q_RANK, MLA_KV_RANK, MLA_ROPE, POOL_W, N_BRANCH * D_MODEL)
IN_W = 5824

kernel_name = 'hybrid_diffattn_mla_pool_moe_dit'


def rmsnorm(x, g):
    xf = x.astype(jnp.float32)
    y = xf * lax.rsqrt(jnp.mean(xf * xf, axis=-1, keepdims=True) + EPS)
    return (y * g.astype(jnp.float32)).astype(x.dtype)


def modulate(h, shift, scale):
    return h * (1.0 + scale) + shift


def adaln_params(cond, w, b):
    return jnp.matmul(jax.nn.silu(cond), w) + b


def split_columns(z):
    offs = []
    acc = 0
    for s in IN_SPLITS[:-1]:
        acc += s
        offs.append(acc)
    return jnp.split(z, offs, axis=-1)


def axial_rope_tables(row_pos, col_pos, rot_dim):
    axis_dim = rot_dim // 2
    n_freq = axis_dim // 2
    inv = jnp.exp(-math.log(ROPE_BASE) * jnp.arange(n_freq, dtype=jnp.float32) * (2.0 / axis_dim))
    ar = row_pos.astype(jnp.float32)[:, None] * inv
    ac = col_pos.astype(jnp.float32)[:, None] * inv
    return (jnp.cos(ar), jnp.sin(ar), jnp.cos(ac), jnp.sin(ac))


def rope_half(x, cos, sin):
    cos = cos[None, :, None, :].astype(x.dtype)
    sin = sin[None, :, None, :].astype(x.dtype)
    x1, x2 = jnp.split(x, 2, axis=-1)
    return jnp.concatenate([x1 * cos - x2 * sin, x2 * cos + x1 * sin], axis=-1)


def rope_2d(x, tabs):
    cr, sr, cc, sc = tabs
    a = x.shape[-1] // 2
    return jnp.concatenate([rope_half(x[..., :a], cr, sr), rope_half(x[..., a:], cc, sc)], axis=-1)


def sweep_query_blocks(fn, qs):
    B, S = qs[0].shape[:2]
    nb = S // Q_BLOCK
    blocks = tuple(jnp.moveaxis(q.reshape((B, nb, Q_BLOCK) + q.shape[2:]), 1, 0) for q in qs)
    out = lax.map(lambda blk: fn(*blk), blocks)
    out = jnp.moveaxis(out, 0, 1)
    return out.reshape((B, S) + out.shape[3:])


def diff_attention(q1, q2, k1, k2, v, lam):
    scale = DA_QK ** -0.5
    s1 = jnp.einsum('bqhd,bkhd->bhqk', q1, k1).astype(jnp.float32) * scale
    s2 = jnp.einsum('bqhd,bkhd->bhqk', q2, k2).astype(jnp.float32) * scale
    a = jax.nn.softmax(s1, axis=-1) - lam * jax.nn.softmax(s2, axis=-1)
    return jnp.einsum('bhqk,bkhd->bqhd', a.astype(v.dtype), v)


def diff_head_norm(o, g, lam_init):
    B, L = o.shape[:2]
    return (rmsnorm(o, g) * (1.0 - lam_init)).reshape(B, L, DA_V_W)


def mla_queries(qd, gq, w_uq):
    B, L = qd.shape[:2]
    qf = jnp.matmul(rmsnorm(qd, gq), w_uq).reshape(B, L, MLA_HEADS, MLA_NOPE + MLA_ROPE)
    return qf[..., :MLA_NOPE], qf[..., MLA_NOPE:]


def mla_keys_values(kvd, gkv, w_ukv):
    B, L = kvd.shape[:2]
    kvf = jnp.matmul(rmsnorm(kvd, gkv), w_ukv).reshape(B, L, MLA_HEADS, MLA_NOPE + MLA_V)
    return kvf[..., :MLA_NOPE], kvf[..., MLA_NOPE:]


def mla_attention(q_nope, q_rope, k_nope, k_rope, v):
    scale = (MLA_NOPE + MLA_ROPE) ** -0.5
    s = (jnp.einsum('bqhd,bkhd->bhqk', q_nope, k_nope)
         + jnp.einsum('bqhr,bkr->bhqk', q_rope, k_rope)).astype(jnp.float32) * scale
    p = jax.nn.softmax(s, axis=-1)
    return jnp.einsum('bhqk,bkhd->bqhd', p.astype(v.dtype), v)


def multi_scale_pool(u, pool_w, pool_scale):
    B, L, _ = u.shape
    ug = u.reshape(B, L, POOL_GROUPS, POOL_GROUP_W).astype(jnp.float32)
    cs = jnp.concatenate([jnp.zeros((B, 1, POOL_GROUPS, POOL_GROUP_W), jnp.float32),
                          jnp.cumsum(ug, axis=1)], axis=1)
    t = jnp.arange(L)
    outs = []
    for g, w in enumerate(POOL_WINDOWS):
        lo = jnp.clip(t - w // 2, 0, L)
        hi = jnp.clip(t - w // 2 + w, 0, L)
        cnt = (hi - lo).astype(jnp.float32)[None, :, None]
        mean = (cs[:, hi, g] - cs[:, lo, g]) / cnt
        outs.append(mean - ug[:, :, g])
    d = jnp.stack(outs, axis=2).astype(u.dtype)
    y = jnp.einsum('blgc,gcd->blgd', d, pool_w).reshape(B, L, POOL_W)
    return y * pool_scale


def merge_branches(branches, gate_logits, w_branch, w_out):
    B, L = branches.shape[:2]
    D = w_out.shape[0]
    g = jax.nn.sigmoid(gate_logits.astype(jnp.float32)).astype(branches.dtype).reshape(B, L, N_BRANCH, D)
    proj = jnp.einsum('blnc,ncd->blnd', branches, w_branch)
    return jnp.matmul(jnp.einsum('blnd,blnd->bld', g, proj), w_out)


def hybrid_mixer(h, hc, w_in, lam_vec, lam_init, subln_g, gq, w_uq, gkv, w_ukv,
                 pool_w, pool_scale, w_branch, w_out, da_tabs, mla_tabs, need_ctx):
    B, S, _ = h.shape
    C = hc.shape[1]
    da_q, da_k, da_v, mla_qd, mla_kvd, mla_kr, pool_in, gate_in = split_columns(jnp.matmul(h, w_in))
    cda_q, cda_k, cda_v, cmla_qd, cmla_kvd, cmla_kr, cpool_in, cgate_in = split_columns(jnp.matmul(hc, w_in))
    lv = lam_vec.astype(jnp.float32)
    lam = jnp.exp(jnp.sum(lv[0] * lv[1])) - jnp.exp(jnp.sum(lv[2] * lv[3])) + lam_init

    q = da_q.reshape(B, S, DA_HEADS, 2, DA_QK)
    k = da_k.reshape(B, S, DA_HEADS, 2, DA_QK)
    v = da_v.reshape(B, S, DA_HEADS, DA_V)
    cq = cda_q.reshape(B, C, DA_HEADS, 2, DA_QK)
    ck = cda_k.reshape(B, C, DA_HEADS, 2, DA_QK)
    cv = cda_v.reshape(B, C, DA_HEADS, DA_V)
    k1 = jnp.concatenate([ck[..., 0, :], rope_2d(k[..., 0, :], da_tabs)], axis=1)
    k2 = jnp.concatenate([ck[..., 1, :], rope_2d(k[..., 1, :], da_tabs)], axis=1)
    v_all = jnp.concatenate([cv, v], axis=1)
    q1 = rope_2d(q[..., 0, :], da_tabs)
    q2 = rope_2d(q[..., 1, :], da_tabs)
    o_da = sweep_query_blocks(lambda a, b: diff_attention(a, b, k1, k2, v_all, lam), (q1, q2))
    o_da = diff_head_norm(o_da, subln_g, lam_init)

    k_nope, v_m = mla_keys_values(mla_kvd, gkv, w_ukv)
    ck_nope, cv_m = mla_keys_values(cmla_kvd, gkv, w_ukv)
    k_rope = rope_2d(mla_kr[:, :, None, :], mla_tabs)[:, :, 0, :]
    kn_all = jnp.concatenate([ck_nope, k_nope], axis=1)
    kr_all = jnp.concatenate([cmla_kr, k_rope], axis=1)
    vm_all = jnp.concatenate([cv_m, v_m], axis=1)
    q_nope, q_rope = mla_queries(mla_qd, gq, w_uq)
    q_rope = rope_2d(q_rope, mla_tabs)
    o_mla = sweep_query_blocks(lambda a, b: mla_attention(a, b, kn_all, kr_all, vm_all),
                               (q_nope, q_rope)).reshape(B, S, MLA_V_W)

    o_pool = multi_scale_pool(pool_in, pool_w, pool_scale)

    mix = merge_branches(jnp.stack([o_da, o_mla, o_pool], axis=2), gate_in, w_branch, w_out)
    if not need_ctx:
        return mix, None

    co_da = diff_head_norm(diff_attention(cq[..., 0, :], cq[..., 1, :], ck[..., 0, :], ck[..., 1, :], cv, lam),
                           subln_g, lam_init)
    cq_nope, cq_rope = mla_queries(cmla_qd, gq, w_uq)
    co_mla = mla_attention(cq_nope, cq_rope, ck_nope, cmla_kr, cv_m).reshape(B, C, MLA_V_W)
    co_pool = multi_scale_pool(cpool_in, pool_w, pool_scale)
    mix_c = merge_branches(jnp.stack([co_da, co_mla, co_pool], axis=2), cgate_in, w_branch, w_out)
    return mix, mix_c


def swiglu(t, wg, wu, wd):
    return jnp.matmul(jax.nn.silu(jnp.matmul(t, wg)) * jnp.matmul(t, wu), wd)


def moe_swiglu(h, router, wg, wu, wd):
    B, L, D = h.shape
    t = h.reshape(B * L, D)
    logits = jnp.matmul(t, router).astype(jnp.float32)
    top_val, top_idx = lax.top_k(logits, TOP_K)
    top_w = jax.nn.softmax(top_val, axis=-1)
    gates = jnp.einsum('nk,nke->ne', top_w,
                       jax.nn.one_hot(top_idx, N_EXPERTS, dtype=jnp.float32)).astype(h.dtype)
    y = jnp.zeros_like(t)
    for e in range(N_EXPERTS):
        y = y + gates[:, e:e + 1] * swiglu(t, wg[e], wu[e], wd[e])
    return y.reshape(B, L, D)


def setup_inputs(seed: int = 0) -> dict:
    key = jax.random.key(seed)
    ks = jax.random.split(key, 32)
    f32 = jnp.float32
    D = D_MODEL
    n_dense = (DEPTH + 1) // 2
    n_moe = DEPTH // 2

    def nrm(k, shape, scale):
        return jax.random.normal(k, shape, f32) * scale

    def gain(k, shape):
        return 1.0 + 0.1 * jax.random.normal(k, shape, f32)

    return {
        'x': nrm(ks[0], (BATCH, SEQ, D), 1.0),
        'c': nrm(ks[1], (BATCH, D), 1.0),
        'ctx': nrm(ks[2], (BATCH, CTX_LEN, D), 1.0),
        'c_ctx': nrm(ks[3], (D,), 1.0),
        'w_mod': nrm(ks[4], (DEPTH, D, N_MOD * D), 0.5 * D ** -0.5),
        'b_mod': nrm(ks[5], (DEPTH, N_MOD * D), 0.02),
        'g_mix': gain(ks[6], (DEPTH, D)),
        'w_in': nrm(ks[7], (DEPTH, D, IN_W), D ** -0.5),
        'da_lambda': nrm(ks[8], (DEPTH, 4, DA_QK), 0.1),
        'da_subln': gain(ks[9], (DEPTH, DA_V)),
        'mla_gq': gain(ks[10], (DEPTH, MLA_Q_RANK)),
        'w_uq': nrm(ks[11], (DEPTH, MLA_Q_RANK, MLA_HEADS * (MLA_NOPE + MLA_ROPE)), MLA_Q_RANK ** -0.5),
        'mla_gkv': gain(ks[12], (DEPTH, MLA_KV_RANK)),
        'w_ukv': nrm(ks[13], (DEPTH, MLA_KV_RANK, MLA_HEADS * (MLA_NOPE + MLA_V)), MLA_KV_RANK ** -0.5),
        'pool_w': nrm(ks[14], (DEPTH, POOL_GROUPS, POOL_GROUP_W, POOL_GROUP_W), POOL_GROUP_W ** -0.5),
        'pool_scale': gain(ks[15], (DEPTH, POOL_W)),
        'w_branch': nrm(ks[16], (DEPTH, N_BRANCH, BRANCH_W, D), BRANCH_W ** -0.5),
        'w_out': nrm(ks[17], (DEPTH, D, D), D ** -0.5),
        'g_ffn': gain(ks[18], (DEPTH, D)),
        'ffn_w_gate': nrm(ks[19], (n_dense, D, D_FF), D ** -0.5),
        'ffn_w_up': nrm(ks[20], (n_dense, D, D_FF), D ** -0.5),
        'ffn_w_down': nrm(ks[21], (n_dense, D_FF, D), D_FF ** -0.5),
        'moe_router': nrm(ks[22], (n_moe, D, N_EXPERTS), D ** -0.5),
        'moe_w_gate': nrm(ks[23], (n_moe, N_EXPERTS, D, D_FF_EXPERT), D ** -0.5),
        'moe_w_up': nrm(ks[24], (n_moe, N_EXPERTS, D, D_FF_EXPERT), D ** -0.5),
        'moe_w_down': nrm(ks[25], (n_moe, N_EXPERTS, D_FF_EXPERT, D), D_FF_EXPERT ** -0.5),
        'g_final': gain(ks[26], (D,)),
    }


def reference(x, c, ctx, c_ctx, w_mod, b_mod, g_mix, w_in, da_lambda, da_subln, mla_gq, w_uq,
              mla_gkv, w_ukv, pool_w, pool_scale, w_branch, w_out, g_ffn, ffn_w_gate, ffn_w_up,
              ffn_w_down, moe_router, moe_w_gate, moe_w_up, moe_w_down, g_final):
    B, S, D = x.shape
    rows = S // GRID_W
    row_pos = jnp.repeat(jnp.arange(rows, dtype=jnp.int32), GRID_W)
    col_pos = jnp.tile(jnp.arange(GRID_W, dtype=jnp.int32), rows)
    da_tabs = axial_rope_tables(row_pos, col_pos, DA_QK)
    mla_tabs = axial_rope_tables(row_pos, col_pos, MLA_ROPE)

    xc = ctx
    for l in range(DEPTH):
        need_ctx = l < DEPTH - 1
        mod = adaln_params(c, w_mod[l], b_mod[l])[:, None, :]
        mod_c = adaln_params(c_ctx, w_mod[l], b_mod[l])[None, None, :]
        sh1, sc1, gt1, sh2, sc2, gt2 = jnp.split(mod, N_MOD, axis=-1)
        csh1, csc1, cgt1, csh2, csc2, cgt2 = jnp.split(mod_c, N_MOD, axis=-1)
        lam_init = 0.8 - 0.6 * math.exp(-0.3 * l)

        h = modulate(rmsnorm(x, g_mix[l]), sh1, sc1)
        hc = modulate(rmsnorm(xc, g_mix[l]), csh1, csc1)
        mix, mix_c = hybrid_mixer(h, hc, w_in[l], da_lambda[l], lam_init, da_subln[l], mla_gq[l], w_uq[l],
                                  mla_gkv[l], w_ukv[l], pool_w[l], pool_scale[l], w_branch[l], w_out[l],
                                  da_tabs, mla_tabs, need_ctx)
        x = x + gt1 * mix
        h = modulate(rmsnorm(x, g_ffn[l]), sh2, sc2)
        j = l // 2
        if l % 2 == 0:
            x = x + gt2 * swiglu(h, ffn_w_gate[j], ffn_w_up[j], ffn_w_down[j])
        else:
            x = x + gt2 * moe_swiglu(h, moe_router[j], moe_w_gate[j], moe_w_up[j], moe_w_down[j])

        if need_ctx:
            xc = xc + cgt1 * mix_c
            hc = modulate(rmsnorm(xc, g_ffn[l]), csh2, csc2)
            if l % 2 == 0:
                xc = xc + cgt2 * swiglu(hc, ffn_w_gate[j], ffn_w_up[j], ffn_w_down[j])
            else:
                xc = xc + cgt2 * moe_swiglu(hc, moe_router[j], moe_w_gate[j], moe_w_up[j], moe_w_down[j])

    return rmsnorm(x, g_final)
```

```python
import math
from contextlib import ExitStack
import numpy as np
import ml_dtypes
import concourse.bass as bass
import concourse.mybir as mybir
from concourse.bass_utils import run_bass_kernel_spmd

F32 = mybir.dt.float32
BF16 = mybir.dt.bfloat16
AF = mybir.ActivationFunctionType
ALU = mybir.AluOpType

D = 1024
KC = 8
S = 8192
NCORE = 8
TOK = 2048
CTX = 256
NKEY = S + CTX
DEPTH = 2
IN_W = 5824
D_FF = 2816
NEXP = 8
D_FFE = 3584
EPS = 1e-6

ENGS = ('pe', 'act', 'dve', 'pool', 'sp')


class Tk:
    __slots__ = ('name', 'w', 'r')

    def __init__(self, name):
        self.name = name
        self.w = {}
        self.r = {}


class Op:
    __slots__ = ('eng', 'fn', 'waits', 'idx', 'signal', 'dma', 'signo')

    def __init__(self, eng, fn, idx):
        self.eng = eng
        self.fn = fn
        self.waits = []
        self.idx = idx
        self.signal = False
        self.dma = None
        self.signo = None


class Sched:
    NDMA = 12
    EPOCH = 12000

    def __init__(self):
        self.ops = {e: [] for e in ENGS}
        self.seen = {e: {} for e in ENGS}
        self.dma_count = {e: 0 for e in ENGS}
        self.dma_slot_val = {}

    def _deps(self, reads, writes):
        deps = {}

        def add(d):
            for k, v in d.items():
                if deps.get(k, -1) < v:
                    deps[k] = v
        for t in reads:
            add(t.w)
        for t in writes:
            add(t.w)
            add(t.r)
        return deps

    def _apply_waits(self, op, deps, raw_keys):
        eng = op.eng
        seen = self.seen[eng]
        for k, v in deps.items():
            if k[0] == 'e' and k[1] == eng and k not in raw_keys:
                continue
            if seen.get(k, -1) >= v:
                continue
            seen[k] = v
            op.waits.append((k, v))
            if k[0] == 'e':
                self.ops[k[1]][v].signal = True

    def op(self, eng, fn, reads=(), writes=(), pwrites=()):
        o = Op(eng, fn, len(self.ops[eng]))
        deps = self._deps(reads, list(writes) + list(pwrites))
        raw = set()
        for t in reads:
            raw.update(t.w.keys())
        self._apply_waits(o, deps, raw)
        self.ops[eng].append(o)
        key = ('e', eng)
        for t in writes:
            t.w = {key: o.idx}
            t.r = {}
        for t in pwrites:
            t.w[key] = o.idx
        for t in reads:
            t.r[key] = o.idx
        return o

    def dma(self, q, out, in_, reads=(), writes=(), pwrites=()):
        n = self.dma_count[q]
        self.dma_count[q] += 1
        slot = n % self.NDMA
        key = ('d', q, slot)
        val = 16 * (n // self.NDMA + 1)
        o = Op(q, lambda e: e.dma_start(out=out, in_=in_), len(self.ops[q]))
        o.dma = (key, val)
        deps = self._deps(reads, list(writes) + list(pwrites))
        if val > 16:
            deps[key] = val - 16
        raw = set()
        for t in reads:
            raw.update(t.w.keys())
        raw.add(key)
        self._apply_waits(o, deps, raw)
        self.ops[q].append(o)
        for t in writes:
            t.w = {key: val}
            t.r = {}
        for t in pwrites:
            t.w[key] = val
        for t in reads:
            t.r[key] = val
        return o

    def coll(self, fn, reads=(), writes=(), pwrites=()):
        q = 'pool'
        n = self.ncoll = getattr(self, 'ncoll', 0) + 1
        key = ('c', n)
        o = Op(q, fn, len(self.ops[q]))
        o.dma = (key, 1)
        deps = self._deps(reads, list(writes) + list(pwrites))
        raw = set()
        for t in reads:
            raw.update(t.w.keys())
        self._apply_waits(o, deps, raw)
        self.ops[q].append(o)
        for t in writes:
            t.w = {key: 1}
            t.r = {}
        for t in pwrites:
            t.w[key] = 1
        for t in reads:
            t.r[key] = 1
        return o

    def emit(self, nc, stack):
        esems = {}
        for e in ENGS:
            k = 0
            for o in self.ops[e]:
                if o.dma is None and o.signal:
                    o.signo = k
                    k += 1
            nep = (k + self.EPOCH - 1) // self.EPOCH
            esems[e] = [stack.enter_context(nc.semaphore(f"s_{e}_{i}")) for i in range(nep)]
        dsems = {}
        for e in ENGS:
            for sl in range(min(self.NDMA, self.dma_count[e])):
                dsems[('d', e, sl)] = stack.enter_context(nc.semaphore(f"d_{e}_{sl}"))
        for i in range(1, getattr(self, 'ncoll', 0) + 1):
            dsems[('c', i)] = stack.enter_context(nc.semaphore(f"c_{i}"))
        ops = self.ops
        EP = self.EPOCH

        def run(ename, eng):
            for o in ops[ename]:
                for k, v in o.waits:
                    if k[0] in ('d', 'c'):
                        eng.wait_ge(dsems[k], v)
                    else:
                        sn = ops[k[1]][v].signo
                        eng.wait_ge(esems[k[1]][sn // EP], sn % EP + 1)
                ins = o.fn(eng)
                if o.dma is not None:
                    ins.then_inc(dsems[o.dma[0]], 1 if o.dma[0][0] == 'c' else 16)
                elif o.signal:
                    ins.then_inc(esems[ename][o.signo // EP], 1)

        with nc.Block() as block:
            @block.tensor
            def _(e):
                run('pe', e)

            @block.scalar
            def _(e):
                run('act', e)

            @block.vector
            def _(e):
                run('dve', e)

            @block.gpsimd
            def _(e):
                run('pool', e)

            @block.sync
            def _(e):
                run('sp', e)


R_KDA, R_KN, R_KR, R_VDA, R_VM, R_U, ROWS = 0, 512, 1024, 1152, 1664, 2176, 2184
NCH = 17
GROWS = NCH * 512 + 32
C_DAQ, C_DAK, C_DAV, C_QD, C_KVD, C_KR, C_POOL, C_GATE = 0, 512, 1024, 1536, 1920, 2176, 2240, 2752
LAM_INIT = [0.8 - 0.6 * math.exp(-0.3 * l) for l in range(DEPTH)]
POOL_WINDOWS = (2, 4, 8, 16)
BIG = 1.0e4


def _cols_layout():
    off = {}
    n = 0

    def add(name, w):
        nonlocal n
        off[name] = (n, w)
        n += w
    add('c', 8)
    add('cctx', 8)
    for l in range(DEPTH):
        add(f'bmod{l}', 48)
        add(f'gmix{l}', 8)
        add(f'gffn{l}', 8)
        add(f'gq{l}', 3)
        add(f'gkv{l}', 2)
        add(f'pscale{l}', 4)
        add(f'subln{l}', 1)
        add(f'lam{l}', 4)
    add('gfin', 8)
    add('pfix', 64)
    add('pfixc', 64)
    add('selL', 4)
    add('selR', 4)
    return off, n


COLS, NCOLS = _cols_layout()


def _colvec(v):
    v = np.asarray(v, np.float32)
    return np.ascontiguousarray(v.reshape(-1, 128).T)


class Ring:
    def __init__(self, mk, name, n, shape, dt):
        self.bufs = [(mk(f"{name}{i}", shape, dt), Tk(f"{name}{i}")) for i in range(n)]
        self.i = 0

    def next(self):
        b = self.bufs[self.i % len(self.bufs)]
        self.i += 1
        return b


def build(stage='all', debug=()):
    nc = bass.Bass("TRN2", target_bir_lowering=False)
    sch = Sched()
    top = ExitStack()

    def dram_in(name, shape, dt=F32):
        return nc.dram_tensor(name, list(shape), dt, kind="ExternalInput").ap()

    def dram_out(name, shape, dt=F32):
        return nc.dram_tensor(name, list(shape), dt, kind="ExternalOutput").ap()

    uniq = [0]

    def mk_sb(stack):
        def f(name, shape, dt):
            uniq[0] += 1
            return stack.enter_context(nc.sbuf_tensor(f"sb{uniq[0]}_{name}", list(shape), dt))
        return f
    sb = mk_sb(top)

    def MM(out, lhsT, rhs, start, stop, reads, writes):
        return sch.op('pe', lambda e: e.matmul(out, lhsT=lhsT, rhs=rhs, start=start, stop=stop),
                      reads=reads, writes=writes)

    def ACT(out, in_, func, reads, writes, scale=None, bias=None, pw=()):
        kw = {}
        if scale is not None:
            kw['scale'] = scale
        if bias is not None:
            kw['bias'] = bias
        return sch.op('act', lambda e: e.activation(out=out, in_=in_, func=func, **kw),
                      reads=reads, writes=writes, pwrites=pw)

    def TT(out, in0, in1, op, reads, writes, eng='dve', pw=()):
        return sch.op(eng, lambda e: e.tensor_tensor(out=out, in0=in0, in1=in1, op=op),
                      reads=reads, writes=writes, pwrites=pw)

    def TS(out, in0, s1, s2, op0, op1, reads, writes, eng='dve', pw=()):
        if op1 is None:
            return sch.op(eng, lambda e: e.tensor_scalar(out=out, in0=in0, scalar1=s1, scalar2=None, op0=op0),
                          reads=reads, writes=writes, pwrites=pw)
        return sch.op(eng, lambda e: e.tensor_scalar(out=out, in0=in0, scalar1=s1, scalar2=s2, op0=op0, op1=op1),
                      reads=reads, writes=writes, pwrites=pw)

    def STT(out, in0, scalar, in1, op0, op1, reads, writes, pw=()):
        return sch.op('dve', lambda e: e.scalar_tensor_tensor(out=out, in0=in0, scalar=scalar, in1=in1,
                                                              op0=op0, op1=op1),
                      reads=reads, writes=writes, pwrites=pw)

    def CP(out, in_, reads, writes, eng='dve', pw=()):
        if eng == 'act':
            return sch.op('act', lambda e: e.copy(out=out, in_=in_), reads=reads, writes=writes, pwrites=pw)
        return sch.op(eng, lambda e: e.tensor_copy(out=out, in_=in_), reads=reads, writes=writes, pwrites=pw)

    def RSTD(out, ps_in, inv_n, reads, t_out):
        ACT(out, ps_in, AF.Sqrt, reads, [t_out], scale=inv_n, bias=EPS)
        sch.op('dve', lambda e: e.reciprocal(out=out, in_=out), reads=[t_out], writes=[t_out])

    def RECIP(out, in_, reads, writes):
        return sch.op('dve', lambda e: e.reciprocal(out=out, in_=in_), reads=reads, writes=writes)

    def barrier():
        last = {e: len(sch.ops[e]) - 1 for e in ENGS}
        dl = {}
        for q in ENGS:
            n = sch.dma_count[q]
            for sl in range(min(n, sch.NDMA)):
                uses = (n - 1 - sl) // sch.NDMA + 1
                dl[('d', q, sl)] = 16 * uses
        for e in ENGS:
            o = Op(e, lambda en: en.nop(), len(sch.ops[e]))
            deps = dict(dl)
            for e2 in ENGS:
                if e2 != e and last[e2] >= 0:
                    k = last[e2]
                    while k >= 0 and sch.ops[e2][k].dma is not None:
                        k -= 1
                    if k >= 0:
                        deps[('e', e2)] = k
            sch._apply_waits(o, deps, set(deps.keys()))
            sch.ops[e].append(o)

    A_ = stage == 'A'
    B_ = stage == 'B'
    C_ = stage == 'C'
    ALL = stage == 'all'

    xT = dram_in("xT", [D, TOK])
    ctxT = dram_in("ctxT", [D, CTX])
    cols_d = dram_in("cols", [128, NCOLS])
    consts_d = dram_in("consts", [128, 384], BF16)
    constf_d = dram_in("constf", [128, 256], F32)
    ropeC_d = dram_in("ropeC", [128, TOK], BF16)
    ropeS_d = dram_in("ropeS", [128, TOK], BF16)
    w_mod = dram_in("w_mod", [DEPTH, D, 6 * D])
    w_in = dram_in("w_in", [DEPTH, D, IN_W])
    w_uq = dram_in("w_uq_p", [DEPTH, 384, 768])
    w_ukv = dram_in("w_ukv_p", [DEPTH, 256, 1024])
    pool_w = dram_in("pool_w", [DEPTH, 4, 128, 128])
    w_branch = dram_in("w_branch", [DEPTH, 3, 512, D])
    w_out = dram_in("w_out", [DEPTH, D, D])
    if B_ or ALL:
        ffn_wg = dram_in("ffn_wg", [D, D_FF])
        ffn_wu = dram_in("ffn_wu", [D, D_FF])
        ffn_wd = dram_in("ffn_wd", [D_FF, D])
    if C_ or ALL:
        router_d = dram_in("router", [D, NEXP])
        moe_wg = dram_in("moe_wg", [NEXP, D, D_FFE])
        moe_wu = dram_in("moe_wu", [NEXP, D, D_FFE])
        moe_wd = dram_in("moe_wd", [NEXP, D_FFE, D])

    kvl, kvc, kvg, halo = {}, {}, {}, {}
    t_kvl, t_kvc, t_kvg = {}, {}, {}
    if A_:
        kvl[0] = dram_out("kvl0", [ROWS, TOK], BF16)
        kvc[0] = dram_out("kvc0", [ROWS, CTX], BF16)
    if B_:
        kvg[0] = dram_in("kvg0", [GROWS, TOK], BF16)
        kvc[0] = dram_in("kvc0", [ROWS, CTX], BF16)
        halo[0] = dram_in("halo0", [128, 4, 16], BF16)
        kvl[1] = dram_out("kvl1", [ROWS, TOK], BF16)
        kvc[1] = dram_out("kvc1", [ROWS, CTX], BF16)
    if ALL:
        for l_ in range(DEPTH):
            kvl[l_] = nc.dram_tensor(f"kvl{l_}", [ROWS, TOK], BF16, kind="Internal").ap()
            kvc[l_] = nc.dram_tensor(f"kvc{l_}", [ROWS, CTX], BF16, kind="Internal").ap()
            kvg[l_] = nc.dram_tensor(f"kvg{l_}", [GROWS, TOK], BF16, kind="Internal").ap()
    if C_:
        kvg[1] = dram_in("kvg1", [GROWS, TOK], BF16)
        kvc[1] = dram_in("kvc1", [ROWS, CTX], BF16)
        halo[1] = dram_in("halo1", [128, 4, 16], BF16)
    u_out = u_in = uc_out = uc_in = None
    if A_ or B_:
        u_out = dram_out("u_out", [128, 4 * (TOK + 16)], BF16)
    if A_:
        uc_out = dram_out("uc_out", [128, 4 * (CTX + 16)], BF16)
    if B_ or C_:
        u_in = dram_in("u_in", [128, 4 * (TOK + 16)], BF16)
    if B_:
        uc_in = dram_in("uc_in", [128, 4 * (CTX + 16)], BF16)
    for l in range(DEPTH):
        t_kvl[l] = Tk(f'kvl{l}')
        t_kvc[l] = Tk(f'kvc{l}')
        t_kvg[l] = [Tk(f'kvg{l}_{c}') for c in range(NCH + 1)]

    psb = [top.enter_context(nc.psum_tensor(f"psb{i}", [128, 512], F32)) for i in range(8)]
    t_ps = [Tk(f'ps{i}') for i in range(8)]

    x_sb = sb("x_sb", [128, KC, TOK], F32)
    xc_sb = sb("xc_sb", [128, KC, CTX], F32)
    t_x = [[Tk(f'x{kc}_{b}') for b in range(4)] for kc in range(KC)]
    t_xc = [Tk(f'xc{kc}') for kc in range(KC)]
    cols = sb("cols_sb", [128, NCOLS], F32)
    t_cols = Tk('cols')
    consts = sb("consts_sb", [128, 384], BF16)
    constf = sb("constf_sb", [128, 256], F32)
    t_consts = Tk('consts')
    ones_bf = consts[:, 0:128]
    ident_bf = consts[:, 128:256]
    perm_bf = consts[:, 256:384]
    ones_f = constf[:, 0:128]
    ident_f = constf[:, 128:256]
    mod_sb = sb("mod_sb", [128, DEPTH, 48, 2], F32)
    modA = sb("modA_sb", [128, DEPTH, 2, 8, 2], F32)
    t_mod = Tk('mod')
    misc = sb("misc_sb", [128, 16], F32)
    t_misc = Tk('misc')
    u_sb = sb("u_sb", [128, 4, TOK + 16], BF16)
    uc_sb = sb("uc_sb", [128, 4, CTX + 16], BF16)
    t_u = Tk('u')
    t_uc = Tk('uc')

    def colap(name, j=0, w=1):
        o, _ = COLS[name]
        return cols[:, o + j:o + j + w]

    sch.dma('sp', cols[:], cols_d[:], writes=[t_cols])
    sch.dma('sp', consts[:], consts_d[:], writes=[t_consts])
    sch.dma('sp', constf[:], constf_d[:], pwrites=[t_consts])
    for kc in range(KC):
        sch.dma('sp', x_sb[:, kc, :], xT[kc * 128:(kc + 1) * 128, :], writes=t_x[kc])
    if not C_:
        for kc in range(KC):
            sch.dma('sp', xc_sb[:, kc, :], ctxT[kc * 128:(kc + 1) * 128, :], writes=[t_xc[kc]])
    if u_in is not None:
        sch.dma('sp', u_sb[:].rearrange("p g t -> p (g t)"), u_in[:], writes=[t_u])
    if uc_in is not None:
        sch.dma('sp', uc_sb[:].rearrange("p g t -> p (g t)"), uc_in[:], writes=[t_uc])

    def mods_stage(pairs, with_misc):
        with ExitStack() as sc:
            lsb = mk_sb(sc)
            silu_c = lsb("silu_c", [128, KC, 2], BF16)
            t_silu = Tk('silu')
            o_c, _ = COLS['c']
            o_cc, _ = COLS['cctx']
            ACT(silu_c[:, :, 0], cols[:, o_c:o_c + 8], AF.Silu, [t_cols], [t_silu])
            ACT(silu_c[:, :, 1], cols[:, o_cc:o_cc + 8], AF.Silu, [t_cols], [], pw=[t_silu])
            wm = [lsb(f"wm{i}", [128, KC, 1024], BF16) for i in range(2)]
            t_wm = [Tk('wm0'), Tk('wm1')]
            t_psmod = t_ps[7]
            ps_mod = psb[7][:, 0:192]
            psv = ps_mod.rearrange("p (l m j) -> p l m j", l=DEPTH, j=2)
            for it, (l, part) in enumerate(pairs):
                wv = w_mod[l].rearrange("(kc p) n -> p kc n", p=128)
                buf = it % 2
                sch.dma('pool', wm[buf][:], wv[:, :, part * 1024:(part + 1) * 1024], writes=[t_wm[buf]])
                for mc in range(8):
                    c0 = (l * 48 + part * 8 + mc) * 2
                    for kc in range(KC):
                        MM(ps_mod[:, c0:c0 + 2], wm[buf][:, kc, mc * 128:(mc + 1) * 128], silu_c[:, kc, :],
                           kc == 0, kc == KC - 1, [t_wm[buf], t_silu], [t_psmod])
            for (l, part) in pairs:
                ob, _ = COLS[f'bmod{l}']
                for j in range(2):
                    TT(mod_sb[:, l, part * 8:(part + 1) * 8, j], psv[:, l, part * 8:(part + 1) * 8, j],
                       cols[:, ob + part * 8:ob + (part + 1) * 8], ALU.add, [t_psmod, t_cols], [], pw=[t_mod])
            for (l, part) in pairs:
                if part not in (1, 4):
                    continue
                which = 0 if part == 1 else 1
                gname = f'gmix{l}' if which == 0 else f'gffn{l}'
                og, _ = COLS[gname]
                for j in range(2):
                    STT(modA[:, l, which, :, j], mod_sb[:, l, part * 8:(part + 1) * 8, j], 1.0,
                        cols[:, og:og + 8], ALU.add, ALU.mult, [t_mod, t_cols], [], pw=[t_mod])
            if with_misc:
                lamt = lsb("lamt", [128, 4], F32)
                t_lamt = Tk('lamt')
                for l in range(DEPTH):
                    ol, _ = COLS[f'lam{l}']
                    lv = cols[:, ol:ol + 4].rearrange("p (a b) -> p a b", b=2)
                    TT(lamt[:, 0:2], lv[:, :, 0], lv[:, :, 1], ALU.mult, [t_cols], [t_lamt])
                    MM(psb[6][:, 0:2], ones_f, lamt[:, 0:2], True, True, [t_lamt, t_consts], [t_ps[6]])
                    ACT(lamt[:, 2:4], psb[6][:, 0:2], AF.Exp, [t_ps[6]], [], pw=[t_lamt])
                    STT(misc[:, 4 * l:4 * l + 1], lamt[:, 3:4], -LAM_INIT[l], lamt[:, 2:3], ALU.add, ALU.subtract,
                        [t_lamt], [], pw=[t_misc])
                    osub, _ = COLS[f'subln{l}']
                    TS(misc[:, 4 * l + 1:4 * l + 2], cols[:, osub:osub + 1], 1.0 - LAM_INIT[l], None, ALU.mult, None,
                       [t_cols], [], pw=[t_misc])
        barrier()

    MODS_FIRST = [(0, 0), (0, 1), (0, 2)]
    MODS_REST = [(0, 3), (0, 4), (0, 5)] + [(1, p_) for p_ in range(6)]
    mods_stage(MODS_FIRST, True)
    if not ALL:
        mods_stage(MODS_REST, False)

    def modcol(l, part, kc, j):
        return mod_sb[:, l, part * 8 + kc, j:j + 1]

    def norm_mod(R, xsrc, t_xs, ntok, l, which, j, h_out, t_h, f32cb=None, bank=7):
        for kc in range(KC):
            sq, t_sq = R['bf'].next()
            ACT(sq[:, :ntok], xsrc(kc), AF.Square, t_xs[kc], [t_sq])
            MM(psb[bank][:, :ntok], ones_bf, sq[:, :ntok], kc == 0, kc == KC - 1, [t_sq, t_consts], [t_ps[bank]])
        rstd, t_rstd = R['rstd'].next()
        RSTD(rstd[:, :ntok], psb[bank][:, :ntok], 1.0 / D, [t_ps[bank]], t_rstd)
        shpart = 0 if which == 0 else 3
        for kc in range(KC):
            tmp, t_tmp = R['f32'].next()
            TT(tmp[:, :ntok], xsrc(kc), rstd[:, :ntok], ALU.mult, list(t_xs[kc]) + [t_rstd], [t_tmp])
            if h_out is not None:
                ACT(h_out[:, kc, :ntok], tmp[:, :ntok], AF.Identity, [t_tmp, t_mod], [] if kc else [t_h],
                    scale=modA[:, l, which, kc, j:j + 1], bias=modcol(l, shpart, kc, j), pw=[t_h] if kc else [])
            if f32cb is not None:
                f32cb(kc, tmp, t_tmp)

    def rope(R, z_bf, t_z, ntok, tok0, out_ap, t_out, ropeC, ropeS, t_rope, bank, pw=False):
        MM(psb[bank][:, :ntok], perm_bf, z_bf, True, True, [t_z, t_consts], [t_ps[bank]])
        t1, t_t1 = R['f32'].next()
        TT(t1[:, :ntok], z_bf, ropeC[:, tok0:tok0 + ntok], ALU.mult, [t_z, t_rope], [t_t1])
        t2, t_t2 = R['f32'].next()
        TT(t2[:, :ntok], psb[bank][:, :ntok], ropeS[:, tok0:tok0 + ntok], ALU.mult, [t_ps[bank], t_rope], [t_t2])
        if isinstance(out_ap, tuple):
            TT(out_ap[0], t1[0:64, :ntok], t2[0:64, :ntok], ALU.add, [t_t1, t_t2], [], pw=[t_out])
            TT(out_ap[1], t1[64:128, :ntok], t2[64:128, :ntok], ALU.add, [t_t1, t_t2], [], pw=[t_out])
            return
        TT(out_ap, t1[:, :ntok], t2[:, :ntok], ALU.add, [t_t1, t_t2], [] if pw else [t_out],
           pw=[t_out] if pw else [])

    def wload(dst, src, tk, first=True):
        sch.dma('pool', dst, src, writes=[tk] if first else [], pwrites=[] if first else [tk])

    def winv(l):
        return w_in[l].rearrange("(kc p) n -> p kc n", p=128)

    def phase1(l, with_ctx_u):
        with ExitStack() as sc:
            lsb = mk_sb(sc)
            R = {'bf': Ring(lsb, "p1bf", 4, [128, 512], BF16), 'f32': Ring(lsb, "p1f", 4, [128, 512], F32),
                 'rstd': Ring(lsb, "p1r", 2, [128, 512], F32)}
            wp = lsb("wP1", [128, KC, 1920], BF16)
            t_wp = Tk('wP1')
            wkv = lsb("wukv", [128, 2, 1024], BF16)
            t_wkv = Tk('wukv')
            ropeC = lsb("ropeC", [128, TOK], BF16)
            ropeS = lsb("ropeS", [128, TOK], BF16)
            t_rope = Tk('rope')
            sch.dma('sp', ropeC[:], ropeC_d[:], writes=[t_rope])
            sch.dma('sp', ropeS[:], ropeS_d[:], pwrites=[t_rope])
            wv = winv(l)
            first = True
            for (d0, s0, n) in ((0, C_DAK, 512), (512, C_DAV, 512), (1024, C_KVD, 256), (1280, C_KR, 64),
                                (1344, C_KR, 64), (1408, C_POOL, 512)):
                wload(wp[:, :, d0:d0 + n], wv[:, :, s0:s0 + n], t_wp, first)
                first = False
            wload(wkv[:], w_ukv[l].rearrange("(kc p) n -> p kc n", p=128), t_wkv)
            hring = [(lsb(f"p1h{i}", [128, KC, 512], BF16), Tk(f'p1h{i}')) for i in range(2)]
            kst = lsb("kst", [128, 9, 512], BF16)
            t_kst = Tk('kst')
            vst = lsb("vst", [128, 4, 1024], BF16)
            t_vst = Tk('vst')
            kvn = lsb("kvn", [128, 2, 512], BF16)
            t_kvn = Tk('kvn')
            kvf = lsb("kvf", [128, 2, 512], F32)
            t_kvf = Tk('kvf')
            hst = lsb("hst", [128, 4, 16], BF16)
            t_hst = Tk('hst')
            og, _ = COLS[f'gkv{l}']
            bankrr = [0]

            def nb():
                b = bankrr[0] % 6
                bankrr[0] += 1
                return b

            blocks = [('l', b) for b in range(4)] + [('c', 0)]
            for bi, (kind, b) in enumerate(blocks):
                isc = kind == 'c'
                ntok = CTX if isc else 512
                tok0 = 0 if isc else b * 512
                j = 1 if isc else 0
                if isc:
                    xsrc = lambda kc: xc_sb[:, kc, :]
                    t_xs = [[t_xc[kc]] for kc in range(KC)]
                else:
                    xsrc = lambda kc, tok0=tok0: x_sb[:, kc, tok0:tok0 + 512]
                    t_xs = [[t_x[kc][b]] for kc in range(KC)]
                h, t_h = hring[bi % 2]
                norm_mod(R, xsrc, t_xs, ntok, l, 0, j, h, t_h, bank=7)
                for ci in range(5):
                    c0 = ci * 128 if ci < 4 else 1280
                    bk = nb()
                    for kc in range(KC):
                        MM(psb[bk][:, :ntok], wp[:, kc, c0:c0 + 128], h[:, kc, :ntok], kc == 0, kc == KC - 1,
                           [t_wp, t_h], [t_ps[bk]])
                    dst = kst[:, ci if ci < 4 else 8, :ntok]
                    if isc:
                        CP(dst, psb[bk][:, :ntok], [t_ps[bk]], [], pw=[t_kst])
                    else:
                        z, t_z = R['bf'].next()
                        CP(z[:, :ntok], psb[bk][:, :ntok], [t_ps[bk]], [t_z], eng='act')
                        rope(R, z[:, :ntok], t_z, ntok, tok0, dst, t_kst, ropeC, ropeS, t_rope, nb(), pw=True)
                bks = []
                for ci in range(2):
                    bk = nb()
                    bks.append(bk)
                    for kc in range(KC):
                        MM(psb[bk][:, :ntok], wp[:, kc, 1024 + ci * 128:1024 + (ci + 1) * 128], h[:, kc, :ntok],
                           kc == 0, kc == KC - 1, [t_wp, t_h], [t_ps[bk]])
                    CP(kvf[:, ci, :ntok], psb[bk][:, :ntok], [t_ps[bk]], [], pw=[t_kvf])
                bk = nb()
                for ci in range(2):
                    sq, t_sq = R['bf'].next()
                    ACT(sq[:, :ntok], kvf[:, ci, :ntok], AF.Square, [t_kvf], [t_sq])
                    MM(psb[bk][:, :ntok], ones_bf, sq[:, :ntok], ci == 0, ci == 1, [t_sq, t_consts], [t_ps[bk]])
                rstd, t_rstd = R['rstd'].next()
                RSTD(rstd[:, :ntok], psb[bk][:, :ntok], 1.0 / 256, [t_ps[bk]], t_rstd)
                for ci in range(2):
                    STT(kvn[:, ci, :ntok], kvf[:, ci, :ntok], cols[:, og + ci:og + ci + 1], rstd[:, :ntok],
                        ALU.mult, ALU.mult, [t_kvf, t_rstd, t_cols], [], pw=[t_kvn])
                for hh in range(4):
                    bk = nb()
                    for kc in range(2):
                        MM(psb[bk][:, :ntok], wkv[:, kc, hh * 128:(hh + 1) * 128], kvn[:, kc, :ntok],
                           kc == 0, kc == 1, [t_wkv, t_kvn], [t_ps[bk]])
                    CP(kst[:, 4 + hh, :ntok], psb[bk][:, :ntok], [t_ps[bk]], [], pw=[t_kst])
                for ti in range(ntok // 128):
                    bk = nb()
                    for kc in range(KC):
                        MM(psb[bk][:, :], h[:, kc, ti * 128:(ti + 1) * 128], wp[:, kc, 512:1024],
                           kc == 0, kc == KC - 1, [t_wp, t_h], [t_ps[bk]])
                    CP(vst[:, ti, 0:512], psb[bk][:, :], [t_ps[bk]], [], eng='act', pw=[t_vst])
                    bk = nb()
                    for kc in range(2):
                        MM(psb[bk][:, :], kvn[:, kc, ti * 128:(ti + 1) * 128], wkv[:, kc, 512:1024],
                           kc == 0, kc == 1, [t_wkv, t_kvn], [t_ps[bk]])
                    CP(vst[:, ti, 512:1024], psb[bk][:, :], [t_ps[bk]], [], pw=[t_vst])
                if (not isc) or with_ctx_u:
                    ud = uc_sb if isc else u_sb
                    tu = t_uc if isc else t_u
                    for gi in range(4):
                        bk = nb()
                        for kc in range(KC):
                            MM(psb[bk][:, :ntok], wp[:, kc, 1408 + gi * 128:1408 + (gi + 1) * 128], h[:, kc, :ntok],
                               kc == 0, kc == KC - 1, [t_wp, t_h], [t_ps[bk]])
                        CP(ud[:, gi, 8 + tok0:8 + tok0 + ntok], psb[bk][:, :ntok], [t_ps[bk]], [], eng='act', pw=[tu])
                dk = kvc[l] if isc else kvl[l]
                tdk = t_kvc[l] if isc else t_kvl[l]
                sch.dma('sp', dk[R_KDA:R_KDA + 512, tok0:tok0 + ntok].rearrange("(c p) t -> p c t", p=128),
                        kst[:, 0:4, :ntok], reads=[t_kst], pwrites=[tdk])
                sch.dma('sp', dk[R_KN:R_KN + 512, tok0:tok0 + ntok].rearrange("(c p) t -> p c t", p=128),
                        kst[:, 4:8, :ntok], reads=[t_kst], pwrites=[tdk])
                sch.dma('sp', dk[R_KR:R_KR + 128, tok0:tok0 + ntok], kst[:, 8, :ntok], reads=[t_kst], pwrites=[tdk])
                for (r0, c0) in ((R_VDA, 0), (R_VM, 512)):
                    if isc:
                        vview = dk[r0:r0 + 512, :].rearrange("(t a) c -> t (a c)", a=2)
                        sch.dma('sp', vview[tok0:tok0 + ntok, :].rearrange("(i p) f -> p i f", p=128),
                                vst[:, 0:ntok // 128, c0:c0 + 512], reads=[t_vst], pwrites=[tdk])
                    else:
                        grp, i0 = b // 2, (b % 2) * 4
                        for hh in range(4):
                            rb = r0 + (hh * 2 + grp) * 64
                            blk = dk[rb:rb + 64, :].rearrange("r (a q) -> (r a) q", a=2).rearrange(
                                "p (i f) -> p i f", f=128)
                            sch.dma('sp', blk[:, i0:i0 + 4, :], vst[:, 0:4, c0 + hh * 128:c0 + (hh + 1) * 128],
                                    reads=[t_vst], pwrites=[tdk])
            if 'h' in debug:
                dh = dram_out("dbg_h", [128, KC * 512], BF16)
                sch.dma('sp', dh[:], hring[1][0][:].rearrange("p k t -> p (k t)"), reads=[hring[1][1]])
            if u_out is not None:
                sch.dma('sp', u_out[:], u_sb[:].rearrange("p g t -> p (g t)"), reads=[t_u])
            if uc_out is not None and with_ctx_u:
                sch.dma('sp', uc_out[:], uc_sb[:].rearrange("p g t -> p (g t)"), reads=[t_uc])
            if l in kvl:
                CP(hst[:, :, 0:8], u_sb[:, :, 8:16], [t_u], [t_hst], eng='pool')
                CP(hst[:, :, 8:16], u_sb[:, :, TOK:TOK + 8], [t_u], [], eng='pool', pw=[t_hst])
                sch.dma('sp', kvl[l][R_U:R_U + 4, :].rearrange("g (p t) -> p g t", t=16), hst[:],
                        reads=[t_hst], pwrites=[t_kvl[l]])
        barrier()

    def phase2(l, do_ctx):
        with ExitStack() as sc:
            lsb = mk_sb(sc)
            R = {"bf": Ring(lsb, "p2bf", 3, [128, 512], BF16), "f32": Ring(lsb, "p2f", 3, [128, 512], F32),
                 'rstd': Ring(lsb, "p2r", 2, [128, 512], F32)}
            wsl = Ring(lsb, "wsl", 4, [128, 2048], BF16)
            h = lsb("p2h", [128, KC, 512], BF16)
            t_h = Tk('p2h')
            ropeC = lsb("ropeCq", [128, 512], BF16)
            ropeS = lsb("ropeSq", [128, 512], BF16)
            t_rope = Tk('ropeq')
            qdaA = lsb("qdaA", [128, 4, 512], BF16)
            qdaB = lsb("qdaB", [128, 4, 512], BF16)
            qmr = lsb("qmr", [128, 4, 512], BF16)
            t_qda = Tk('qda')
            sch.op('dve', lambda e: e.memset(qdaA[64:128, :, :], 0.0), pwrites=[t_qda])
            sch.op('dve', lambda e: e.memset(qdaB[0:64, :, :], 0.0), pwrites=[t_qda])
            qm = lsb("qm", [128, 4, 512], BF16)
            t_qm = Tk('qm')
            for hh_ in range(4):
                if hh_ % 2 == 0:
                    sch.op('dve', lambda e, hh_=hh_: e.memset(qmr[64:128, hh_, :], 0.0), pwrites=[t_qm])
                else:
                    sch.op('dve', lambda e, hh_=hh_: e.memset(qmr[0:64, hh_, :], 0.0), pwrites=[t_qm])
            qn = lsb("qn", [128, 3, 512], BF16)
            t_qn = Tk('qn')
            o_da = lsb("o_da", [128, 4, 512], BF16)
            o_mla = lsb("o_mla", [128, 4, 512], BF16)
            o_pool = lsb("o_pool", [128, 4, 512], BF16)
            t_oda, t_omla, t_opool = Tk('oda'), Tk('omla'), Tk('opool')
            da_a = lsb("da_a", [128, 4, 512], BF16)
            t_daa = Tk('daa')
            merged = lsb("merged", [128, KC, 512], BF16)
            t_merged = Tk('merged')
            pring = Ring(lsb, "pT", 4, [128, 512], BF16)
            fA, fB, fC = (lsb(n_, [128, 512], F32) for n_ in ("fA", "fB", "fC"))
            t_fA, t_fB, t_fC = Tk('fA'), Tk('fB'), Tk('fC')
            slots = []
            for i in range(2):
                slots.append(dict(K1=lsb(f"kK1_{i}", [128, 1024], BF16), tK1=Tk(f'kK1_{i}'),
                                  K2=lsb(f"kK2_{i}", [128, 1024], BF16), tK2=Tk(f'kK2_{i}'),
                                  V=lsb(f"kV_{i}", [128, 8, 128], BF16), tV=Tk(f'kV_{i}')))
            pa = lsb("pa", [128, 528], F32)
            pb_ = lsb("pb", [128, 528], F32)
            t_pa, t_pb = Tk('pa'), Tk('pb')
            dbf = lsb("dbf", [128, 512], BF16)
            t_dbf = Tk('dbf')
            wv = winv(l)
            ogq, _ = COLS[f'gq{l}']
            ops_, _ = COLS[f'pscale{l}']
            neglam = misc[:, 4 * l:4 * l + 1]
            sublnS = misc[:, 4 * l + 1:4 * l + 2]

            if ALL:
                hall = lsb("hall", [128, 4, 4, 16], BF16)
                t_hall = Tk('hall')
                hb = NCH * 512
                for r in range(4):
                    sch.dma('sp', hall[:, r, :, :], kvg[l][hb + r * 8:hb + r * 8 + 4, :].rearrange("g (p t) -> p g t", t=16)[:, :, 0:16],
                            reads=[t_kvg[l][NCH]], writes=[t_hall] if r == 0 else [], pwrites=[] if r == 0 else [t_hall])
                oL, _ = COLS['selL']
                oR, _ = COLS['selR']
                for r in range(4):
                    if r == 0:
                        TS(u_sb[:, :, 0:8], hall[:, r, :, 8:16], cols[:, oL + r:oL + r + 1], None, ALU.mult, None,
                           [t_hall, t_cols], [], pw=[t_u])
                        TS(u_sb[:, :, TOK + 8:TOK + 16], hall[:, r, :, 0:8], cols[:, oR + r:oR + r + 1], None, ALU.mult, None,
                           [t_hall, t_cols], [], pw=[t_u])
                    else:
                        STT(u_sb[:, :, 0:8], hall[:, r, :, 8:16], cols[:, oL + r:oL + r + 1], u_sb[:, :, 0:8],
                            ALU.mult, ALU.add, [t_hall, t_cols, t_u], [], pw=[t_u])
                        STT(u_sb[:, :, TOK + 8:TOK + 16], hall[:, r, :, 0:8], cols[:, oR + r:oR + r + 1],
                            u_sb[:, :, TOK + 8:TOK + 16], ALU.mult, ALU.add, [t_hall, t_cols, t_u], [], pw=[t_u])
            if l in halo:
                sch.dma('sp', u_sb[:, :, 0:8], halo[l][:, :, 0:8], pwrites=[t_u])
                sch.dma('sp', u_sb[:, :, TOK + 8:TOK + 16], halo[l][:, :, 8:16], pwrites=[t_u])
            if do_ctx:
                sch.op('dve', lambda e: e.memset(uc_sb[:, :, 0:8], 0.0), pwrites=[t_uc])
                sch.op('dve', lambda e: e.memset(uc_sb[:, :, CTX + 8:CTX + 16], 0.0), pwrites=[t_uc])

            def load_group(kind, hh, g, slot):
                if kind == 'da':
                    rk, rv = R_KDA, R_VDA
                else:
                    rk, rv = R_KN, R_VM
                if g[0] == 'c':
                    src, tsrc = kvc[l], [t_kvc[l]]
                    vv = src[rv:rv + 512, :].rearrange("(t a) c -> t (a c)", a=2)
                    sch.dma('sp', slot['K1'][:, :CTX], src[rk + hh * 128:rk + (hh + 1) * 128, :],
                            reads=tsrc, writes=[slot['tK1']])
                    if kind == 'mla':
                        sch.dma('sp', slot['K2'][:, :CTX], src[R_KR:R_KR + 128, :], reads=tsrc, writes=[slot['tK2']])
                    sch.dma('sp', slot['V'][:, :CTX // 128, :],
                            vv[:, hh * 128:(hh + 1) * 128].rearrange("(i p) f -> p i f", p=128),
                            reads=tsrc, writes=[slot['tV']])
                    return
                _, r, hf = g
                src = kvg[l]
                col0 = hf * 1024

                def rows(row0):
                    c = row0 // 128
                    return src[c * 512 + r * 128:c * 512 + (r + 1) * 128, :], t_kvg[l][c]
                ap, tk = rows(rk + hh * 128)
                sch.dma('sp', slot['K1'][:, :1024], ap[:, col0:col0 + 1024], reads=[tk], writes=[slot['tK1']])
                if kind == 'mla':
                    ap, tk = rows(R_KR)
                    sch.dma('sp', slot['K2'][:, :1024], ap[:, col0:col0 + 1024], reads=[tk], writes=[slot['tK2']])
                rb = rv + (hh * 2 + hf) * 64
                c = rb // 128
                o_ = c * 512 + r * 128 + (rb % 128)
                blk = src[o_:o_ + 64, :].rearrange("r (a q) -> (r a) q", a=2).rearrange("p (i f) -> p i f", f=128)
                sch.dma('sp', slot['V'][:, :, :], blk, reads=[t_kvg[l][c]], writes=[slot['tV']])

            def attention(nq, groups):
                loads = [(kind, hh, g) for kind in ('da', 'mla') for hh in range(4) for g in groups]
                issued = [0]

                def ensure(n):
                    while issued[0] < min(n, len(loads)):
                        k_, h_, g_ = loads[issued[0]]
                        load_group(k_, h_, g_, slots[issued[0] % 2])
                        issued[0] += 1
                ensure(1)
                idx = 0
                for kind in ('da', 'mla'):
                    for hh in range(4):
                        tiles = []
                        for g in groups:
                            nt = 2 if g[0] == 'c' else 8
                            tiles += [(idx, tt) for tt in range(nt)]
                            idx += 1
                        n = len(tiles)
                        if kind == 'da':
                            def S1(i):
                                gi, tt = tiles[i]
                                if tt == 0:
                                    ensure(gi + 2)
                                sl = slots[gi % 2]
                                bk = 0 if i % 2 == 0 else 6
                                MM(psb[bk][:, :nq], sl['K1'][:, tt * 128:(tt + 1) * 128], qdaA[:, hh, :nq],
                                   True, True, [sl['tK1'], t_qda], [t_ps[bk]])

                            def S2(i):
                                gi, tt = tiles[i]
                                sl = slots[gi % 2]
                                bk = 1 if i % 2 == 0 else 7
                                MM(psb[bk][:, :nq], sl['K1'][:, tt * 128:(tt + 1) * 128], qdaB[:, hh, :nq],
                                   True, True, [sl['tK1'], t_qda], [t_ps[bk]])
                            S1(0)
                            S2(0)
                            for i in range(n):
                                gi, tt = tiles[i]
                                sl = slots[gi % 2]
                                p1, t_p1 = pring.next()
                                p2, t_p2 = pring.next()
                                b1, b2 = (0, 1) if i % 2 == 0 else (6, 7)
                                ACT(p1[:, :nq], psb[b1][:, :nq], AF.Exp, [t_ps[b1]], [t_p1], scale=0.125)
                                ACT(p2[:, :nq], psb[b2][:, :nq], AF.Exp, [t_ps[b2]], [t_p2], scale=0.125)
                                if i + 1 < n:
                                    S1(i + 1)
                                    S2(i + 1)
                                MM(psb[2][:, :nq], sl['V'][:, tt, :], p1[:, :nq], i == 0, i == n - 1,
                                   [sl['tV'], t_p1], [t_ps[2]])
                                MM(psb[3][:, :nq], sl['V'][:, tt, :], p2[:, :nq], i == 0, i == n - 1,
                                   [sl['tV'], t_p2], [t_ps[3]])
                                MM(psb[4][:, :nq], ones_bf, p1[:, :nq], i == 0, i == n - 1, [t_p1, t_consts], [t_ps[4]])
                                if i == 0:
                                    CP(pb_[:, :nq], p2[:, :nq], [t_p2], [t_pb])
                                else:
                                    TT(pb_[:, :nq], pb_[:, :nq], p2[:, :nq], ALU.add, [t_pb, t_p2], [], pw=[t_pb])
                            MM(psb[5][:, :nq], ones_f, pb_[:, :nq], True, True, [t_pb, t_consts], [t_ps[5]])
                            RECIP(fA[:, :nq], psb[4][:, :nq], [t_ps[4]], [t_fA])
                            TT(fB[:, :nq], psb[2][:, :nq], fA[:, :nq], ALU.mult, [t_ps[2], t_fA], [t_fB])
                            RECIP(fA[:, :nq], psb[5][:, :nq], [t_ps[5]], [t_fA])
                            TT(fC[:, :nq], psb[3][:, :nq], fA[:, :nq], ALU.mult, [t_ps[3], t_fA], [t_fC])
                            STT(da_a[:, hh, :nq], fC[:, :nq], neglam, fB[:, :nq], ALU.mult, ALU.add,
                                [t_fC, t_fB, t_misc], [], pw=[t_daa])
                        else:
                            pbase = 64 * (hh % 2)

                            def SM(i):
                                gi, tt = tiles[i]
                                if tt == 0:
                                    ensure(gi + 2)
                                sl = slots[gi % 2]
                                bk = i % 2
                                MM(psb[bk][:, :nq], sl['K1'][:, tt * 128:(tt + 1) * 128], qm[:, hh, :nq],
                                   True, False, [sl['tK1'], t_qm], [t_ps[bk]])
                                MM(psb[bk][:, :nq], sl['K2'][:, tt * 128:(tt + 1) * 128],
                                   qmr[:, hh, :nq], False, True, [sl['tK2'], t_qm], [t_ps[bk]])
                            SM(0)
                            for i in range(n):
                                gi, tt = tiles[i]
                                sl = slots[gi % 2]
                                if i + 1 < n:
                                    SM(i + 1)
                                p1, t_p1 = pring.next()
                                ACT(p1[:, :nq], psb[i % 2][:, :nq], AF.Exp, [t_ps[i % 2]], [t_p1], scale=192.0 ** -0.5)
                                MM(psb[2][:, :nq], sl['V'][:, tt, :], p1[:, :nq], i == 0, i == n - 1,
                                   [sl['tV'], t_p1], [t_ps[2]])
                                if i == 0:
                                    CP(pa[:, :nq], p1[:, :nq], [t_p1], [t_pa])
                                else:
                                    TT(pa[:, :nq], pa[:, :nq], p1[:, :nq], ALU.add, [t_pa, t_p1], [], pw=[t_pa])
                            MM(psb[4][:, :nq], ones_f, pa[:, :nq], True, True, [t_pa, t_consts], [t_ps[4]])
                            RECIP(fA[:, :nq], psb[4][:, :nq], [t_ps[4]], [t_fA])
                            TT(o_mla[:, hh, :nq], psb[2][:, :nq], fA[:, :nq], ALU.mult, [t_ps[2], t_fA], [],
                               pw=[t_omla])
                for hh in range(4):
                    sq, t_sq = R['bf'].next()
                    TT(sq[:, :nq], da_a[:, hh, :nq], da_a[:, hh, :nq], ALU.mult, [t_daa], [t_sq])
                    MM(psb[6][:, :nq], ones_bf, sq[:, :nq], True, True, [t_sq, t_consts], [t_ps[6]])
                    rstd, t_rstd = R['rstd'].next()
                    RSTD(rstd[:, :nq], psb[6][:, :nq], 1.0 / 128, [t_ps[6]], t_rstd)
                    STT(o_da[:, hh, :nq], da_a[:, hh, :nq], sublnS, rstd[:, :nq], ALU.mult, ALU.mult,
                        [t_daa, t_rstd, t_misc], [], pw=[t_oda])

            blocks = ([('c', 0)] if do_ctx else []) + [('l', b) for b in range(4)]
            bankrr = [0]

            def nb():
                b = bankrr[0] % 6
                bankrr[0] += 1
                return b

            for (kind, b) in blocks:
                isc = kind == 'c'
                nq = CTX if isc else 512
                tok0 = 0 if isc else b * 512
                j = 1 if isc else 0
                if isc:
                    xsrc = lambda kc: xc_sb[:, kc, :]
                    xdst = lambda kc: xc_sb[:, kc, :]
                    t_xs = [[t_xc[kc]] for kc in range(KC)]
                else:
                    xsrc = lambda kc, tok0=tok0: x_sb[:, kc, tok0:tok0 + 512]
                    xdst = xsrc
                    t_xs = [[t_x[kc][b]] for kc in range(KC)]
                norm_mod(R, xsrc, t_xs, nq, l, 0, j, h, t_h, bank=7)
                if not isc:
                    sch.dma('sp', ropeC[:], ropeC_d[:, tok0:tok0 + 512], writes=[t_rope])
                    sch.dma('sp', ropeS[:], ropeS_d[:, tok0:tok0 + 512], pwrites=[t_rope])
                for half in range(2):
                    ws, t_ws = wsl.next()
                    wq = ws[:, :].rearrange("p (k n) -> p k n", k=KC)
                    wload(wq, wv[:, :, C_DAQ + half * 256:C_DAQ + (half + 1) * 256], t_ws)
                    for dd in range(2):
                        hh = half * 2 + dd
                        bk = nb()
                        for kc in range(KC):
                            MM(psb[bk][:, :nq], wq[:, kc, dd * 128:(dd + 1) * 128], h[:, kc, :nq], kc == 0, kc == KC - 1,
                               [t_ws, t_h], [t_ps[bk]])
                        if isc:
                            CP(qdaA[0:64, hh, :nq], psb[bk][0:64, :nq], [t_ps[bk]], [], pw=[t_qda])
                            CP(qdaB[64:128, hh, :nq], psb[bk][64:128, :nq], [t_ps[bk]], [], pw=[t_qda])
                        else:
                            z, t_z = R['bf'].next()
                            CP(z[:, :nq], psb[bk][:, :nq], [t_ps[bk]], [t_z], eng='act')
                            rope(R, z[:, :nq], t_z, nq, 0, (qdaA[0:64, hh, :nq], qdaB[64:128, hh, :nq]), t_qda,
                                 ropeC, ropeS, t_rope, nb(), pw=True)
                qbanks = []
                for (c0, ncol) in ((0, 256), (256, 128)):
                    ws, t_ws = wsl.next()
                    wq = ws[:, 0:KC * ncol].rearrange("p (k n) -> p k n", k=KC)
                    wload(wq, wv[:, :, C_QD + c0:C_QD + c0 + ncol], t_ws)
                    for dd in range(ncol // 128):
                        bk = nb()
                        qbanks.append(bk)
                        for kc in range(KC):
                            MM(psb[bk][:, :nq], wq[:, kc, dd * 128:(dd + 1) * 128], h[:, kc, :nq], kc == 0, kc == KC - 1,
                               [t_ws, t_h], [t_ps[bk]])
                for ci, bk in enumerate(qbanks):
                    sq, t_sq = R['bf'].next()
                    ACT(sq[:, :nq], psb[bk][:, :nq], AF.Square, [t_ps[bk]], [t_sq])
                    MM(psb[6][:, :nq], ones_bf, sq[:, :nq], ci == 0, ci == 2, [t_sq, t_consts], [t_ps[6]])
                rstd, t_rstd = R['rstd'].next()
                RSTD(rstd[:, :nq], psb[6][:, :nq], 1.0 / 384, [t_ps[6]], t_rstd)
                for ci, bk in enumerate(qbanks):
                    STT(qn[:, ci, :nq], psb[bk][:, :nq], cols[:, ogq + ci:ogq + ci + 1], rstd[:, :nq],
                        ALU.mult, ALU.mult, [t_ps[bk], t_rstd, t_cols], [], pw=[t_qn])
                wuv = w_uq[l].rearrange("(kc p) n -> p kc n", p=128)
                ws, t_ws = wsl.next()
                wun = ws[:, 0:3 * 512].rearrange("p (k n) -> p k n", k=3)
                wload(wun, wuv[:, :, 0:512], t_ws)
                ws2, t_ws2 = wsl.next()
                wur = ws2[:, 0:3 * 256].rearrange("p (k n) -> p k n", k=3)
                wload(wur, wuv[:, :, 512:768], t_ws2)
                for hh in range(4):
                    bk = nb()
                    for kc in range(3):
                        MM(psb[bk][:, :nq], wun[:, kc, hh * 128:(hh + 1) * 128], qn[:, kc, :nq], kc == 0, kc == 2,
                           [t_ws, t_qn], [t_ps[bk]])
                    CP(qm[:, hh, :nq], psb[bk][:, :nq], [t_ps[bk]], [], pw=[t_qm])
                for rc in range(2):
                    bk = nb()
                    for kc in range(3):
                        MM(psb[bk][:, :nq], wur[:, kc, rc * 128:(rc + 1) * 128], qn[:, kc, :nq], kc == 0, kc == 2,
                           [t_ws2, t_qn], [t_ps[bk]])
                    if isc:
                        CP(qmr[0:64, 2 * rc, :nq], psb[bk][0:64, :nq], [t_ps[bk]], [], pw=[t_qm])
                        CP(qmr[64:128, 2 * rc + 1, :nq], psb[bk][64:128, :nq], [t_ps[bk]], [], pw=[t_qm])
                    else:
                        z, t_z = R['bf'].next()
                        CP(z[:, :nq], psb[bk][:, :nq], [t_ps[bk]], [t_z], eng='act')
                        rope(R, z[:, :nq], t_z, nq, 0, (qmr[0:64, 2 * rc, :nq], qmr[64:128, 2 * rc + 1, :nq]), t_qm,
                             ropeC, ropeS, t_rope, nb(), pw=True)
                groups = [('c',)] + ([] if isc else [('l', r, hf) for r in range(4) for hf in range(2)])
                attention(nq, groups)
                usrc = uc_sb if isc else u_sb
                tus = t_uc if isc else t_u
                E = nq + 16
                ws, t_ws = wsl.next()
                wpl = ws[:, 0:512].rearrange("p (g d) -> p g d", g=4)
                wload(wpl, pool_w[l].rearrange("g c d -> c g d"), t_ws)
                fixname = 'pfixc' if isc else 'pfix'
                ofx, _ = COLS[fixname]
                for gi, w in enumerate(POOL_WINDOWS):
                    ue = usrc[:, gi, tok0:tok0 + E]
                    TT(pa[:, 1:E], ue[:, 0:E - 1], ue[:, 1:E], ALU.add, [tus], [t_pa])
                    cur, tcur = pa, t_pa
                    if w >= 4:
                        TT(pb_[:, 2:E - 1], pa[:, 1:E - 2], pa[:, 3:E], ALU.add, [t_pa], [t_pb])
                        cur, tcur = pb_, t_pb
                    if w >= 8:
                        TT(pa[:, 4:E - 3], pb_[:, 2:E - 5], pb_[:, 6:E - 1], ALU.add, [t_pb], [t_pa])
                        cur, tcur = pa, t_pa
                    if w >= 16:
                        TT(pb_[:, 8:E - 7], pa[:, 4:E - 11], pa[:, 12:E - 3], ALU.add, [t_pa], [t_pb])
                        cur, tcur = pb_, t_pb
                    if isc or b == 0:
                        TT(cur[:, 8:16], cur[:, 8:16], cols[:, ofx + gi * 16:ofx + gi * 16 + 8], ALU.mult,
                           [tcur, t_cols], [], pw=[tcur])
                    if isc or b == 3:
                        TT(cur[:, nq:nq + 8], cur[:, nq:nq + 8], cols[:, ofx + gi * 16 + 8:ofx + gi * 16 + 16], ALU.mult,
                           [tcur, t_cols], [], pw=[tcur])
                    STT(dbf[:, :nq], cur[:, 8:8 + nq], 1.0 / w, ue[:, 8:8 + nq], ALU.mult, ALU.subtract,
                        [tcur, tus], [t_dbf])
                    bk = nb()
                    MM(psb[bk][:, :nq], wpl[:, gi, :], dbf[:, :nq], True, True, [t_ws, t_dbf], [t_ps[bk]])
                    TS(o_pool[:, gi, :nq], psb[bk][:, :nq], cols[:, ops_ + gi:ops_ + gi + 1], None, ALU.mult, None,
                       [t_ps[bk], t_cols], [], pw=[t_opool])
                wbv = w_branch[l].rearrange("n (cc p) d -> p n cc d", p=128)
                for n_, (o_n, t_on) in enumerate(((o_da, t_oda), (o_mla, t_omla), (o_pool, t_opool))):
                    for half in range(2):
                        ws, t_wb = wsl.next()
                        wb = ws[:, :].rearrange("p (c d) -> p c d", c=4)
                        wload(wb, wbv[:, n_, :, half * 512:(half + 1) * 512], t_wb)
                        for quarter in range(2):
                            dpair = half * 2 + quarter
                            ws2, t_wg = wsl.next()
                            wg = ws2[:, :].rearrange("p (k n) -> p k n", k=KC)
                            wload(wg, wv[:, :, C_GATE + n_ * 1024 + dpair * 256:C_GATE + n_ * 1024 + (dpair + 1) * 256], t_wg)
                            for dd in range(2):
                                dch = dpair * 2 + dd
                                bg = nb()
                                for kc in range(KC):
                                    MM(psb[bg][:, :nq], wg[:, kc, dd * 128:(dd + 1) * 128], h[:, kc, :nq],
                                       kc == 0, kc == KC - 1, [t_wg, t_h], [t_ps[bg]])
                                sig, t_sig = R['bf'].next()
                                ACT(sig[:, :nq], psb[bg][:, :nq], AF.Sigmoid, [t_ps[bg]], [t_sig])
                                bp = nb()
                                for cc in range(4):
                                    MM(psb[bp][:, :nq], wb[:, cc, (dch % 4) * 128:(dch % 4 + 1) * 128], o_n[:, cc, :nq],
                                       cc == 0, cc == 3, [t_wb, t_on], [t_ps[bp]])
                                if n_ == 0:
                                    TT(merged[:, dch, :nq], sig[:, :nq], psb[bp][:, :nq], ALU.mult, [t_sig, t_ps[bp]], [],
                                       pw=[t_merged])
                                else:
                                    tmp, t_tmp = R['f32'].next()
                                    TT(tmp[:, :nq], sig[:, :nq], psb[bp][:, :nq], ALU.mult, [t_sig, t_ps[bp]], [t_tmp])
                                    TT(merged[:, dch, :nq], merged[:, dch, :nq], tmp[:, :nq], ALU.add, [t_merged, t_tmp], [],
                                       pw=[t_merged])
                wov = w_out[l].rearrange("(kc p) n -> p kc n", p=128)
                for q4 in range(4):
                    ws, t_wo = wsl.next()
                    wo = ws[:, :].rearrange("p (k n) -> p k n", k=KC)
                    wload(wo, wov[:, :, q4 * 256:(q4 + 1) * 256], t_wo)
                    for dd in range(2):
                        dco = q4 * 2 + dd
                        bk = nb()
                        for kc in range(KC):
                            MM(psb[bk][:, :nq], wo[:, kc, dd * 128:(dd + 1) * 128], merged[:, kc, :nq],
                               kc == 0, kc == KC - 1, [t_wo, t_merged], [t_ps[bk]])
                        STT(xdst(dco), psb[bk][:, :nq], modcol(l, 2, dco, j), xsrc(dco), ALU.mult, ALU.add,
                            [t_ps[bk], t_mod] + t_xs[dco], [], pw=t_xs[dco])
                if 'mix' in debug and (not isc) and b == 0:
                    dmg = dram_out("dbg_merged", [128, KC * 512], BF16)
                    sch.dma('sp', dmg[:], merged[:].rearrange("p k t -> p (k t)"), reads=[t_merged])
                    dod = dram_out("dbg_o", [128, 3 * 4 * 512], BF16)
                    sch.dma('sp', dod[:, 0:2048], o_da[:].rearrange("p k t -> p (k t)"), reads=[t_oda])
                    sch.dma('sp', dod[:, 2048:4096], o_mla[:].rearrange("p k t -> p (k t)"), reads=[t_omla])
                    sch.dma('sp', dod[:, 4096:6144], o_pool[:].rearrange("p k t -> p (k t)"), reads=[t_opool])
        barrier()

    def phase3(l, do_ctx):
        moe = (l % 2 == 1)
        with ExitStack() as sc:
            lsb = mk_sb(sc)
            R = {'bf': Ring(lsb, "p3bf", 3, [128, 512], BF16), 'f32': Ring(lsb, "p3f", 4, [128, 512], F32),
                 'rstd': Ring(lsb, "p3r", 2, [128, 512], F32)}
            NT = TOK + (CTX if do_ctx else 0)
            h2 = lsb("h2", [128, KC, NT], BF16)
            t_h2 = Tk('h2')
            wsl = Ring(lsb, "w3", 5, [128, 4096], BF16)
            actb = [(lsb(f"actb{i}", [128, 4, 512], BF16), Tk(f'actb{i}')) for i in range(2)]
            blocks = [('l', b) for b in range(4)] + ([('c', 0)] if do_ctx else [])
            if moe:
                router_sb = lsb("router", [128, KC, NEXP], F32)
                t_router = Tk('router')
                sch.dma('sp', router_sb[:], router_d.rearrange("(kc p) e -> p kc e", p=128), writes=[t_router])
                gates = lsb("gates", [128, 16, NEXP], F32)
                gtmp = lsb("gtmp", [128, 16, NEXP], F32)
                gm = lsb("gm", [128, 16], F32)
                t_gates = Tk('gates')
                gbc = [(lsb(f"gbc{i}", [128, TOK], BF16), Tk(f'gbc{i}')) for i in range(2)]
                gmat = [(lsb(f"gmat{i}", [128, 128], F32), Tk(f'gmat{i}')) for i in range(2)]
                lgT = lsb("lgT", [32, 512], F32)
                t_lgT = Tk('lgT')
                sch.op('dve', lambda e: e.memset(lgT[:], 0.0), writes=[t_lgT])
            for (kind, b) in blocks:
                isc = kind == 'c'
                ntok = CTX if isc else 512
                j = 1 if isc else 0
                off = TOK if isc else b * 512
                if isc:
                    xsrc = lambda kc: xc_sb[:, kc, :]
                    t_xs = [[t_xc[kc]] for kc in range(KC)]
                else:
                    xsrc = lambda kc, off=off: x_sb[:, kc, off:off + 512]
                    t_xs = [[t_x[kc][b]] for kc in range(KC)]
                cb = None
                if moe:
                    def cb(kc, tmp, t_tmp, b=b, j=j):
                        hf, t_hf = R['f32'].next()
                        ACT(hf[:, :512], tmp[:, :512], AF.Identity, [t_tmp, t_mod], [t_hf],
                            scale=modA[:, l, 1, kc, j:j + 1], bias=modcol(l, 3, kc, j))
                        MM(psb[6][0:NEXP, :], router_sb[:, kc, :], hf[:, :512], kc == 0, kc == KC - 1,
                           [t_hf, t_router], [t_ps[6]])
                        if kc == KC - 1:
                            CP(lgT[0:NEXP, :], psb[6][0:NEXP, :], [t_ps[6]], [], pw=[t_lgT])
                            for ti in range(4):
                                c0 = (b * 4 + ti) * NEXP
                                MM(psb[5][:, c0:c0 + NEXP], lgT[0:32, ti * 128:(ti + 1) * 128],
                                   ident_f[0:32, 0:NEXP], True, True, [t_lgT, t_consts], [t_ps[5]])
                norm_mod(R, xsrc, t_xs, ntok, l, 1, j, h2[:, :, off:off + ntok], t_h2, f32cb=cb, bank=7)
            if moe:
                lg = psb[5][:, 0:16 * NEXP].rearrange("p (t e) -> p t e", e=NEXP)
                CP(gates[:], lg, [t_ps[5]], [t_gates])
                sch.op('dve', lambda e: e.tensor_reduce(out=gm[:], in_=gates[:], axis=mybir.AxisListType.X, op=ALU.max),
                       reads=[t_gates], pwrites=[t_gates])
                gmb = gm[:, :, None].to_broadcast([128, 16, NEXP])
                TT(gtmp[:], gates[:], gmb, ALU.is_equal, [t_gates], [], pw=[t_gates])
                STT(gtmp[:], gtmp[:], -BIG, gates[:], ALU.mult, ALU.add, [t_gates], [], pw=[t_gates])
                gm2 = lsb("gm2", [128, 16], F32)
                sch.op('dve', lambda e: e.tensor_reduce(out=gm2[:], in_=gtmp[:], axis=mybir.AxisListType.X, op=ALU.max),
                       reads=[t_gates], pwrites=[t_gates])
                gm2b = gm2[:, :, None].to_broadcast([128, 16, NEXP])
                TT(gtmp[:], gates[:], gm2b, ALU.is_ge, [t_gates], [], pw=[t_gates])
                TT(gates[:], gates[:], gmb, ALU.subtract, [t_gates], [], pw=[t_gates])
                ACT(gates[:], gates[:], AF.Exp, [t_gates], [], pw=[t_gates])
                TT(gates[:], gates[:], gtmp[:], ALU.mult, [t_gates], [], pw=[t_gates])
                sch.op('dve', lambda e: e.tensor_reduce(out=gm[:], in_=gates[:], axis=mybir.AxisListType.X, op=ALU.add),
                       reads=[t_gates], pwrites=[t_gates])
                sch.op('dve', lambda e: e.reciprocal(out=gm[:], in_=gm[:]), reads=[t_gates], pwrites=[t_gates])
                TT(gates[:], gates[:], gmb, ALU.mult, [t_gates], [], pw=[t_gates])
                if 'gates' in debug:
                    dg = dram_out("dbg_gates", [128, 16 * NEXP])
                    sch.dma('sp', dg[:], gates[:].rearrange("p t e -> p (t e)"), reads=[t_gates])

            def ffn_expert(wgv, wuv, wdv, dff, gb, t_gb):
                nfg = (dff + 511) // 512
                bi = 0
                for fg in range(nfg):
                    nf = min(512, dff - fg * 512)
                    nfc = nf // 128
                    ws, t_wg = wsl.next()
                    wg = ws[:, 0:KC * nf].rearrange("p (k n) -> p k n", k=KC)
                    wload(wg, wgv[:, :, fg * 512:fg * 512 + nf], t_wg)
                    ws, t_wu = wsl.next()
                    wu = ws[:, 0:KC * nf].rearrange("p (k n) -> p k n", k=KC)
                    wload(wu, wuv[:, :, fg * 512:fg * 512 + nf], t_wu)
                    ws, t_wd = wsl.next()
                    wd = ws[:, 0:nfc * 1024].rearrange("p (c n) -> p c n", c=nfc)
                    wload(wd, wdv[:, fg * 4:fg * 4 + nfc, :], t_wd)
                    for (kind, b) in blocks:
                        isc = kind == 'c'
                        ntok = CTX if isc else 512
                        j = 1 if isc else 0
                        off = TOK if isc else b * 512
                        t_xs = [[t_xc[kc]] for kc in range(KC)] if isc else [[t_x[kc][b]] for kc in range(KC)]
                        xs = (lambda kc: xc_sb[:, kc, :]) if isc else (lambda kc, off=off: x_sb[:, kc, off:off + 512])
                        ab, t_ab = actb[bi % 2]
                        bi += 1
                        for fc in range(nfc):
                            pg, pu = (0, 1) if fc % 2 == 0 else (2, 3)
                            for kc in range(KC):
                                MM(psb[pg][:, :ntok], wg[:, kc, fc * 128:(fc + 1) * 128], h2[:, kc, off:off + ntok],
                                   kc == 0, kc == KC - 1, [t_wg, t_h2], [t_ps[pg]])
                            for kc in range(KC):
                                MM(psb[pu][:, :ntok], wu[:, kc, fc * 128:(fc + 1) * 128], h2[:, kc, off:off + ntok],
                                   kc == 0, kc == KC - 1, [t_wu, t_h2], [t_ps[pu]])
                            sg, t_sg = R['f32'].next()
                            ACT(sg[:, :ntok], psb[pg][:, :ntok], AF.Silu, [t_ps[pg]], [t_sg])
                            if gb is None:
                                TT(ab[:, fc, :ntok], sg[:, :ntok], psb[pu][:, :ntok], ALU.mult, [t_sg, t_ps[pu]], [],
                                   pw=[t_ab])
                            else:
                                TT(sg[:, :ntok], sg[:, :ntok], psb[pu][:, :ntok], ALU.mult, [t_sg, t_ps[pu]], [t_sg])
                                TT(ab[:, fc, :ntok], sg[:, :ntok], gb[:, off:off + ntok], ALU.mult, [t_sg, t_gb], [],
                                   pw=[t_ab])
                        for dch in range(KC):
                            py = 4 + (dch % 2)
                            for fc in range(nfc):
                                MM(psb[py][:, :ntok], wd[:, fc, dch * 128:(dch + 1) * 128], ab[:, fc, :ntok],
                                   fc == 0, fc == nfc - 1, [t_wd, t_ab], [t_ps[py]])
                            STT(xs(dch), psb[py][:, :ntok], modcol(l, 5, dch, j), xs(dch), ALU.mult, ALU.add,
                                [t_ps[py], t_mod] + t_xs[dch], [], pw=t_xs[dch])

            if not moe:
                ffn_expert(ffn_wg.rearrange("(kc p) n -> p kc n", p=128), ffn_wu.rearrange("(kc p) n -> p kc n", p=128),
                           ffn_wd.rearrange("(c p) n -> p c n", p=128), D_FF, None, None)
            else:
                for ex in range(NEXP):
                    gb, t_gb = gbc[ex % 2]
                    for ti in range(16):
                        gmt, t_gmt = gmat[ti % 2]
                        TS(gmt[:], ones_f, gates[:, ti, ex:ex + 1], None, ALU.mult, None, [t_consts, t_gates], [t_gmt])
                        bk = 6 + (ti // 4) % 2
                        MM(psb[bk][:, (ti % 4) * 128:(ti % 4 + 1) * 128], gmt[:], ident_f, True, True,
                           [t_gmt, t_consts], [t_ps[bk]])
                        if ti % 4 == 3:
                            CP(gb[:, (ti // 4) * 512:(ti // 4 + 1) * 512], psb[bk][:, :], [t_ps[bk]], [],
                               pw=[t_gb])
                    ffn_expert(moe_wg[ex].rearrange("(kc p) n -> p kc n", p=128),
                               moe_wu[ex].rearrange("(kc p) n -> p kc n", p=128),
                               moe_wd[ex].rearrange("(c p) n -> p c n", p=128), D_FFE, gb, t_gb)
        barrier()

    def final_norm():
        with ExitStack() as sc:
            lsb = mk_sb(sc)
            R = {'bf': Ring(lsb, "fnbf", 3, [128, 512], BF16), 'f32': Ring(lsb, "fnf", 4, [128, 512], F32),
                 'rstd': Ring(lsb, "fnr", 2, [128, 512], F32)}
            ogf, _ = COLS['gfin']
            for b in range(4):
                off = b * 512
                for kc in range(KC):
                    sq, t_sq = R['bf'].next()
                    ACT(sq[:], x_sb[:, kc, off:off + 512], AF.Square, [t_x[kc][b]], [t_sq])
                    MM(psb[7][:], ones_bf, sq[:], kc == 0, kc == KC - 1, [t_sq, t_consts], [t_ps[7]])
                rstd, t_rstd = R['rstd'].next()
                RSTD(rstd[:], psb[7][:], 1.0 / D, [t_ps[7]], t_rstd)
                for kc in range(KC):
                    STT(x_sb[:, kc, off:off + 512], x_sb[:, kc, off:off + 512], cols[:, ogf + kc:ogf + kc + 1], rstd[:],
                        ALU.mult, ALU.mult, [t_x[kc][b], t_rstd, t_cols], [], pw=[t_x[kc][b]])
        barrier()

    def gather(l):
        for c in range(NCH + 1):
            if c < NCH:
                src = kvl[l][c * 128:(c + 1) * 128, :]
                dst = kvg[l][c * 512:(c + 1) * 512, :]
            else:
                src = kvl[l][R_U:R_U + 8, :]
                dst = kvg[l][NCH * 512:NCH * 512 + 32, :]
            sch.coll((lambda e, src=src, dst=dst: e.collective_compute(
                "AllGather", ALU.bypass, replica_groups=[[0, 1, 2, 3], [4, 5, 6, 7]], ins=[src], outs=[dst])),
                reads=[t_kvl[l]], writes=[t_kvg[l][c]])

    if A_:
        phase1(0, True)
    if B_:
        phase2(0, True)
        phase3(0, True)
        phase1(1, False)
        x1T = dram_out("x1T", [D, TOK])
        t_o1 = Tk('o1')
        for kc in range(KC):
            sch.dma('sp', x1T[kc * 128:(kc + 1) * 128, :], x_sb[:, kc, :], reads=t_x[kc], pwrites=[t_o1])
    if ALL:
        phase1(0, True)
        gather(0)
        mods_stage(MODS_REST, False)
        phase2(0, True)
        phase3(0, True)
        phase1(1, False)
        gather(1)
        phase2(1, False)
        phase3(1, False)
        final_norm()
    if C_:
        phase2(1, False)
        if 'xa' in debug:
            dxa = dram_out("dbg_xa", [D, TOK])
            for kc in range(KC):
                sch.dma('sp', dxa[kc * 128:(kc + 1) * 128, :], x_sb[:, kc, :], reads=t_x[kc])
            barrier()
        phase3(1, False)
        final_norm()

    if 'mod' in debug:
        dmod = dram_out("dbg_mod", [128, DEPTH * 96])
        sch.dma('sp', dmod[:], mod_sb[:].rearrange("p l m j -> p (l m j)"), reads=[t_mod])
        dmisc = dram_out("dbg_misc", [128, 16])
        sch.dma('sp', dmisc[:], misc[:], reads=[t_misc])
    if 'u' in debug:
        du = dram_out("dbg_u", [128, 4 * (TOK + 16)], BF16)
        sch.dma('sp', du[:], u_sb[:].rearrange("p g t -> p (g t)"), reads=[t_u])
    if 'x' in debug:
        dx = dram_out("dbg_x", [D, TOK])
        for kc in range(KC):
            sch.dma('sp', dx[kc * 128:(kc + 1) * 128, :], x_sb[:, kc, :], reads=t_x[kc])
        dxc = dram_out("dbg_xc", [D, CTX])
        for kc in range(KC):
            sch.dma('sp', dxc[kc * 128:(kc + 1) * 128, :], xc_sb[:, kc, :], reads=[t_xc[kc]])

    t_out = Tk('out')
    if C_ or ALL:
        outT = dram_out("outT", [D, TOK])
        for kc in range(KC):
            sch.dma('sp', outT[kc * 128:(kc + 1) * 128, :], x_sb[:, kc, :], reads=t_x[kc], pwrites=[t_out])
    barrier()
    sch.emit(nc, top)
    top.close()
    return nc


def _rope_tables():
    n_freq = 16
    inv = np.exp(-math.log(10000.0) * np.arange(n_freq, dtype=np.float32) * np.float32(2.0 / 32)).astype(np.float32)
    t = np.arange(S)
    row = (t // 64).astype(np.float32)
    colp = (t % 64).astype(np.float32)
    ar = row[:, None] * inv[None, :]
    ac = colp[:, None] * inv[None, :]
    C = np.zeros((128, S), np.float32)
    Sg = np.zeros((128, S), np.float32)
    for p in range(128):
        d = p % 64
        ang = ar if d < 32 else ac
        i = d % 16
        sign = -1.0 if (d % 32) < 16 else 1.0
        C[p] = np.cos(ang[:, i])
        Sg[p] = sign * np.sin(ang[:, i])
    return C, Sg


def _pool_fix(L, t_start, n):
    f = np.ones((4, 16), np.float32)
    for g, w in enumerate(POOL_WINDOWS):
        for k in range(16):
            t = t_start + k if k < 8 else t_start + n - 16 + k
            lo = min(max(t - w // 2, 0), L)
            hi = min(max(t - w // 2 + w, 0), L)
            f[g, k] = w / float(hi - lo)
    return f


def host_common(inp):
    cols = np.zeros((128, NCOLS), np.float32)

    def put(name, arr):
        o, w = COLS[name]
        assert arr.shape == (128, w), (name, arr.shape, w)
        cols[:, o:o + w] = arr
    put('cctx', _colvec(inp['c_ctx']))
    for l in range(DEPTH):
        put(f'bmod{l}', _colvec(inp['b_mod'][l]))
        put(f'gmix{l}', _colvec(inp['g_mix'][l]))
        put(f'gffn{l}', _colvec(inp['g_ffn'][l]))
        put(f'gq{l}', _colvec(inp['mla_gq'][l]))
        put(f'gkv{l}', _colvec(inp['mla_gkv'][l]))
        put(f'pscale{l}', _colvec(inp['pool_scale'][l]))
        put(f'subln{l}', _colvec(inp['da_subln'][l]))
        lam = np.zeros((128, 4), np.float32)
        lam[:64, :] = np.asarray(inp['da_lambda'][l], np.float32).T
        lam[64:, :] = 0.0
        put(f'lam{l}', lam)
    put('gfin', _colvec(inp['g_final']))
    put('pfixc', np.broadcast_to(_pool_fix(CTX, 0, CTX).reshape(1, 64), (128, 64)).copy())
    consts = np.zeros((128, 384), np.float32)
    consts[:, 0:128] = 1.0
    consts[:, 128:256] = np.eye(128, dtype=np.float32)
    for m in range(128):
        partner = (m & ~31) | ((m & 31) ^ 16)
        consts[partner, 256 + m] = 1.0
    constf = np.ascontiguousarray(consts[:, 0:256])
    consts = consts.astype(ml_dtypes.bfloat16)
    C, Sg = _rope_tables()
    uq_perm = np.concatenate([np.arange(h * 192, h * 192 + 128) for h in range(4)] +
                             [np.arange(h * 192 + 128, h * 192 + 192) for h in range(4)])
    ukv_perm = np.concatenate([np.arange(h * 256, h * 256 + 128) for h in range(4)] +
                              [np.arange(h * 256 + 128, h * 256 + 256) for h in range(4)])
    f32 = lambda a: np.ascontiguousarray(np.asarray(a, np.float32))
    shared = {
        "consts": consts, "constf": constf,
        "w_mod": f32(inp['w_mod']), "w_in": f32(inp['w_in']),
        "w_uq_p": f32(np.asarray(inp['w_uq'])[:, :, uq_perm]),
        "w_ukv_p": f32(np.asarray(inp['w_ukv'])[:, :, ukv_perm]),
        "pool_w": f32(inp['pool_w']), "w_branch": f32(inp['w_branch']), "w_out": f32(inp['w_out']),
    }
    percore = []
    for core in range(NCORE):
        b = core // 4
        t0 = (core % 4) * TOK
        cc = cols.copy()
        o, w = COLS['c']
        cc[:, o:o + w] = _colvec(inp['c'][b])
        o, w = COLS['pfix']
        cc[:, o:o + w] = np.broadcast_to(_pool_fix(S, t0, TOK).reshape(1, 64), (128, 64))
        s_ = core % 4
        o, w = COLS['selL']
        if s_ > 0:
            cc[:, o + s_ - 1] = 1.0
        o, w = COLS['selR']
        if s_ < 3:
            cc[:, o + s_ + 1] = 1.0
        percore.append({
            "cols": cc,
            "ropeC": np.ascontiguousarray(C[:, t0:t0 + TOK]).astype(ml_dtypes.bfloat16),
            "ropeS": np.ascontiguousarray(Sg[:, t0:t0 + TOK]).astype(ml_dtypes.bfloat16),
        })
    return shared, percore


def _gather_kv(res, name_l, name_c):
    per = []
    for core in range(NCORE):
        b, s = core // 4, core % 4
        sh = [np.asarray(res[b * 4 + r][name_l]) for r in range(4)]
        kvg = np.concatenate([sh[r][c * 128:(c + 1) * 128] for c in range(NCH) for r in range(4)] +
                             [sh[r][R_U:R_U + 8] for r in range(4)], axis=0)
        hl = np.zeros((128, 4, 16), ml_dtypes.bfloat16)
        if s > 0:
            nb_ = np.asarray(res[core - 1][name_l])[R_U:R_U + 4].reshape(4, 128, 16)
            hl[:, :, 0:8] = nb_[:, :, 8:16].transpose(1, 0, 2)
        if s < 3:
            nb_ = np.asarray(res[core + 1][name_l])[R_U:R_U + 4].reshape(4, 128, 16)
            hl[:, :, 8:16] = nb_[:, :, 0:8].transpose(1, 0, 2)
        per.append((np.ascontiguousarray(kvg), np.asarray(res[core][name_c]), hl))
    return per


def stage_maps(inputs, shared, percore, stage, prev=None):
    f32 = lambda a: np.ascontiguousarray(np.asarray(a, np.float32))
    x = np.asarray(inputs['x'], np.float32)
    ctx = np.asarray(inputs['ctx'], np.float32)
    extra = {}
    if stage in ('B', 'all'):
        extra.update(ffn_wg=f32(inputs['ffn_w_gate'][0]), ffn_wu=f32(inputs['ffn_w_up'][0]),
                     ffn_wd=f32(inputs['ffn_w_down'][0]))
    if stage in ('C', 'all'):
        extra.update(router=f32(inputs['moe_router'][0]), moe_wg=f32(inputs['moe_w_gate'][0]),
                     moe_wu=f32(inputs['moe_w_up'][0]), moe_wd=f32(inputs['moe_w_down'][0]))
    maps = []
    for core in range(NCORE):
        b = core // 4
        t0 = (core % 4) * TOK
        m = dict(shared)
        m.update(percore[core])
        m.update(extra)
        m["ctxT"] = np.ascontiguousarray(ctx[b].T)
        if stage == 'C':
            m["xT"] = np.ascontiguousarray(prev['x1T'][core])
        else:
            m["xT"] = np.ascontiguousarray(x[b, t0:t0 + TOK, :].T)
        if stage == 'B':
            m["kvg0"], m["kvc0"], m["halo0"] = prev['kv'][core]
            m["u_in"] = prev['u'][core]
            m["uc_in"] = prev['uc'][core]
        if stage == 'C':
            m["kvg1"], m["kvc1"], m["halo1"] = prev['kv'][core]
            m["u_in"] = prev['u'][core]
        maps.append(m)
    return maps


def kernel_unfused(**inputs):
    shared, percore = host_common(inputs)
    cores = list(range(NCORE))
    ra = run_bass_kernel_spmd(build('A'), stage_maps(inputs, shared, percore, 'A'), core_ids=cores).results
    kv0 = _gather_kv(ra, 'kvl0', 'kvc0')
    pa = dict(kv=kv0, u=[np.asarray(ra[c]['u_out']) for c in cores], uc=[np.asarray(ra[c]['uc_out']) for c in cores])
    rb = run_bass_kernel_spmd(build('B'), stage_maps(inputs, shared, percore, 'B', pa), core_ids=cores).results
    kv1 = _gather_kv(rb, 'kvl1', 'kvc1')
    pb = dict(kv=kv1, x1T=[np.asarray(rb[c]['x1T']) for c in cores], u=[np.asarray(rb[c]['u_out']) for c in cores])
    rc = run_bass_kernel_spmd(build('C'), stage_maps(inputs, shared, percore, 'C', pb), core_ids=cores).results
    out = np.zeros((2, S, D), np.float32)
    for core in cores:
        b = core // 4
        t0 = (core % 4) * TOK
        out[b, t0:t0 + TOK, :] = np.asarray(rc[core]["outT"]).T
    return out


def kernel(**inputs):
    shared, percore = host_common(inputs)
    cores = list(range(NCORE))
    res = run_bass_kernel_spmd(build('all'), stage_maps(inputs, shared, percore, 'all'), core_ids=cores).results
    out = np.zeros((2, S, D), np.float32)
    for core in cores:
        b = core // 4
        t0 = (core % 4) * TOK
        out[b, t0:t0 + TOK, :] = np.asarray(res[core]["outT"]).T
    return out
```

```python
import math
from contextlib import ExitStack
import numpy as np
import ml_dtypes
import concourse.bass as bass
import concourse.mybir as mybir
from concourse.bass_utils import run_bass_kernel_spmd

F32 = mybir.dt.float32
BF16 = mybir.dt.bfloat16
AF = mybir.ActivationFunctionType
ALU = mybir.AluOpType

D = 1024
KC = 8
S = 8192
NCORE = 8
TOK = 2048
CTX = 256
NKEY = S + CTX
DEPTH = 2
IN_W = 5824
D_FF = 2816
NEXP = 8
D_FFE = 3584
EPS = 1e-6

ENGS = ('pe', 'act', 'dve', 'pool', 'sp')


class Tk:
    __slots__ = ('name', 'w', 'r')

    def __init__(self, name):
        self.name = name
        self.w = {}
        self.r = {}


class Op:
    __slots__ = ('eng', 'fn', 'waits', 'idx', 'signal', 'dma', 'signo')

    def __init__(self, eng, fn, idx):
        self.eng = eng
        self.fn = fn
        self.waits = []
        self.idx = idx
        self.signal = False
        self.dma = None
        self.signo = None


class Sched:
    NDMA = 12
    EPOCH = 12000

    def __init__(self):
        self.ops = {e: [] for e in ENGS}
        self.seen = {e: {} for e in ENGS}
        self.dma_count = {e: 0 for e in ENGS}
        self.dma_slot_val = {}

    def _deps(self, reads, writes):
        deps = {}

        def add(d):
            for k, v in d.items():
                if deps.get(k, -1) < v:
                    deps[k] = v
        for t in reads:
            add(t.w)
        for t in writes:
            add(t.w)
            add(t.r)
        return deps

    def _apply_waits(self, op, deps, raw_keys):
        eng = op.eng
        seen = self.seen[eng]
        for k, v in deps.items():
            if k[0] == 'e' and k[1] == eng and k not in raw_keys:
                continue
            if seen.get(k, -1) >= v:
                continue
            seen[k] = v
            op.waits.append((k, v))
            if k[0] == 'e':
                self.ops[k[1]][v].signal = True

    def op(self, eng, fn, reads=(), writes=(), pwrites=()):
        o = Op(eng, fn, len(self.ops[eng]))
        deps = self._deps(reads, list(writes) + list(pwrites))
        raw = set()
        for t in reads:
            raw.update(t.w.keys())
        self._apply_waits(o, deps, raw)
        self.ops[eng].append(o)
        key = ('e', eng)
        for t in writes:
            t.w = {key: o.idx}
            t.r = {}
        for t in pwrites:
            t.w[key] = o.idx
        for t in reads:
            t.r[key] = o.idx
        return o

    def dma(self, q, out, in_, reads=(), writes=(), pwrites=()):
        n = self.dma_count[q]
        self.dma_count[q] += 1
        slot = n % self.NDMA
        key = ('d', q, slot)
        val = 16 * (n // self.NDMA + 1)
        o = Op(q, lambda e: e.dma_start(out=out, in_=in_), len(self.ops[q]))
        o.dma = (key, val)
        deps = self._deps(reads, list(writes) + list(pwrites))
        if val > 16:
            deps[key] = val - 16
        raw = set()
        for t in reads:
            raw.update(t.w.keys())
        raw.add(key)
        self._apply_waits(o, deps, raw)
        self.ops[q].append(o)
        for t in writes:
            t.w = {key: val}
            t.r = {}
        for t in pwrites:
            t.w[key] = val
        for t in reads:
            t.r[key] = val
        return o

    def coll(self, fn, reads=(), writes=(), pwrites=()):
        q = 'pool'
        n = self.ncoll = getattr(self, 'ncoll', 0) + 1
        key = ('c', n)
        o = Op(q, fn, len(self.ops[q]))
        o.dma = (key, 1)
        deps = self._deps(reads, list(writes) + list(pwrites))
        raw = set()
        for t in reads:
            raw.update(t.w.keys())
        self._apply_waits(o, deps, raw)
        self.ops[q].append(o)
        for t in writes:
            t.w = {key: 1}
            t.r = {}
        for t in pwrites:
            t.w[key] = 1
        for t in reads:
            t.r[key] = 1
        return o

    def emit(self, nc, stack):
        esems = {}
        for e in ENGS:
            k = 0
            for o in self.ops[e]:
                if o.dma is None and o.signal:
                    o.signo = k
                    k += 1
            nep = (k + self.EPOCH - 1) // self.EPOCH
            esems[e] = [stack.enter_context(nc.semaphore(f"s_{e}_{i}")) for i in range(nep)]
        dsems = {}
        for e in ENGS:
            for sl in range(min(self.NDMA, self.dma_count[e])):
                dsems[('d', e, sl)] = stack.enter_context(nc.semaphore(f"d_{e}_{sl}"))
        for i in range(1, getattr(self, 'ncoll', 0) + 1):
            dsems[('c', i)] = stack.enter_context(nc.semaphore(f"c_{i}"))
        ops = self.ops
        EP = self.EPOCH

        def run(ename, eng):
            for o in ops[ename]:
                for k, v in o.waits:
                    if k[0] in ('d', 'c'):
                        eng.wait_ge(dsems[k], v)
                    else:
                        sn = ops[k[1]][v].signo
                        eng.wait_ge(esems[k[1]][sn // EP], sn % EP + 1)
                ins = o.fn(eng)
                if o.dma is not None:
                    ins.then_inc(dsems[o.dma[0]], 1 if o.dma[0][0] == 'c' else 16)
                elif o.signal:
                    ins.then_inc(esems[ename][o.signo // EP], 1)

        with nc.Block() as block:
            @block.tensor
            def _(e):
                run('pe', e)

            @block.scalar
            def _(e):
                run('act', e)

            @block.vector
            def _(e):
                run('dve', e)

            @block.gpsimd
            def _(e):
                run('pool', e)

            @block.sync
            def _(e):
                run('sp', e)


R_KDA, R_KN, R_KR, R_VDA, R_VM, R_U, ROWS = 0, 512, 1024, 1152, 1664, 2176, 2184
NCH = 17
GROWS = NCH * 512 + 32
C_DAQ, C_DAK, C_DAV, C_QD, C_KVD, C_KR, C_POOL, C_GATE = 0, 512, 1024, 1536, 1920, 2176, 2240, 2752
LAM_INIT = [0.8 - 0.6 * math.exp(-0.3 * l) for l in range(DEPTH)]
POOL_WINDOWS = (2, 4, 8, 16)
BIG = 1.0e4


def _cols_layout():
    off = {}
    n = 0

    def add(name, w):
        nonlocal n
        off[name] = (n, w)
        n += w
    add('c', 8)
    add('cctx', 8)
    for l in range(DEPTH):
        add(f'bmod{l}', 48)
        add(f'gmix{l}', 8)
        add(f'gffn{l}', 8)
        add(f'gq{l}', 3)
        add(f'gkv{l}', 2)
        add(f'pscale{l}', 4)
        add(f'subln{l}', 1)
        add(f'lam{l}', 4)
    add('gfin', 8)
    add('pfix', 64)
    add('pfixc', 64)
    add('selL', 4)
    add('selR', 4)
    return off, n


COLS, NCOLS = _cols_layout()


def _colvec(v):
    v = np.asarray(v, np.float32)
    return np.ascontiguousarray(v.reshape(-1, 128).T)


class Ring:
    def __init__(self, mk, name, n, shape, dt):
        self.bufs = [(mk(f"{name}{i}", shape, dt), Tk(f"{name}{i}")) for i in range(n)]
        self.i = 0

    def next(self):
        b = self.bufs[self.i % len(self.bufs)]
        self.i += 1
        return b


def build(stage='all', debug=()):
    nc = bass.Bass("TRN2", target_bir_lowering=False)
    sch = Sched()
    top = ExitStack()

    def dram_in(name, shape, dt=F32):
        return nc.dram_tensor(name, list(shape), dt, kind="ExternalInput").ap()

    def dram_out(name, shape, dt=F32):
        return nc.dram_tensor(name, list(shape), dt, kind="ExternalOutput").ap()

    uniq = [0]

    def mk_sb(stack):
        def f(name, shape, dt):
            uniq[0] += 1
            return stack.enter_context(nc.sbuf_tensor(f"sb{uniq[0]}_{name}", list(shape), dt))
        return f
    sb = mk_sb(top)

    def MM(out, lhsT, rhs, start, stop, reads, writes):
        return sch.op('pe', lambda e: e.matmul(out, lhsT=lhsT, rhs=rhs, start=start, stop=stop),
                      reads=reads, writes=writes)

    def ACT(out, in_, func, reads, writes, scale=None, bias=None, pw=()):
        kw = {}
        if scale is not None:
            kw['scale'] = scale
        if bias is not None:
            kw['bias'] = bias
        return sch.op('act', lambda e: e.activation(out=out, in_=in_, func=func, **kw),
                      reads=reads, writes=writes, pwrites=pw)

    def TT(out, in0, in1, op, reads, writes, eng='dve', pw=()):
        return sch.op(eng, lambda e: e.tensor_tensor(out=out, in0=in0, in1=in1, op=op),
                      reads=reads, writes=writes, pwrites=pw)

    def TS(out, in0, s1, s2, op0, op1, reads, writes, eng='dve', pw=()):
        if op1 is None:
            return sch.op(eng, lambda e: e.tensor_scalar(out=out, in0=in0, scalar1=s1, scalar2=None, op0=op0),
                          reads=reads, writes=writes, pwrites=pw)
        return sch.op(eng, lambda e: e.tensor_scalar(out=out, in0=in0, scalar1=s1, scalar2=s2, op0=op0, op1=op1),
                      reads=reads, writes=writes, pwrites=pw)

    def STT(out, in0, scalar, in1, op0, op1, reads, writes, pw=()):
        return sch.op('dve', lambda e: e.scalar_tensor_tensor(out=out, in0=in0, scalar=scalar, in1=in1,
                                                              op0=op0, op1=op1),
                      reads=reads, writes=writes, pwrites=pw)

    def CP(out, in_, reads, writes, eng='dve', pw=()):
        if eng == 'act':
            return sch.op('act', lambda e: e.copy(out=out, in_=in_), reads=reads, writes=writes, pwrites=pw)
        return sch.op(eng, lambda e: e.tensor_copy(out=out, in_=in_), reads=reads, writes=writes, pwrites=pw)

    def RSTD(out, ps_in, inv_n, reads, t_out):
        ACT(out, ps_in, AF.Sqrt, reads, [t_out], scale=inv_n, bias=EPS)
        sch.op('dve', lambda e: e.reciprocal(out=out, in_=out), reads=[t_out], writes=[t_out])

    def RECIP(out, in_, reads, writes):
        return sch.op('dve', lambda e: e.reciprocal(out=out, in_=in_), reads=reads, writes=writes)

    def barrier():
        last = {e: len(sch.ops[e]) - 1 for e in ENGS}
        dl = {}
        for q in ENGS:
            n = sch.dma_count[q]
            for sl in range(min(n, sch.NDMA)):
                uses = (n - 1 - sl) // sch.NDMA + 1
                dl[('d', q, sl)] = 16 * uses
        for e in ENGS:
            o = Op(e, lambda en: en.nop(), len(sch.ops[e]))
            deps = dict(dl)
            for e2 in ENGS:
                if e2 != e and last[e2] >= 0:
                    k = last[e2]
                    while k >= 0 and sch.ops[e2][k].dma is not None:
                        k -= 1
                    if k >= 0:
                        deps[('e', e2)] = k
            sch._apply_waits(o, deps, set(deps.keys()))
            sch.ops[e].append(o)

    A_ = stage == 'A'
    B_ = stage == 'B'
    C_ = stage == 'C'
    ALL = stage == 'all'

    xT = dram_in("xT", [D, TOK])
    ctxT = dram_in("ctxT", [D, CTX])
    cols_d = dram_in("cols", [128, NCOLS])
    consts_d = dram_in("consts", [128, 384], BF16)
    constf_d = dram_in("constf", [128, 256], F32)
    ropeC_d = dram_in("ropeC", [128, TOK], BF16)
    ropeS_d = dram_in("ropeS", [128, TOK], BF16)
    w_mod = dram_in("w_mod", [DEPTH, D, 6 * D])
    w_in = dram_in("w_in", [DEPTH, D, IN_W])
    w_uq = dram_in("w_uq_p", [DEPTH, 384, 768])
    w_ukv = dram_in("w_ukv_p", [DEPTH, 256, 1024])
    pool_w = dram_in("pool_w", [DEPTH, 4, 128, 128])
    w_branch = dram_in("w_branch", [DEPTH, 3, 512, D])
    w_out = dram_in("w_out", [DEPTH, D, D])
    if B_ or ALL:
        ffn_wg = dram_in("ffn_wg", [D, D_FF])
        ffn_wu = dram_in("ffn_wu", [D, D_FF])
        ffn_wd = dram_in("ffn_wd", [D_FF, D])
    if C_ or ALL:
        router_d = dram_in("router", [D, NEXP])
        moe_wg = dram_in("moe_wg", [NEXP, D, D_FFE])
        moe_wu = dram_in("moe_wu", [NEXP, D, D_FFE])
        moe_wd = dram_in("moe_wd", [NEXP, D_FFE, D])

    kvl, kvc, kvg, halo = {}, {}, {}, {}
    t_kvl, t_kvc, t_kvg = {}, {}, {}
    if A_:
        kvl[0] = dram_out("kvl0", [ROWS, TOK], BF16)
        kvc[0] = dram_out("kvc0", [ROWS, CTX], BF16)
    if B_:
        kvg[0] = dram_in("kvg0", [GROWS, TOK], BF16)
        kvc[0] = dram_in("kvc0", [ROWS, CTX], BF16)
        halo[0] = dram_in("halo0", [128, 4, 16], BF16)
        kvl[1] = dram_out("kvl1", [ROWS, TOK], BF16)
        kvc[1] = dram_out("kvc1", [ROWS, CTX], BF16)
    if ALL:
        for l_ in range(DEPTH):
            kvl[l_] = nc.dram_tensor(f"kvl{l_}", [ROWS, TOK], BF16, kind="Internal").ap()
            kvc[l_] = nc.dram_tensor(f"kvc{l_}", [ROWS, CTX], BF16, kind="Internal").ap()
            kvg[l_] = nc.dram_tensor(f"kvg{l_}", [GROWS, TOK], BF16, kind="Internal").ap()
    if C_:
        kvg[1] = dram_in("kvg1", [GROWS, TOK], BF16)
        kvc[1] = dram_in("kvc1", [ROWS, CTX], BF16)
        halo[1] = dram_in("halo1", [128, 4, 16], BF16)
    u_out = u_in = uc_out = uc_in = None
    if A_ or B_:
        u_out = dram_out("u_out", [128, 4 * (TOK + 16)], BF16)
    if A_:
        uc_out = dram_out("uc_out", [128, 4 * (CTX + 16)], BF16)
    if B_ or C_:
        u_in = dram_in("u_in", [128, 4 * (TOK + 16)], BF16)
    if B_:
        uc_in = dram_in("uc_in", [128, 4 * (CTX + 16)], BF16)
    for l in range(DEPTH):
        t_kvl[l] = Tk(f'kvl{l}')
        t_kvc[l] = Tk(f'kvc{l}')
        t_kvg[l] = [Tk(f'kvg{l}_{c}') for c in range(NCH + 1)]

    psb = [top.enter_context(nc.psum_tensor(f"psb{i}", [128, 512], F32)) for i in range(8)]
    t_ps = [Tk(f'ps{i}') for i in range(8)]

    x_sb = sb("x_sb", [128, KC, TOK], F32)
    xc_sb = sb("xc_sb", [128, KC, CTX], F32)
    t_x = [[Tk(f'x{kc}_{b}') for b in range(4)] for kc in range(KC)]
    t_xc = [Tk(f'xc{kc}') for kc in range(KC)]
    cols = sb("cols_sb", [128, NCOLS], F32)
    t_cols = Tk('cols')
    consts = sb("consts_sb", [128, 384], BF16)
    constf = sb("constf_sb", [128, 256], F32)
    t_consts = Tk('consts')
    ones_bf = consts[:, 0:128]
    ident_bf = consts[:, 128:256]
    perm_bf = consts[:, 256:384]
    ones_f = constf[:, 0:128]
    ident_f = constf[:, 128:256]
    mod_sb = sb("mod_sb", [128, DEPTH, 48, 2], F32)
    modA = sb("modA_sb", [128, DEPTH, 2, 8, 2], F32)
    t_mod = Tk('mod')
    misc = sb("misc_sb", [128, 16], F32)
    t_misc = Tk('misc')
    u_sb = sb("u_sb", [128, 4, TOK + 16], BF16)
    uc_sb = sb("uc_sb", [128, 4, CTX + 16], BF16)
    t_u = Tk('u')
    t_uc = Tk('uc')

    def colap(name, j=0, w=1):
        o, _ = COLS[name]
        return cols[:, o + j:o + j + w]

    sch.dma('sp', cols[:], cols_d[:], writes=[t_cols])
    sch.dma('sp', consts[:], consts_d[:], writes=[t_consts])
    sch.dma('sp', constf[:], constf_d[:], pwrites=[t_consts])
    for kc in range(KC):
        sch.dma('sp', x_sb[:, kc, :], xT[kc * 128:(kc + 1) * 128, :], writes=t_x[kc])
    if not C_:
        for kc in range(KC):
            sch.dma('sp', xc_sb[:, kc, :], ctxT[kc * 128:(kc + 1) * 128, :], writes=[t_xc[kc]])
    if u_in is not None:
        sch.dma('sp', u_sb[:].rearrange("p g t -> p (g t)"), u_in[:], writes=[t_u])
    if uc_in is not None:
        sch.dma('sp', uc_sb[:].rearrange("p g t -> p (g t)"), uc_in[:], writes=[t_uc])

    def mods_stage(pairs, with_misc):
        with ExitStack() as sc:
            lsb = mk_sb(sc)
            silu_c = lsb("silu_c", [128, KC, 2], BF16)
            t_silu = Tk('silu')
            o_c, _ = COLS['c']
            o_cc, _ = COLS['cctx']
            ACT(silu_c[:, :, 0], cols[:, o_c:o_c + 8], AF.Silu, [t_cols], [t_silu])
            ACT(silu_c[:, :, 1], cols[:, o_cc:o_cc + 8], AF.Silu, [t_cols], [], pw=[t_silu])
            wm = [lsb(f"wm{i}", [128, KC, 1024], BF16) for i in range(2)]
            t_wm = [Tk('wm0'), Tk('wm1')]
            t_psmod = t_ps[7]
            ps_mod = psb[7][:, 0:192]
            psv = ps_mod.rearrange("p (l m j) -> p l m j", l=DEPTH, j=2)
            for it, (l, part) in enumerate(pairs):
                wv = w_mod[l].rearrange("(kc p) n -> p kc n", p=128)
                buf = it % 2
                sch.dma('pool', wm[buf][:], wv[:, :, part * 1024:(part + 1) * 1024], writes=[t_wm[buf]])
                for mc in range(8):
                    c0 = (l * 48 + part * 8 + mc) * 2
                    for kc in range(KC):
                        MM(ps_mod[:, c0:c0 + 2], wm[buf][:, kc, mc * 128:(mc + 1) * 128], silu_c[:, kc, :],
                           kc == 0, kc == KC - 1, [t_wm[buf], t_silu], [t_psmod])
            for (l, part) in pairs:
                ob, _ = COLS[f'bmod{l}']
                for j in range(2):
                    TT(mod_sb[:, l, part * 8:(part + 1) * 8, j], psv[:, l, part * 8:(part + 1) * 8, j],
                       cols[:, ob + part * 8:ob + (part + 1) * 8], ALU.add, [t_psmod, t_cols], [], pw=[t_mod])
            for (l, part) in pairs:
                if part not in (1, 4):
                    continue
                which = 0 if part == 1 else 1
                gname = f'gmix{l}' if which == 0 else f'gffn{l}'
                og, _ = COLS[gname]
                for j in range(2):
                    STT(modA[:, l, which, :, j], mod_sb[:, l, part * 8:(part + 1) * 8, j], 1.0,
                        cols[:, og:og + 8], ALU.add, ALU.mult, [t_mod, t_cols], [], pw=[t_mod])
            if with_misc:
                lamt = lsb("lamt", [128, 4], F32)
                t_lamt = Tk('lamt')
                for l in range(DEPTH):
                    ol, _ = COLS[f'lam{l}']
                    lv = cols[:, ol:ol + 4].rearrange("p (a b) -> p a b", b=2)
                    TT(lamt[:, 0:2], lv[:, :, 0], lv[:, :, 1], ALU.mult, [t_cols], [t_lamt])
                    MM(psb[6][:, 0:2], ones_f, lamt[:, 0:2], True, True, [t_lamt, t_consts], [t_ps[6]])
                    ACT(lamt[:, 2:4], psb[6][:, 0:2], AF.Exp, [t_ps[6]], [], pw=[t_lamt])
                    STT(misc[:, 4 * l:4 * l + 1], lamt[:, 3:4], -LAM_INIT[l], lamt[:, 2:3], ALU.add, ALU.subtract,
                        [t_lamt], [], pw=[t_misc])
                    osub, _ = COLS[f'subln{l}']
                    TS(misc[:, 4 * l + 1:4 * l + 2], cols[:, osub:osub + 1], 1.0 - LAM_INIT[l], None, ALU.mult, None,
                       [t_cols], [], pw=[t_misc])
        barrier()

    MODS_FIRST = [(0, 0), (0, 1), (0, 2)]
    MODS_REST = [(0, 3), (0, 4), (0, 5)] + [(1, p_) for p_ in range(6)]
    mods_stage(MODS_FIRST, True)
    if not ALL:
        mods_stage(MODS_REST, False)

    def modcol(l, part, kc, j):
        return mod_sb[:, l, part * 8 + kc, j:j + 1]

    def norm_mod(R, xsrc, t_xs, ntok, l, which, j, h_out, t_h, f32cb=None, bank=7):
        for kc in range(KC):
            sq, t_sq = R['bf'].next()
            ACT(sq[:, :ntok], xsrc(kc), AF.Square, t_xs[kc], [t_sq])
            MM(psb[bank][:, :ntok], ones_bf, sq[:, :ntok], kc == 0, kc == KC - 1, [t_sq, t_consts], [t_ps[bank]])
        rstd, t_rstd = R['rstd'].next()
        RSTD(rstd[:, :ntok], psb[bank][:, :ntok], 1.0 / D, [t_ps[bank]], t_rstd)
        shpart = 0 if which == 0 else 3
        for kc in range(KC):
            tmp, t_tmp = R['f32'].next()
            TT(tmp[:, :ntok], xsrc(kc), rstd[:, :ntok], ALU.mult, list(t_xs[kc]) + [t_rstd], [t_tmp])
            if h_out is not None:
                ACT(h_out[:, kc, :ntok], tmp[:, :ntok], AF.Identity, [t_tmp, t_mod], [] if kc else [t_h],
                    scale=modA[:, l, which, kc, j:j + 1], bias=modcol(l, shpart, kc, j), pw=[t_h] if kc else [])
            if f32cb is not None:
                f32cb(kc, tmp, t_tmp)

    def rope(R, z_bf, t_z, ntok, tok0, out_ap, t_out, ropeC, ropeS, t_rope, bank, pw=False):
        MM(psb[bank][:, :ntok], perm_bf, z_bf, True, True, [t_z, t_consts], [t_ps[bank]])
        t1, t_t1 = R['f32'].next()
        TT(t1[:, :ntok], z_bf, ropeC[:, tok0:tok0 + ntok], ALU.mult, [t_z, t_rope], [t_t1])
        t2, t_t2 = R['f32'].next()
        TT(t2[:, :ntok], psb[bank][:, :ntok], ropeS[:, tok0:tok0 + ntok], ALU.mult, [t_ps[bank], t_rope], [t_t2])
        if isinstance(out_ap, tuple):
            TT(out_ap[0], t1[0:64, :ntok], t2[0:64, :ntok], ALU.add, [t_t1, t_t2], [], pw=[t_out])
            TT(out_ap[1], t1[64:128, :ntok], t2[64:128, :ntok], ALU.add, [t_t1, t_t2], [], pw=[t_out])
            return
        TT(out_ap, t1[:, :ntok], t2[:, :ntok], ALU.add, [t_t1, t_t2], [] if pw else [t_out],
           pw=[t_out] if pw else [])

    def wload(dst, src, tk, first=True):
        sch.dma('pool', dst, src, writes=[tk] if first else [], pwrites=[] if first else [tk])

    def winv(l):
        return w_in[l].rearrange("(kc p) n -> p kc n", p=128)

    def phase1(l, with_ctx_u):
        with ExitStack() as sc:
            lsb = mk_sb(sc)
            R = {'bf': Ring(lsb, "p1bf", 4, [128, 512], BF16), 'f32': Ring(lsb, "p1f", 4, [128, 512], F32),
                 'rstd': Ring(lsb, "p1r", 2, [128, 512], F32)}
            wp = lsb("wP1", [128, KC, 1920], BF16)
            t_wp = Tk('wP1')
            wkv = lsb("wukv", [128, 2, 1024], BF16)
            t_wkv = Tk('wukv')
            ropeC = lsb("ropeC", [128, TOK], BF16)
            ropeS = lsb("ropeS", [128, TOK], BF16)
            t_rope = Tk('rope')
            sch.dma('sp', ropeC[:], ropeC_d[:], writes=[t_rope])
            sch.dma('sp', ropeS[:], ropeS_d[:], pwrites=[t_rope])
            wv = winv(l)
            first = True
            for (d0, s0, n) in ((0, C_DAK, 512), (512, C_DAV, 512), (1024, C_KVD, 256), (1280, C_KR, 64),
                                (1344, C_KR, 64), (1408, C_POOL, 512)):
                wload(wp[:, :, d0:d0 + n], wv[:, :, s0:s0 + n], t_wp, first)
                first = False
            wload(wkv[:], w_ukv[l].rearrange("(kc p) n -> p kc n", p=128), t_wkv)
            hring = [(lsb(f"p1h{i}", [128, KC, 512], BF16), Tk(f'p1h{i}')) for i in range(2)]
            kst = lsb("kst", [128, 9, 512], BF16)
            t_kst = Tk('kst')
            vst = lsb("vst", [128, 4, 1024], BF16)
            t_vst = Tk('vst')
            kvn = lsb("kvn", [128, 2, 512], BF16)
            t_kvn = Tk('kvn')
            kvf = lsb("kvf", [128, 2, 512], F32)
            t_kvf = Tk('kvf')
            hst = lsb("hst", [128, 4, 16], BF16)
            t_hst = Tk('hst')
            og, _ = COLS[f'gkv{l}']
            bankrr = [0]

            def nb():
                b = bankrr[0] % 6
                bankrr[0] += 1
                return b

            blocks = [('l', b) for b in range(4)] + [('c', 0)]
            for bi, (kind, b) in enumerate(blocks):
                isc = kind == 'c'
                ntok = CTX if isc else 512
                tok0 = 0 if isc else b * 512
                j = 1 if isc else 0
                if isc:
                    xsrc = lambda kc: xc_sb[:, kc, :]
                    t_xs = [[t_xc[kc]] for kc in range(KC)]
                else:
                    xsrc = lambda kc, tok0=tok0: x_sb[:, kc, tok0:tok0 + 512]
                    t_xs = [[t_x[kc][b]] for kc in range(KC)]
                h, t_h = hring[bi % 2]
                norm_mod(R, xsrc, t_xs, ntok, l, 0, j, h, t_h, bank=7)
                for ci in range(5):
                    c0 = ci * 128 if ci < 4 else 1280
                    bk = nb()
                    for kc in range(KC):
                        MM(psb[bk][:, :ntok], wp[:, kc, c0:c0 + 128], h[:, kc, :ntok], kc == 0, kc == KC - 1,
                           [t_wp, t_h], [t_ps[bk]])
                    dst = kst[:, ci if ci < 4 else 8, :ntok]
                    if isc:
                        CP(dst, psb[bk][:, :ntok], [t_ps[bk]], [], pw=[t_kst])
                    else:
                        z, t_z = R['bf'].next()
                        CP(z[:, :ntok], psb[bk][:, :ntok], [t_ps[bk]], [t_z], eng='act')
                        rope(R, z[:, :ntok], t_z, ntok, tok0, dst, t_kst, ropeC, ropeS, t_rope, nb(), pw=True)
                bks = []
                for ci in range(2):
                    bk = nb()
                    bks.append(bk)
                    for kc in range(KC):
                        MM(psb[bk][:, :ntok], wp[:, kc, 1024 + ci * 128:1024 + (ci + 1) * 128], h[:, kc, :ntok],
                           kc == 0, kc == KC - 1, [t_wp, t_h], [t_ps[bk]])
                    CP(kvf[:, ci, :ntok], psb[bk][:, :ntok], [t_ps[bk]], [], pw=[t_kvf])
                bk = nb()
                for ci in range(2):
                    sq, t_sq = R['bf'].next()
                    ACT(sq[:, :ntok], kvf[:, ci, :ntok], AF.Square, [t_kvf], [t_sq])
                    MM(psb[bk][:, :ntok], ones_bf, sq[:, :ntok], ci == 0, ci == 1, [t_sq, t_consts], [t_ps[bk]])
                rstd, t_rstd = R['rstd'].next()
                RSTD(rstd[:, :ntok], psb[bk][:, :ntok], 1.0 / 256, [t_ps[bk]], t_rstd)
                for ci in range(2):
                    STT(kvn[:, ci, :ntok], kvf[:, ci, :ntok], cols[:, og + ci:og + ci + 1], rstd[:, :ntok],
                        ALU.mult, ALU.mult, [t_kvf, t_rstd, t_cols], [], pw=[t_kvn])
                for hh in range(4):
                    bk = nb()
                    for kc in range(2):
                        MM(psb[bk][:, :ntok], wkv[:, kc, hh * 128:(hh + 1) * 128], kvn[:, kc, :ntok],
                           kc == 0, kc == 1, [t_wkv, t_kvn], [t_ps[bk]])
                    CP(kst[:, 4 + hh, :ntok], psb[bk][:, :ntok], [t_ps[bk]], [], pw=[t_kst])
                for ti in range(ntok // 128):
                    bk = nb()
                    for kc in range(KC):
                        MM(psb[bk][:, :], h[:, kc, ti * 128:(ti + 1) * 128], wp[:, kc, 512:1024],
                           kc == 0, kc == KC - 1, [t_wp, t_h], [t_ps[bk]])
                    CP(vst[:, ti, 0:512], psb[bk][:, :], [t_ps[bk]], [], eng='act', pw=[t_vst])
                    bk = nb()
                    for kc in range(2):
                        MM(psb[bk][:, :], kvn[:, kc, ti * 128:(ti + 1) * 128], wkv[:, kc, 512:1024],
                           kc == 0, kc == 1, [t_wkv, t_kvn], [t_ps[bk]])
                    CP(vst[:, ti, 512:1024], psb[bk][:, :], [t_ps[bk]], [], pw=[t_vst])
                if (not isc) or with_ctx_u:
                    ud = uc_sb if isc else u_sb
                    tu = t_uc if isc else t_u
                    for gi in range(4):
                        bk = nb()
                        for kc in range(KC):
                            MM(psb[bk][:, :ntok], wp[:, kc, 1408 + gi * 128:1408 + (gi + 1) * 128], h[:, kc, :ntok],
                               kc == 0, kc == KC - 1, [t_wp, t_h], [t_ps[bk]])
                        CP(ud[:, gi, 8 + tok0:8 + tok0 + ntok], psb[bk][:, :ntok], [t_ps[bk]], [], eng='act', pw=[tu])
                dk = kvc[l] if isc else kvl[l]
                tdk = t_kvc[l] if isc else t_kvl[l]
                sch.dma('sp', dk[R_KDA:R_KDA + 512, tok0:tok0 + ntok].rearrange("(c p) t -> p c t", p=128),
                        kst[:, 0:4, :ntok], reads=[t_kst], pwrites=[tdk])
                sch.dma('sp', dk[R_KN:R_KN + 512, tok0:tok0 + ntok].rearrange("(c p) t -> p c t", p=128),
                        kst[:, 4:8, :ntok], reads=[t_kst], pwrites=[tdk])
                sch.dma('sp', dk[R_KR:R_KR + 128, tok0:tok0 + ntok], kst[:, 8, :ntok], reads=[t_kst], pwrites=[tdk])
                for (r0, c0) in ((R_VDA, 0), (R_VM, 512)):
                    if isc:
                        vview = dk[r0:r0 + 512, :].rearrange("(t a) c -> t (a c)", a=2)
                        sch.dma('sp', vview[tok0:tok0 + ntok, :].rearrange("(i p) f -> p i f", p=128),
                                vst[:, 0:ntok // 128, c0:c0 + 512], reads=[t_vst], pwrites=[tdk])
                    else:
                        grp, i0 = b // 2, (b % 2) * 4
                        for hh in range(4):
                            rb = r0 + (hh * 2 + grp) * 64
                            blk = dk[rb:rb + 64, :].rearrange("r (a q) -> (r a) q", a=2).rearrange(
                                "p (i f) -> p i f", f=128)
                            sch.dma('sp', blk[:, i0:i0 + 4, :], vst[:, 0:4, c0 + hh * 128:c0 + (hh + 1) * 128],
                                    reads=[t_vst], pwrites=[tdk])
            if 'h' in debug:
                dh = dram_out("dbg_h", [128, KC * 512], BF16)
                sch.dma('sp', dh[:], hring[1][0][:].rearrange("p k t -> p (k t)"), reads=[hring[1][1]])
            if u_out is not None:
                sch.dma('sp', u_out[:], u_sb[:].rearrange("p g t -> p (g t)"), reads=[t_u])
            if uc_out is not None and with_ctx_u:
                sch.dma('sp', uc_out[:], uc_sb[:].rearrange("p g t -> p (g t)"), reads=[t_uc])
            if l in kvl:
                CP(hst[:, :, 0:8], u_sb[:, :, 8:16], [t_u], [t_hst], eng='pool')
                CP(hst[:, :, 8:16], u_sb[:, :, TOK:TOK + 8], [t_u], [], eng='pool', pw=[t_hst])
                sch.dma('sp', kvl[l][R_U:R_U + 4, :].rearrange("g (p t) -> p g t", t=16), hst[:],
                        reads=[t_hst], pwrites=[t_kvl[l]])
        barrier()

    def phase2(l, do_ctx):
        with ExitStack() as sc:
            lsb = mk_sb(sc)
            R = {"bf": Ring(lsb, "p2bf", 3, [128, 512], BF16), "f32": Ring(lsb, "p2f", 3, [128, 512], F32),
                 'rstd': Ring(lsb, "p2r", 1, [128, 512], F32)}
            wsl = Ring(lsb, "wsl", 3, [128, 2048], BF16)
            h = lsb("p2h", [128, KC, 512], BF16)
            t_h = Tk('p2h')
            ropeC = lsb("ropeCq", [128, 512], BF16)
            ropeS = lsb("ropeSq", [128, 512], BF16)
            t_rope = Tk('ropeq')
            qdaA = lsb("qdaA", [128, 4, 512], BF16)
            qdaB = lsb("qdaB", [128, 4, 512], BF16)
            qmr = lsb("qmr", [128, 4, 512], BF16)
            t_qda = Tk('qda')
            sch.op('dve', lambda e: e.memset(qdaA[64:128, :, :], 0.0), pwrites=[t_qda])
            sch.op('dve', lambda e: e.memset(qdaB[0:64, :, :], 0.0), pwrites=[t_qda])
            qm = lsb("qm", [128, 4, 512], BF16)
            t_qm = Tk('qm')
            for hh_ in range(4):
                if hh_ % 2 == 0:
                    sch.op('dve', lambda e, hh_=hh_: e.memset(qmr[64:128, hh_, :], 0.0), pwrites=[t_qm])
                else:
                    sch.op('dve', lambda e, hh_=hh_: e.memset(qmr[0:64, hh_, :], 0.0), pwrites=[t_qm])
            qn = lsb("qn", [128, 3, 512], BF16)
            t_qn = Tk('qn')
            o_da = lsb("o_da", [128, 4, 512], BF16)
            o_mla = lsb("o_mla", [128, 4, 512], BF16)
            o_pool = lsb("o_pool", [128, 4, 512], BF16)
            t_oda, t_omla, t_opool = Tk('oda'), Tk('omla'), Tk('opool')
            da_a = lsb("da_a", [128, 4, 512], BF16)
            t_daa = Tk('daa')
            merged = lsb("merged", [128, KC, 512], BF16)
            t_merged = Tk('merged')
            pring = Ring(lsb, "pT", 4, [128, 512], BF16)
            fA, fB, fC = (lsb(n_, [128, 512], F32) for n_ in ("fA", "fB", "fC"))
            t_fA, t_fB, t_fC = Tk('fA'), Tk('fB'), Tk('fC')
            slots = []
            for i in range(3):
                slots.append(dict(K1=lsb(f"kK1_{i}", [128, 1024], BF16), tK1=Tk(f'kK1_{i}'),
                                  K2=lsb(f"kK2_{i}", [128, 1024], BF16), tK2=Tk(f'kK2_{i}'),
                                  V=lsb(f"kV_{i}", [128, 8, 128], BF16), tV=Tk(f'kV_{i}')))
            pa = lsb("pa", [128, 528], F32)
            pb_ = lsb("pb", [128, 528], F32)
            t_pa, t_pb = Tk('pa'), Tk('pb')
            dbf = lsb("dbf", [128, 512], BF16)
            t_dbf = Tk('dbf')
            wv = winv(l)
            ogq, _ = COLS[f'gq{l}']
            ops_, _ = COLS[f'pscale{l}']
            neglam = misc[:, 4 * l:4 * l + 1]
            sublnS = misc[:, 4 * l + 1:4 * l + 2]

            if ALL:
                hall = lsb("hall", [128, 4, 4, 16], BF16)
                t_hall = Tk('hall')
                hb = NCH * 512
                for r in range(4):
                    sch.dma('sp', hall[:, r, :, :], kvg[l][hb + r * 8:hb + r * 8 + 4, :].rearrange("g (p t) -> p g t", t=16)[:, :, 0:16],
                            reads=[t_kvg[l][NCH]], writes=[t_hall] if r == 0 else [], pwrites=[] if r == 0 else [t_hall])
                oL, _ = COLS['selL']
                oR, _ = COLS['selR']
                for r in range(4):
                    if r == 0:
                        TS(u_sb[:, :, 0:8], hall[:, r, :, 8:16], cols[:, oL + r:oL + r + 1], None, ALU.mult, None,
                           [t_hall, t_cols], [], pw=[t_u])
                        TS(u_sb[:, :, TOK + 8:TOK + 16], hall[:, r, :, 0:8], cols[:, oR + r:oR + r + 1], None, ALU.mult, None,
                           [t_hall, t_cols], [], pw=[t_u])
                    else:
                        STT(u_sb[:, :, 0:8], hall[:, r, :, 8:16], cols[:, oL + r:oL + r + 1], u_sb[:, :, 0:8],
                            ALU.mult, ALU.add, [t_hall, t_cols, t_u], [], pw=[t_u])
                        STT(u_sb[:, :, TOK + 8:TOK + 16], hall[:, r, :, 0:8], cols[:, oR + r:oR + r + 1],
                            u_sb[:, :, TOK + 8:TOK + 16], ALU.mult, ALU.add, [t_hall, t_cols, t_u], [], pw=[t_u])
            if l in halo:
                sch.dma('sp', u_sb[:, :, 0:8], halo[l][:, :, 0:8], pwrites=[t_u])
                sch.dma('sp', u_sb[:, :, TOK + 8:TOK + 16], halo[l][:, :, 8:16], pwrites=[t_u])
            if do_ctx:
                sch.op('dve', lambda e: e.memset(uc_sb[:, :, 0:8], 0.0), pwrites=[t_uc])
                sch.op('dve', lambda e: e.memset(uc_sb[:, :, CTX + 8:CTX + 16], 0.0), pwrites=[t_uc])

            def load_group(kind, hh, g, slot):
                if kind == 'da':
                    rk, rv = R_KDA, R_VDA
                else:
                    rk, rv = R_KN, R_VM
                if g[0] == 'c':
                    src, tsrc = kvc[l], [t_kvc[l]]
                    vv = src[rv:rv + 512, :].rearrange("(t a) c -> t (a c)", a=2)
                    sch.dma('sp', slot['K1'][:, :CTX], src[rk + hh * 128:rk + (hh + 1) * 128, :],
                            reads=tsrc, writes=[slot['tK1']])
                    if kind == 'mla':
                        sch.dma('sp', slot['K2'][:, :CTX], src[R_KR:R_KR + 128, :], reads=tsrc, writes=[slot['tK2']])
                    sch.dma('sp', slot['V'][:, :CTX // 128, :],
                            vv[:, hh * 128:(hh + 1) * 128].rearrange("(i p) f -> p i f", p=128),
                            reads=tsrc, writes=[slot['tV']])
                    return
                _, r, hf = g
                src = kvg[l]
                col0 = hf * 1024

                def rows(row0):
                    c = row0 // 128
                    return src[c * 512 + r * 128:c * 512 + (r + 1) * 128, :], t_kvg[l][c]
                ap, tk = rows(rk + hh * 128)
                sch.dma('sp', slot['K1'][:, :1024], ap[:, col0:col0 + 1024], reads=[tk], writes=[slot['tK1']])
                if kind == 'mla':
                    ap, tk = rows(R_KR)
                    sch.dma('sp', slot['K2'][:, :1024], ap[:, col0:col0 + 1024], reads=[tk], writes=[slot['tK2']])
                rb = rv + (hh * 2 + hf) * 64
                c = rb // 128
                o_ = c * 512 + r * 128 + (rb % 128)
                blk = src[o_:o_ + 64, :].rearrange("r (a q) -> (r a) q", a=2).rearrange("p (i f) -> p i f", f=128)
                sch.dma('sp', slot['V'][:, :, :], blk, reads=[t_kvg[l][c]], writes=[slot['tV']])

            def attention(nq, groups):
                loads = [(kind, hh, g) for kind in ('da', 'mla') for hh in range(4) for g in groups]
                issued = [0]

                def ensure(n):
                    while issued[0] < min(n, len(loads)):
                        k_, h_, g_ = loads[issued[0]]
                        load_group(k_, h_, g_, slots[issued[0] % 3])
                        issued[0] += 1
                ensure(2)
                idx = 0
                for kind in ('da', 'mla'):
                    for hh in range(4):
                        tiles = []
                        for g in groups:
                            nt = 2 if g[0] == 'c' else 8
                            tiles += [(idx, tt) for tt in range(nt)]
                            idx += 1
                        n = len(tiles)
                        if kind == 'da':
                            def S1(i):
                                gi, tt = tiles[i]
                                if tt == 0:
                                    ensure(gi + 3)
                                sl = slots[gi % 3]
                                bk = 0 if i % 2 == 0 else 6
                                MM(psb[bk][:, :nq], sl['K1'][:, tt * 128:(tt + 1) * 128], qdaA[:, hh, :nq],
                                   True, True, [sl['tK1'], t_qda], [t_ps[bk]])

                            def S2(i):
                                gi, tt = tiles[i]
                                sl = slots[gi % 3]
                                bk = 1 if i % 2 == 0 else 7
                                MM(psb[bk][:, :nq], sl['K1'][:, tt * 128:(tt + 1) * 128], qdaB[:, hh, :nq],
                                   True, True, [sl['tK1'], t_qda], [t_ps[bk]])
                            S1(0)
                            S2(0)
                            for i in range(n):
                                gi, tt = tiles[i]
                                sl = slots[gi % 3]
                                p1, t_p1 = pring.next()
                                p2, t_p2 = pring.next()
                                b1, b2 = (0, 1) if i % 2 == 0 else (6, 7)
                                ACT(p1[:, :nq], psb[b1][:, :nq], AF.Exp, [t_ps[b1]], [t_p1], scale=0.125)
                                ACT(p2[:, :nq], psb[b2][:, :nq], AF.Exp, [t_ps[b2]], [t_p2], scale=0.125)
                                if i + 1 < n:
                                    S1(i + 1)
                                    S2(i + 1)
                                MM(psb[2][:, :nq], sl['V'][:, tt, :], p1[:, :nq], i == 0, i == n - 1,
                                   [sl['tV'], t_p1], [t_ps[2]])
                                MM(psb[3][:, :nq], sl['V'][:, tt, :], p2[:, :nq], i == 0, i == n - 1,
                                   [sl['tV'], t_p2], [t_ps[3]])
                                if i == 0:
                                    CP(pa[:, :nq], p1[:, :nq], [t_p1], [t_pa])
                                    CP(pb_[:, :nq], p2[:, :nq], [t_p2], [t_pb])
                                else:
                                    TT(pa[:, :nq], pa[:, :nq], p1[:, :nq], ALU.add, [t_pa, t_p1], [], pw=[t_pa])
                                    TT(pb_[:, :nq], pb_[:, :nq], p2[:, :nq], ALU.add, [t_pb, t_p2], [], pw=[t_pb])
                            MM(psb[4][:, :nq], ones_f, pa[:, :nq], True, True, [t_pa, t_consts], [t_ps[4]])
                            MM(psb[5][:, :nq], ones_f, pb_[:, :nq], True, True, [t_pb, t_consts], [t_ps[5]])
                            RECIP(fA[:, :nq], psb[4][:, :nq], [t_ps[4]], [t_fA])
                            TT(fB[:, :nq], psb[2][:, :nq], fA[:, :nq], ALU.mult, [t_ps[2], t_fA], [t_fB])
                            RECIP(fA[:, :nq], psb[5][:, :nq], [t_ps[5]], [t_fA])
                            TT(fC[:, :nq], psb[3][:, :nq], fA[:, :nq], ALU.mult, [t_ps[3], t_fA], [t_fC])
                            STT(da_a[:, hh, :nq], fC[:, :nq], neglam, fB[:, :nq], ALU.mult, ALU.add,
                                [t_fC, t_fB, t_misc], [], pw=[t_daa])
                        else:
                            pbase = 64 * (hh % 2)

                            def SM(i):
                                gi, tt = tiles[i]
                                if tt == 0:
                                    ensure(gi + 3)
                                sl = slots[gi % 3]
                                bk = i % 2
                                MM(psb[bk][:, :nq], sl['K1'][:, tt * 128:(tt + 1) * 128], qm[:, hh, :nq],
                                   True, False, [sl['tK1'], t_qm], [t_ps[bk]])
                                MM(psb[bk][:, :nq], sl['K2'][:, tt * 128:(tt + 1) * 128],
                                   qmr[:, hh, :nq], False, True, [sl['tK2'], t_qm], [t_ps[bk]])
                            SM(0)
                            for i in range(n):
                                gi, tt = tiles[i]
                                sl = slots[gi % 3]
                                if i + 1 < n:
                                    SM(i + 1)
                                p1, t_p1 = pring.next()
                                ACT(p1[:, :nq], psb[i % 2][:, :nq], AF.Exp, [t_ps[i % 2]], [t_p1], scale=192.0 ** -0.5)
                                MM(psb[2][:, :nq], sl['V'][:, tt, :], p1[:, :nq], i == 0, i == n - 1,
                                   [sl['tV'], t_p1], [t_ps[2]])
                                if i == 0:
                                    CP(pa[:, :nq], p1[:, :nq], [t_p1], [t_pa])
                                else:
                                    TT(pa[:, :nq], pa[:, :nq], p1[:, :nq], ALU.add, [t_pa, t_p1], [], pw=[t_pa])
                            MM(psb[4][:, :nq], ones_f, pa[:, :nq], True, True, [t_pa, t_consts], [t_ps[4]])
                            RECIP(fA[:, :nq], psb[4][:, :nq], [t_ps[4]], [t_fA])
                            TT(o_mla[:, hh, :nq], psb[2][:, :nq], fA[:, :nq], ALU.mult, [t_ps[2], t_fA], [],
                               pw=[t_omla])
                for hh in range(4):
                    sq, t_sq = R['bf'].next()
                    TT(sq[:, :nq], da_a[:, hh, :nq], da_a[:, hh, :nq], ALU.mult, [t_daa], [t_sq])
                    MM(psb[6][:, :nq], ones_bf, sq[:, :nq], True, True, [t_sq, t_consts], [t_ps[6]])
                    rstd, t_rstd = R['rstd'].next()
                    RSTD(rstd[:, :nq], psb[6][:, :nq], 1.0 / 128, [t_ps[6]], t_rstd)
                    STT(o_da[:, hh, :nq], da_a[:, hh, :nq], sublnS, rstd[:, :nq], ALU.mult, ALU.mult,
                        [t_daa, t_rstd, t_misc], [], pw=[t_oda])

            blocks = ([('c', 0)] if do_ctx else []) + [('l', b) for b in range(4)]
            bankrr = [0]

            def nb():
                b = bankrr[0] % 6
                bankrr[0] += 1
                return b

            for (kind, b) in blocks:
                isc = kind == 'c'
                nq = CTX if isc else 512
                tok0 = 0 if isc else b * 512
                j = 1 if isc else 0
                if isc:
                    xsrc = lambda kc: xc_sb[:, kc, :]
                    xdst = lambda kc: xc_sb[:, kc, :]
                    t_xs = [[t_xc[kc]] for kc in range(KC)]
                else:
                    xsrc = lambda kc, tok0=tok0: x_sb[:, kc, tok0:tok0 + 512]
                    xdst = xsrc
                    t_xs = [[t_x[kc][b]] for kc in range(KC)]
                norm_mod(R, xsrc, t_xs, nq, l, 0, j, h, t_h, bank=7)
                if not isc:
                    sch.dma('sp', ropeC[:], ropeC_d[:, tok0:tok0 + 512], writes=[t_rope])
                    sch.dma('sp', ropeS[:], ropeS_d[:, tok0:tok0 + 512], pwrites=[t_rope])
                for half in range(2):
                    ws, t_ws = wsl.next()
                    wq = ws[:, :].rearrange("p (k n) -> p k n", k=KC)
                    wload(wq, wv[:, :, C_DAQ + half * 256:C_DAQ + (half + 1) * 256], t_ws)
                    for dd in range(2):
                        hh = half * 2 + dd
                        bk = nb()
                        for kc in range(KC):
                            MM(psb[bk][:, :nq], wq[:, kc, dd * 128:(dd + 1) * 128], h[:, kc, :nq], kc == 0, kc == KC - 1,
                               [t_ws, t_h], [t_ps[bk]])
                        if isc:
                            CP(qdaA[0:64, hh, :nq], psb[bk][0:64, :nq], [t_ps[bk]], [], pw=[t_qda])
                            CP(qdaB[64:128, hh, :nq], psb[bk][64:128, :nq], [t_ps[bk]], [], pw=[t_qda])
                        else:
                            z, t_z = R['bf'].next()
                            CP(z[:, :nq], psb[bk][:, :nq], [t_ps[bk]], [t_z], eng='act')
                            rope(R, z[:, :nq], t_z, nq, 0, (qdaA[0:64, hh, :nq], qdaB[64:128, hh, :nq]), t_qda,
                                 ropeC, ropeS, t_rope, nb(), pw=True)
                qbanks = []
                for (c0, ncol) in ((0, 256), (256, 128)):
                    ws, t_ws = wsl.next()
                    wq = ws[:, 0:KC * ncol].rearrange("p (k n) -> p k n", k=KC)
                    wload(wq, wv[:, :, C_QD + c0:C_QD + c0 + ncol], t_ws)
                    for dd in range(ncol // 128):
                        bk = nb()
                        qbanks.append(bk)
                        for kc in range(KC):
                            MM(psb[bk][:, :nq], wq[:, kc, dd * 128:(dd + 1) * 128], h[:, kc, :nq], kc == 0, kc == KC - 1,
                               [t_ws, t_h], [t_ps[bk]])
                for ci, bk in enumerate(qbanks):
                    sq, t_sq = R['bf'].next()
                    ACT(sq[:, :nq], psb[bk][:, :nq], AF.Square, [t_ps[bk]], [t_sq])
                    MM(psb[6][:, :nq], ones_bf, sq[:, :nq], ci == 0, ci == 2, [t_sq, t_consts], [t_ps[6]])
                rstd, t_rstd = R['rstd'].next()
                RSTD(rstd[:, :nq], psb[6][:, :nq], 1.0 / 384, [t_ps[6]], t_rstd)
                for ci, bk in enumerate(qbanks):
                    STT(qn[:, ci, :nq], psb[bk][:, :nq], cols[:, ogq + ci:ogq + ci + 1], rstd[:, :nq],
                        ALU.mult, ALU.mult, [t_ps[bk], t_rstd, t_cols], [], pw=[t_qn])
                wuv = w_uq[l].rearrange("(kc p) n -> p kc n", p=128)
                ws, t_ws = wsl.next()
                wun = ws[:, 0:3 * 512].rearrange("p (k n) -> p k n", k=3)
                wload(wun, wuv[:, :, 0:512], t_ws)
                ws2, t_ws2 = wsl.next()
                wur = ws2[:, 0:3 * 256].rearrange("p (k n) -> p k n", k=3)
                wload(wur, wuv[:, :, 512:768], t_ws2)
                for hh in range(4):
                    bk = nb()
                    for kc in range(3):
                        MM(psb[bk][:, :nq], wun[:, kc, hh * 128:(hh + 1) * 128], qn[:, kc, :nq], kc == 0, kc == 2,
                           [t_ws, t_qn], [t_ps[bk]])
                    CP(qm[:, hh, :nq], psb[bk][:, :nq], [t_ps[bk]], [], pw=[t_qm])
                for rc in range(2):
                    bk = nb()
                    for kc in range(3):
                        MM(psb[bk][:, :nq], wur[:, kc, rc * 128:(rc + 1) * 128], qn[:, kc, :nq], kc == 0, kc == 2,
                           [t_ws2, t_qn], [t_ps[bk]])
                    if isc:
                        CP(qmr[0:64, 2 * rc, :nq], psb[bk][0:64, :nq], [t_ps[bk]], [], pw=[t_qm])
                        CP(qmr[64:128, 2 * rc + 1, :nq], psb[bk][64:128, :nq], [t_ps[bk]], [], pw=[t_qm])
                    else:
                        z, t_z = R['bf'].next()
                        CP(z[:, :nq], psb[bk][:, :nq], [t_ps[bk]], [t_z], eng='act')
                        rope(R, z[:, :nq], t_z, nq, 0, (qmr[0:64, 2 * rc, :nq], qmr[64:128, 2 * rc + 1, :nq]), t_qm,
                             ropeC, ropeS, t_rope, nb(), pw=True)
                groups = [('c',)] + ([] if isc else [('l', r, hf) for r in range(4) for hf in range(2)])
                attention(nq, groups)
                usrc = uc_sb if isc else u_sb
                tus = t_uc if isc else t_u
                E = nq + 16
                ws, t_ws = wsl.next()
                wpl = ws[:, 0:512].rearrange("p (g d) -> p g d", g=4)
                wload(wpl, pool_w[l].rearrange("g c d -> c g d"), t_ws)
                fixname = 'pfixc' if isc else 'pfix'
                ofx, _ = COLS[fixname]
                for gi, w in enumerate(POOL_WINDOWS):
                    ue = usrc[:, gi, tok0:tok0 + E]
                    TT(pa[:, 1:E], ue[:, 0:E - 1], ue[:, 1:E], ALU.add, [tus], [t_pa])
                    cur, tcur = pa, t_pa
                    if w >= 4:
                        TT(pb_[:, 2:E - 1], pa[:, 1:E - 2], pa[:, 3:E], ALU.add, [t_pa], [t_pb])
                        cur, tcur = pb_, t_pb
                    if w >= 8:
                        TT(pa[:, 4:E - 3], pb_[:, 2:E - 5], pb_[:, 6:E - 1], ALU.add, [t_pb], [t_pa])
                        cur, tcur = pa, t_pa
                    if w >= 16:
                        TT(pb_[:, 8:E - 7], pa[:, 4:E - 11], pa[:, 12:E - 3], ALU.add, [t_pa], [t_pb])
                        cur, tcur = pb_, t_pb
                    if isc or b == 0:
                        TT(cur[:, 8:16], cur[:, 8:16], cols[:, ofx + gi * 16:ofx + gi * 16 + 8], ALU.mult,
                           [tcur, t_cols], [], pw=[tcur])
                    if isc or b == 3:
                        TT(cur[:, nq:nq + 8], cur[:, nq:nq + 8], cols[:, ofx + gi * 16 + 8:ofx + gi * 16 + 16], ALU.mult,
                           [tcur, t_cols], [], pw=[tcur])
                    STT(dbf[:, :nq], cur[:, 8:8 + nq], 1.0 / w, ue[:, 8:8 + nq], ALU.mult, ALU.subtract,
                        [tcur, tus], [t_dbf])
                    bk = nb()
                    MM(psb[bk][:, :nq], wpl[:, gi, :], dbf[:, :nq], True, True, [t_ws, t_dbf], [t_ps[bk]])
                    TS(o_pool[:, gi, :nq], psb[bk][:, :nq], cols[:, ops_ + gi:ops_ + gi + 1], None, ALU.mult, None,
                       [t_ps[bk], t_cols], [], pw=[t_opool])
                wbv = w_branch[l].rearrange("n (cc p) d -> p n cc d", p=128)
                for n_, (o_n, t_on) in enumerate(((o_da, t_oda), (o_mla, t_omla), (o_pool, t_opool))):
                    for half in range(2):
                        ws, t_wb = wsl.next()
                        wb = ws[:, :].rearrange("p (c d) -> p c d", c=4)
                        wload(wb, wbv[:, n_, :, half * 512:(half + 1) * 512], t_wb)
                        for quarter in range(2):
                            dpair = half * 2 + quarter
                            ws2, t_wg = wsl.next()
                            wg = ws2[:, :].rearrange("p (k n) -> p k n", k=KC)
                            wload(wg, wv[:, :, C_GATE + n_ * 1024 + dpair * 256:C_GATE + n_ * 1024 + (dpair + 1) * 256], t_wg)
                            for dd in range(2):
                                dch = dpair * 2 + dd
                                bg = nb()
                                for kc in range(KC):
                                    MM(psb[bg][:, :nq], wg[:, kc, dd * 128:(dd + 1) * 128], h[:, kc, :nq],
                                       kc == 0, kc == KC - 1, [t_wg, t_h], [t_ps[bg]])
                                sig, t_sig = R['bf'].next()
                                ACT(sig[:, :nq], psb[bg][:, :nq], AF.Sigmoid, [t_ps[bg]], [t_sig])
                                bp = nb()
                                for cc in range(4):
                                    MM(psb[bp][:, :nq], wb[:, cc, (dch % 4) * 128:(dch % 4 + 1) * 128], o_n[:, cc, :nq],
                                       cc == 0, cc == 3, [t_wb, t_on], [t_ps[bp]])
                                if n_ == 0:
                                    TT(merged[:, dch, :nq], sig[:, :nq], psb[bp][:, :nq], ALU.mult, [t_sig, t_ps[bp]], [],
                                       pw=[t_merged])
                                else:
                                    tmp, t_tmp = R['f32'].next()
                                    TT(tmp[:, :nq], sig[:, :nq], psb[bp][:, :nq], ALU.mult, [t_sig, t_ps[bp]], [t_tmp])
                                    TT(merged[:, dch, :nq], merged[:, dch, :nq], tmp[:, :nq], ALU.add, [t_merged, t_tmp], [],
                                       pw=[t_merged])
                wov = w_out[l].rearrange("(kc p) n -> p kc n", p=128)
                for q4 in range(4):
                    ws, t_wo = wsl.next()
                    wo = ws[:, :].rearrange("p (k n) -> p k n", k=KC)
                    wload(wo, wov[:, :, q4 * 256:(q4 + 1) * 256], t_wo)
                    for dd in range(2):
                        dco = q4 * 2 + dd
                        bk = nb()
                        for kc in range(KC):
                            MM(psb[bk][:, :nq], wo[:, kc, dd * 128:(dd + 1) * 128], merged[:, kc, :nq],
                               kc == 0, kc == KC - 1, [t_wo, t_merged], [t_ps[bk]])
                        STT(xdst(dco), psb[bk][:, :nq], modcol(l, 2, dco, j), xsrc(dco), ALU.mult, ALU.add,
                            [t_ps[bk], t_mod] + t_xs[dco], [], pw=t_xs[dco])
                if 'mix' in debug and (not isc) and b == 0:
                    dmg = dram_out("dbg_merged", [128, KC * 512], BF16)
                    sch.dma('sp', dmg[:], merged[:].rearrange("p k t -> p (k t)"), reads=[t_merged])
                    dod = dram_out("dbg_o", [128, 3 * 4 * 512], BF16)
                    sch.dma('sp', dod[:, 0:2048], o_da[:].rearrange("p k t -> p (k t)"), reads=[t_oda])
                    sch.dma('sp', dod[:, 2048:4096], o_mla[:].rearrange("p k t -> p (k t)"), reads=[t_omla])
                    sch.dma('sp', dod[:, 4096:6144], o_pool[:].rearrange("p k t -> p (k t)"), reads=[t_opool])
        barrier()

    def phase3(l, do_ctx):
        moe = (l % 2 == 1)
        with ExitStack() as sc:
            lsb = mk_sb(sc)
            R = {'bf': Ring(lsb, "p3bf", 3, [128, 512], BF16), 'f32': Ring(lsb, "p3f", 4, [128, 512], F32),
                 'rstd': Ring(lsb, "p3r", 2, [128, 512], F32)}
            NT = TOK + (CTX if do_ctx else 0)
            h2 = lsb("h2", [128, KC, NT], BF16)
            t_h2 = Tk('h2')
            wsl = Ring(lsb, "w3", 5, [128, 4096], BF16)
            actb = [(lsb(f"actb{i}", [128, 4, 512], BF16), Tk(f'actb{i}')) for i in range(2)]
            blocks = [('l', b) for b in range(4)] + ([('c', 0)] if do_ctx else [])
            if moe:
                router_sb = lsb("router", [128, KC, NEXP], F32)
                t_router = Tk('router')
                sch.dma('sp', router_sb[:], router_d.rearrange("(kc p) e -> p kc e", p=128), writes=[t_router])
                gates = lsb("gates", [128, 16, NEXP], F32)
                gtmp = lsb("gtmp", [128, 16, NEXP], F32)
                gm = lsb("gm", [128, 16], F32)
                t_gates = Tk('gates')
                gbc = [(lsb(f"gbc{i}", [128, TOK], BF16), Tk(f'gbc{i}')) for i in range(2)]
                gmat = [(lsb(f"gmat{i}", [128, 128], F32), Tk(f'gmat{i}')) for i in range(2)]
                lgT = lsb("lgT", [32, 512], F32)
                t_lgT = Tk('lgT')
                sch.op('dve', lambda e: e.memset(lgT[:], 0.0), writes=[t_lgT])
            for (kind, b) in blocks:
                isc = kind == 'c'
                ntok = CTX if isc else 512
                j = 1 if isc else 0
                off = TOK if isc else b * 512
                if isc:
                    xsrc = lambda kc: xc_sb[:, kc, :]
                    t_xs = [[t_xc[kc]] for kc in range(KC)]
                else:
                    xsrc = lambda kc, off=off: x_sb[:, kc, off:off + 512]
                    t_xs = [[t_x[kc][b]] for kc in range(KC)]
                cb = None
                if moe:
                    def cb(kc, tmp, t_tmp, b=b, j=j):
                        hf, t_hf = R['f32'].next()
                        ACT(hf[:, :512], tmp[:, :512], AF.Identity, [t_tmp, t_mod], [t_hf],
                            scale=modA[:, l, 1, kc, j:j + 1], bias=modcol(l, 3, kc, j))
                        MM(psb[6][0:NEXP, :], router_sb[:, kc, :], hf[:, :512], kc == 0, kc == KC - 1,
                           [t_hf, t_router], [t_ps[6]])
                        if kc == KC - 1:
                            CP(lgT[0:NEXP, :], psb[6][0:NEXP, :], [t_ps[6]], [], pw=[t_lgT])
                            for ti in range(4):
                                c0 = (b * 4 + ti) * NEXP
                                MM(psb[5][:, c0:c0 + NEXP], lgT[0:32, ti * 128:(ti + 1) * 128],
                                   ident_f[0:32, 0:NEXP], True, True, [t_lgT, t_consts], [t_ps[5]])
                norm_mod(R, xsrc, t_xs, ntok, l, 1, j, h2[:, :, off:off + ntok], t_h2, f32cb=cb, bank=7)
            if moe:
                lg = psb[5][:, 0:16 * NEXP].rearrange("p (t e) -> p t e", e=NEXP)
                CP(gates[:], lg, [t_ps[5]], [t_gates])
                sch.op('dve', lambda e: e.tensor_reduce(out=gm[:], in_=gates[:], axis=mybir.AxisListType.X, op=ALU.max),
                       reads=[t_gates], pwrites=[t_gates])
                gmb = gm[:, :, None].to_broadcast([128, 16, NEXP])
                TT(gtmp[:], gates[:], gmb, ALU.is_equal, [t_gates], [], pw=[t_gates])
                STT(gtmp[:], gtmp[:], -BIG, gates[:], ALU.mult, ALU.add, [t_gates], [], pw=[t_gates])
                gm2 = lsb("gm2", [128, 16], F32)
                sch.op('dve', lambda e: e.tensor_reduce(out=gm2[:], in_=gtmp[:], axis=mybir.AxisListType.X, op=ALU.max),
                       reads=[t_gates], pwrites=[t_gates])
                gm2b = gm2[:, :, None].to_broadcast([128, 16, NEXP])
                TT(gtmp[:], gates[:], gm2b, ALU.is_ge, [t_gates], [], pw=[t_gates])
                TT(gates[:], gates[:], gmb, ALU.subtract, [t_gates], [], pw=[t_gates])
                ACT(gates[:], gates[:], AF.Exp, [t_gates], [], pw=[t_gates])
                TT(gates[:], gates[:], gtmp[:], ALU.mult, [t_gates], [], pw=[t_gates])
                sch.op('dve', lambda e: e.tensor_reduce(out=gm[:], in_=gates[:], axis=mybir.AxisListType.X, op=ALU.add),
                       reads=[t_gates], pwrites=[t_gates])
                sch.op('dve', lambda e: e.reciprocal(out=gm[:], in_=gm[:]), reads=[t_gates], pwrites=[t_gates])
                TT(gates[:], gates[:], gmb, ALU.mult, [t_gates], [], pw=[t_gates])
                if 'gates' in debug:
                    dg = dram_out("dbg_gates", [128, 16 * NEXP])
                    sch.dma('sp', dg[:], gates[:].rearrange("p t e -> p (t e)"), reads=[t_gates])

            def ffn_expert(wgv, wuv, wdv, dff, gb, t_gb):
                nfg = (dff + 511) // 512
                bi = 0
                for fg in range(nfg):
                    nf = min(512, dff - fg * 512)
                    nfc = nf // 128
                    ws, t_wg = wsl.next()
                    wg = ws[:, 0:KC * nf].rearrange("p (k n) -> p k n", k=KC)
                    wload(wg, wgv[:, :, fg * 512:fg * 512 + nf], t_wg)
                    ws, t_wu = wsl.next()
                    wu = ws[:, 0:KC * nf].rearrange("p (k n) -> p k n", k=KC)
                    wload(wu, wuv[:, :, fg * 512:fg * 512 + nf], t_wu)
                    ws, t_wd = wsl.next()
                    wd = ws[:, 0:nfc * 1024].rearrange("p (c n) -> p c n", c=nfc)
                    wload(wd, wdv[:, fg * 4:fg * 4 + nfc, :], t_wd)
                    for (kind, b) in blocks:
                        isc = kind == 'c'
                        ntok = CTX if isc else 512
                        j = 1 if isc else 0
                        off = TOK if isc else b * 512
                        t_xs = [[t_xc[kc]] for kc in range(KC)] if isc else [[t_x[kc][b]] for kc in range(KC)]
                        xs = (lambda kc: xc_sb[:, kc, :]) if isc else (lambda kc, off=off: x_sb[:, kc, off:off + 512])
                        ab, t_ab = actb[bi % 2]
                        bi += 1
                        for fc in range(nfc):
                            pg, pu = (0, 1) if fc % 2 == 0 else (2, 3)
                            for kc in range(KC):
                                MM(psb[pg][:, :ntok], wg[:, kc, fc * 128:(fc + 1) * 128], h2[:, kc, off:off + ntok],
                                   kc == 0, kc == KC - 1, [t_wg, t_h2], [t_ps[pg]])
                            for kc in range(KC):
                                MM(psb[pu][:, :ntok], wu[:, kc, fc * 128:(fc + 1) * 128], h2[:, kc, off:off + ntok],
                                   kc == 0, kc == KC - 1, [t_wu, t_h2], [t_ps[pu]])
                            sg, t_sg = R['f32'].next()
                            ACT(sg[:, :ntok], psb[pg][:, :ntok], AF.Silu, [t_ps[pg]], [t_sg])
                            if gb is None:
                                TT(ab[:, fc, :ntok], sg[:, :ntok], psb[pu][:, :ntok], ALU.mult, [t_sg, t_ps[pu]], [],
                                   pw=[t_ab])
                            else:
                                TT(sg[:, :ntok], sg[:, :ntok], psb[pu][:, :ntok], ALU.mult, [t_sg, t_ps[pu]], [t_sg])
                                TT(ab[:, fc, :ntok], sg[:, :ntok], gb[:, off:off + ntok], ALU.mult, [t_sg, t_gb], [],
                                   pw=[t_ab])
                        for dch in range(KC):
                            py = 4 + (dch % 2)
                            for fc in range(nfc):
                                MM(psb[py][:, :ntok], wd[:, fc, dch * 128:(dch + 1) * 128], ab[:, fc, :ntok],
                                   fc == 0, fc == nfc - 1, [t_wd, t_ab], [t_ps[py]])
                            STT(xs(dch), psb[py][:, :ntok], modcol(l, 5, dch, j), xs(dch), ALU.mult, ALU.add,
                                [t_ps[py], t_mod] + t_xs[dch], [], pw=t_xs[dch])

            if not moe:
                ffn_expert(ffn_wg.rearrange("(kc p) n -> p kc n", p=128), ffn_wu.rearrange("(kc p) n -> p kc n", p=128),
                           ffn_wd.rearrange("(c p) n -> p c n", p=128), D_FF, None, None)
            else:
                for ex in range(NEXP):
                    gb, t_gb = gbc[ex % 2]
                    for ti in range(16):
                        gmt, t_gmt = gmat[ti % 2]
                        TS(gmt[:], ones_f, gates[:, ti, ex:ex + 1], None, ALU.mult, None, [t_consts, t_gates], [t_gmt])
                        bk = 6 + (ti // 4) % 2
                        MM(psb[bk][:, (ti % 4) * 128:(ti % 4 + 1) * 128], gmt[:], ident_f, True, True,
                           [t_gmt, t_consts], [t_ps[bk]])
                        if ti % 4 == 3:
                            CP(gb[:, (ti // 4) * 512:(ti // 4 + 1) * 512], psb[bk][:, :], [t_ps[bk]], [],
                               pw=[t_gb])
                    ffn_expert(moe_wg[ex].rearrange("(kc p) n -> p kc n", p=128),
                               moe_wu[ex].rearrange("(kc p) n -> p kc n", p=128),
                               moe_wd[ex].rearrange("(c p) n -> p c n", p=128), D_FFE, gb, t_gb)
        barrier()

    def final_norm():
        with ExitStack() as sc:
            lsb = mk_sb(sc)
            R = {'bf': Ring(lsb, "fnbf", 3, [128, 512], BF16), 'f32': Ring(lsb, "fnf", 4, [128, 512], F32),
                 'rstd': Ring(lsb, "fnr", 2, [128, 512], F32)}
            ogf, _ = COLS['gfin']
            for b in range(4):
                off = b * 512
                for kc in range(KC):
                    sq, t_sq = R['bf'].next()
                    ACT(sq[:], x_sb[:, kc, off:off + 512], AF.Square, [t_x[kc][b]], [t_sq])
                    MM(psb[7][:], ones_bf, sq[:], kc == 0, kc == KC - 1, [t_sq, t_consts], [t_ps[7]])
                rstd, t_rstd = R['rstd'].next()
                RSTD(rstd[:], psb[7][:], 1.0 / D, [t_ps[7]], t_rstd)
                for kc in range(KC):
                    STT(x_sb[:, kc, off:off + 512], x_sb[:, kc, off:off + 512], cols[:, ogf + kc:ogf + kc + 1], rstd[:],
                        ALU.mult, ALU.mult, [t_x[kc][b], t_rstd, t_cols], [], pw=[t_x[kc][b]])
        barrier()

    def gather(l):
        for c in range(NCH + 1):
            if c < NCH:
                src = kvl[l][c * 128:(c + 1) * 128, :]
                dst = kvg[l][c * 512:(c + 1) * 512, :]
            else:
                src = kvl[l][R_U:R_U + 8, :]
                dst = kvg[l][NCH * 512:NCH * 512 + 32, :]
            sch.coll((lambda e, src=src, dst=dst: e.collective_compute(
                "AllGather", ALU.bypass, replica_groups=[[0, 1, 2, 3], [4, 5, 6, 7]], ins=[src], outs=[dst])),
                reads=[t_kvl[l]], writes=[t_kvg[l][c]])

    if A_:
        phase1(0, True)
    if B_:
        phase2(0, True)
        phase3(0, True)
        phase1(1, False)
        x1T = dram_out("x1T", [D, TOK])
        t_o1 = Tk('o1')
        for kc in range(KC):
            sch.dma('sp', x1T[kc * 128:(kc + 1) * 128, :], x_sb[:, kc, :], reads=t_x[kc], pwrites=[t_o1])
    if ALL:
        phase1(0, True)
        gather(0)
        mods_stage(MODS_REST, False)
        phase2(0, True)
        phase3(0, True)
        phase1(1, False)
        gather(1)
        phase2(1, False)
        phase3(1, False)
        final_norm()
    if C_:
        phase2(1, False)
        if 'xa' in debug:
            dxa = dram_out("dbg_xa", [D, TOK])
            for kc in range(KC):
                sch.dma('sp', dxa[kc * 128:(kc + 1) * 128, :], x_sb[:, kc, :], reads=t_x[kc])
            barrier()
        phase3(1, False)
        final_norm()

    if 'mod' in debug:
        dmod = dram_out("dbg_mod", [128, DEPTH * 96])
        sch.dma('sp', dmod[:], mod_sb[:].rearrange("p l m j -> p (l m j)"), reads=[t_mod])
        dmisc = dram_out("dbg_misc", [128, 16])
        sch.dma('sp', dmisc[:], misc[:], reads=[t_misc])
    if 'u' in debug:
        du = dram_out("dbg_u", [128, 4 * (TOK + 16)], BF16)
        sch.dma('sp', du[:], u_sb[:].rearrange("p g t -> p (g t)"), reads=[t_u])
    if 'x' in debug:
        dx = dram_out("dbg_x", [D, TOK])
        for kc in range(KC):
            sch.dma('sp', dx[kc * 128:(kc + 1) * 128, :], x_sb[:, kc, :], reads=t_x[kc])
        dxc = dram_out("dbg_xc", [D, CTX])
        for kc in range(KC):
            sch.dma('sp', dxc[kc * 128:(kc + 1) * 128, :], xc_sb[:, kc, :], reads=[t_xc[kc]])

    t_out = Tk('out')
    if C_ or ALL:
        outT = dram_out("outT", [D, TOK])
        for kc in range(KC):
            sch.dma('sp', outT[kc * 128:(kc + 1) * 128, :], x_sb[:, kc, :], reads=t_x[kc], pwrites=[t_out])
    barrier()
    sch.emit(nc, top)
    top.close()
    return nc


def _rope_tables():
    n_freq = 16
    inv = np.exp(-math.log(10000.0) * np.arange(n_freq, dtype=np.float32) * np.float32(2.0 / 32)).astype(np.float32)
    t = np.arange(S)
    row = (t // 64).astype(np.float32)
    colp = (t % 64).astype(np.float32)
    ar = row[:, None] * inv[None, :]
    ac = colp[:, None] * inv[None, :]
    C = np.zeros((128, S), np.float32)
    Sg = np.zeros((128, S), np.float32)
    for p in range(128):
        d = p % 64
        ang = ar if d < 32 else ac
        i = d % 16
        sign = -1.0 if (d % 32) < 16 else 1.0
        C[p] = np.cos(ang[:, i])
        Sg[p] = sign * np.sin(ang[:, i])
    return C, Sg


def _pool_fix(L, t_start, n):
    f = np.ones((4, 16), np.float32)
    for g, w in enumerate(POOL_WINDOWS):
        for k in range(16):
            t = t_start + k if k < 8 else t_start + n - 16 + k
            lo = min(max(t - w // 2, 0), L)
            hi = min(max(t - w // 2 + w, 0), L)
            f[g, k] = w / float(hi - lo)
    return f


def host_common(inp):
    cols = np.zeros((128, NCOLS), np.float32)

    def put(name, arr):
        o, w = COLS[name]
        assert arr.shape == (128, w), (name, arr.shape, w)
        cols[:, o:o + w] = arr
    put('cctx', _colvec(inp['c_ctx']))
    for l in range(DEPTH):
        put(f'bmod{l}', _colvec(inp['b_mod'][l]))
        put(f'gmix{l}', _colvec(inp['g_mix'][l]))
        put(f'gffn{l}', _colvec(inp['g_ffn'][l]))
        put(f'gq{l}', _colvec(inp['mla_gq'][l]))
        put(f'gkv{l}', _colvec(inp['mla_gkv'][l]))
        put(f'pscale{l}', _colvec(inp['pool_scale'][l]))
        put(f'subln{l}', _colvec(inp['da_subln'][l]))
        lam = np.zeros((128, 4), np.float32)
        lam[:64, :] = np.asarray(inp['da_lambda'][l], np.float32).T
        lam[64:, :] = 0.0
        put(f'lam{l}', lam)
    put('gfin', _colvec(inp['g_final']))
    put('pfixc', np.broadcast_to(_pool_fix(CTX, 0, CTX).reshape(1, 64), (128, 64)).copy())
    consts = np.zeros((128, 384), np.float32)
    consts[:, 0:128] = 1.0
    consts[:, 128:256] = np.eye(128, dtype=np.float32)
    for m in range(128):
        partner = (m & ~31) | ((m & 31) ^ 16)
        consts[partner, 256 + m] = 1.0
    constf = np.ascontiguousarray(consts[:, 0:256])
    consts = consts.astype(ml_dtypes.bfloat16)
    C, Sg = _rope_tables()
    uq_perm = np.concatenate([np.arange(h * 192, h * 192 + 128) for h in range(4)] +
                             [np.arange(h * 192 + 128, h * 192 + 192) for h in range(4)])
    ukv_perm = np.concatenate([np.arange(h * 256, h * 256 + 128) for h in range(4)] +
                              [np.arange(h * 256 + 128, h * 256 + 256) for h in range(4)])
    f32 = lambda a: np.ascontiguousarray(np.asarray(a, np.float32))
    shared = {
        "consts": consts, "constf": constf,
        "w_mod": f32(inp['w_mod']), "w_in": f32(inp['w_in']),
        "w_uq_p": f32(np.asarray(inp['w_uq'])[:, :, uq_perm]),
        "w_ukv_p": f32(np.asarray(inp['w_ukv'])[:, :, ukv_perm]),
        "pool_w": f32(inp['pool_w']), "w_branch": f32(inp['w_branch']), "w_out": f32(inp['w_out']),
    }
    percore = []
    for core in range(NCORE):
        b = core // 4
        t0 = (core % 4) * TOK
        cc = cols.copy()
        o, w = COLS['c']
        cc[:, o:o + w] = _colvec(inp['c'][b])
        o, w = COLS['pfix']
        cc[:, o:o + w] = np.broadcast_to(_pool_fix(S, t0, TOK).reshape(1, 64), (128, 64))
        s_ = core % 4
        o, w = COLS['selL']
        if s_ > 0:
            cc[:, o + s_ - 1] = 1.0
        o, w = COLS['selR']
        if s_ < 3:
            cc[:, o + s_ + 1] = 1.0
        percore.append({
            "cols": cc,
            "ropeC": np.ascontiguousarray(C[:, t0:t0 + TOK]).astype(ml_dtypes.bfloat16),
            "ropeS": np.ascontiguousarray(Sg[:, t0:t0 + TOK]).astype(ml_dtypes.bfloat16),
        })
    return shared, percore


def _gather_kv(res, name_l, name_c):
    per = []
    for core in range(NCORE):
        b, s = core // 4, core % 4
        sh = [np.asarray(res[b * 4 + r][name_l]) for r in range(4)]
        kvg = np.concatenate([sh[r][c * 128:(c + 1) * 128] for c in range(NCH) for r in range(4)] +
                             [sh[r][R_U:R_U + 8] for r in range(4)], axis=0)
        hl = np.zeros((128, 4, 16), ml_dtypes.bfloat16)
        if s > 0:
            nb_ = np.asarray(res[core - 1][name_l])[R_U:R_U + 4].reshape(4, 128, 16)
            hl[:, :, 0:8] = nb_[:, :, 8:16].transpose(1, 0, 2)
        if s < 3:
            nb_ = np.asarray(res[core + 1][name_l])[R_U:R_U + 4].reshape(4, 128, 16)
            hl[:, :, 8:16] = nb_[:, :, 0:8].transpose(1, 0, 2)
        per.append((np.ascontiguousarray(kvg), np.asarray(res[core][name_c]), hl))
    return per


def stage_maps(inputs, shared, percore, stage, prev=None):
    f32 = lambda a: np.ascontiguousarray(np.asarray(a, np.float32))
    x = np.asarray(inputs['x'], np.float32)
    ctx = np.asarray(inputs['ctx'], np.float32)
    extra = {}
    if stage in ('B', 'all'):
        extra.update(ffn_wg=f32(inputs['ffn_w_gate'][0]), ffn_wu=f32(inputs['ffn_w_up'][0]),
                     ffn_wd=f32(inputs['ffn_w_down'][0]))
    if stage in ('C', 'all'):
        extra.update(router=f32(inputs['moe_router'][0]), moe_wg=f32(inputs['moe_w_gate'][0]),
                     moe_wu=f32(inputs['moe_w_up'][0]), moe_wd=f32(inputs['moe_w_down'][0]))
    maps = []
    for core in range(NCORE):
        b = core // 4
        t0 = (core % 4) * TOK
        m = dict(shared)
        m.update(percore[core])
        m.update(extra)
        m["ctxT"] = np.ascontiguousarray(ctx[b].T)
        if stage == 'C':
            m["xT"] = np.ascontiguousarray(prev['x1T'][core])
        else:
            m["xT"] = np.ascontiguousarray(x[b, t0:t0 + TOK, :].T)
        if stage == 'B':
            m["kvg0"], m["kvc0"], m["halo0"] = prev['kv'][core]
            m["u_in"] = prev['u'][core]
            m["uc_in"] = prev['uc'][core]
        if stage == 'C':
            m["kvg1"], m["kvc1"], m["halo1"] = prev['kv'][core]
            m["u_in"] = prev['u'][core]
        maps.append(m)
    return maps


def kernel_unfused(**inputs):
    shared, percore = host_common(inputs)
    cores = list(range(NCORE))
    ra = run_bass_kernel_spmd(build('A'), stage_maps(inputs, shared, percore, 'A'), core_ids=cores).results
    kv0 = _gather_kv(ra, 'kvl0', 'kvc0')
    pa = dict(kv=kv0, u=[np.asarray(ra[c]['u_out']) for c in cores], uc=[np.asarray(ra[c]['uc_out']) for c in cores])
    rb = run_bass_kernel_spmd(build('B'), stage_maps(inputs, shared, percore, 'B', pa), core_ids=cores).results
    kv1 = _gather_kv(rb, 'kvl1', 'kvc1')
    pb = dict(kv=kv1, x1T=[np.asarray(rb[c]['x1T']) for c in cores], u=[np.asarray(rb[c]['u_out']) for c in cores])
    rc = run_bass_kernel_spmd(build('C'), stage_maps(inputs, shared, percore, 'C', pb), core_ids=cores).results
    out = np.zeros((2, S, D), np.float32)
    for core in cores:
        b = core // 4
        t0 = (core % 4) * TOK
        out[b, t0:t0 + TOK, :] = np.asarray(rc[core]["outT"]).T
    return out


def kernel(**inputs):
    shared, percore = host_common(inputs)
    cores = list(range(NCORE))
    res = run_bass_kernel_spmd(build('all'), stage_maps(inputs, shared, percore, 'all'), core_ids=cores).results
    out = np.zeros((2, S, D), np.float32)
    for core in cores:
        b = core // 4
        t0 = (core % 4) * TOK
        out[b, t0:t0 + TOK, :] = np.asarray(res[core]["outT"]).T
    return out
```

```python
import math
from contextlib import ExitStack
import numpy as np
import ml_dtypes
import concourse.bass as bass
import concourse.mybir as mybir
from concourse.bass_utils import run_bass_kernel_spmd

F32 = mybir.dt.float32
BF16 = mybir.dt.bfloat16
AF = mybir.ActivationFunctionType
ALU = mybir.AluOpType

D = 1024
KC = 8
S = 8192
NCORE = 8
TOK = 2048
CTX = 256
NKEY = S + CTX
DEPTH = 2
IN_W = 5824
D_FF = 2816
NEXP = 8
D_FFE = 3584
EPS = 1e-6

ENGS = ('pe', 'act', 'dve', 'pool', 'sp')


class Tk:
    __slots__ = ('name', 'w', 'r')

    def __init__(self, name):
        self.name = name
        self.w = {}
        self.r = {}


class Op:
    __slots__ = ('eng', 'fn', 'waits', 'idx', 'signal', 'dma', 'signo')

    def __init__(self, eng, fn, idx):
        self.eng = eng
        self.fn = fn
        self.waits = []
        self.idx = idx
        self.signal = False
        self.dma = None
        self.signo = None


class Sched:
    NDMA = 12
    EPOCH = 12000

    def __init__(self):
        self.ops = {e: [] for e in ENGS}
        self.seen = {e: {} for e in ENGS}
        self.dma_count = {e: 0 for e in ENGS}
        self.dma_slot_val = {}

    def _deps(self, reads, writes):
        deps = {}

        def add(d):
            for k, v in d.items():
                if deps.get(k, -1) < v:
                    deps[k] = v
        for t in reads:
            add(t.w)
        for t in writes:
            add(t.w)
            add(t.r)
        return deps

    def _apply_waits(self, op, deps, raw_keys):
        eng = op.eng
        seen = self.seen[eng]
        for k, v in deps.items():
            if k[0] == 'e' and k[1] == eng and k not in raw_keys:
                continue
            if seen.get(k, -1) >= v:
                continue
            seen[k] = v
            op.waits.append((k, v))
            if k[0] == 'e':
                self.ops[k[1]][v].signal = True

    def op(self, eng, fn, reads=(), writes=(), pwrites=()):
        o = Op(eng, fn, len(self.ops[eng]))
        deps = self._deps(reads, list(writes) + list(pwrites))
        raw = set()
        for t in reads:
            raw.update(t.w.keys())
        self._apply_waits(o, deps, raw)
        self.ops[eng].append(o)
        key = ('e', eng)
        for t in writes:
            t.w = {key: o.idx}
            t.r = {}
        for t in pwrites:
            t.w[key] = o.idx
        for t in reads:
            t.r[key] = o.idx
        return o

    def dma(self, q, out, in_, reads=(), writes=(), pwrites=()):
        n = self.dma_count[q]
        self.dma_count[q] += 1
        slot = n % self.NDMA
        key = ('d', q, slot)
        val = 16 * (n // self.NDMA + 1)
        o = Op(q, lambda e: e.dma_start(out=out, in_=in_), len(self.ops[q]))
        o.dma = (key, val)
        deps = self._deps(reads, list(writes) + list(pwrites))
        if val > 16:
            deps[key] = val - 16
        raw = set()
        for t in reads:
            raw.update(t.w.keys())
        raw.add(key)
        self._apply_waits(o, deps, raw)
        self.ops[q].append(o)
        for t in writes:
            t.w = {key: val}
            t.r = {}
        for t in pwrites:
            t.w[key] = val
        for t in reads:
            t.r[key] = val
        return o

    def coll(self, fn, reads=(), writes=(), pwrites=()):
        q = 'pool'
        n = self.ncoll = getattr(self, 'ncoll', 0) + 1
        key = ('c', n)
        o = Op(q, fn, len(self.ops[q]))
        o.dma = (key, 1)
        deps = self._deps(reads, list(writes) + list(pwrites))
        raw = set()
        for t in reads:
            raw.update(t.w.keys())
        self._apply_waits(o, deps, raw)
        self.ops[q].append(o)
        for t in writes:
            t.w = {key: 1}
            t.r = {}
        for t in pwrites:
            t.w[key] = 1
        for t in reads:
            t.r[key] = 1
        return o

    def emit(self, nc, stack):
        esems = {}
        for e in ENGS:
            k = 0
            for o in self.ops[e]:
                if o.dma is None and o.signal:
                    o.signo = k
                    k += 1
            nep = (k + self.EPOCH - 1) // self.EPOCH
            esems[e] = [stack.enter_context(nc.semaphore(f"s_{e}_{i}")) for i in range(nep)]
        dsems = {}
        for e in ENGS:
            for sl in range(min(self.NDMA, self.dma_count[e])):
                dsems[('d', e, sl)] = stack.enter_context(nc.semaphore(f"d_{e}_{sl}"))
        for i in range(1, getattr(self, 'ncoll', 0) + 1):
            dsems[('c', i)] = stack.enter_context(nc.semaphore(f"c_{i}"))
        ops = self.ops
        EP = self.EPOCH

        def run(ename, eng):
            for o in ops[ename]:
                for k, v in o.waits:
                    if k[0] in ('d', 'c'):
                        eng.wait_ge(dsems[k], v)
                    else:
                        sn = ops[k[1]][v].signo
                        eng.wait_ge(esems[k[1]][sn // EP], sn % EP + 1)
                ins = o.fn(eng)
                if o.dma is not None:
                    ins.then_inc(dsems[o.dma[0]], 1 if o.dma[0][0] == 'c' else 16)
                elif o.signal:
                    ins.then_inc(esems[ename][o.signo // EP], 1)

        with nc.Block() as block:
            @block.tensor
            def _(e):
                run('pe', e)

            @block.scalar
            def _(e):
                run('act', e)

            @block.vector
            def _(e):
                run('dve', e)

            @block.gpsimd
            def _(e):
                run('pool', e)

            @block.sync
            def _(e):
                run('sp', e)


R_KDA, R_KN, R_KR, R_VDA, R_VM, R_U, ROWS = 0, 512, 1024, 1152, 1664, 2176, 2184
NCH = 17
GROWS = NCH * 512 + 32
C_DAQ, C_DAK, C_DAV, C_QD, C_KVD, C_KR, C_POOL, C_GATE = 0, 512, 1024, 1536, 1920, 2176, 2240, 2752
LAM_INIT = [0.8 - 0.6 * math.exp(-0.3 * l) for l in range(DEPTH)]
POOL_WINDOWS = (2, 4, 8, 16)
BIG = 1.0e4


def _cols_layout():
    off = {}
    n = 0

    def add(name, w):
        nonlocal n
        off[name] = (n, w)
        n += w
    add('c', 8)
    add('cctx', 8)
    for l in range(DEPTH):
        add(f'bmod{l}', 48)
        add(f'gmix{l}', 8)
        add(f'gffn{l}', 8)
        add(f'gq{l}', 3)
        add(f'gkv{l}', 2)
        add(f'pscale{l}', 4)
        add(f'subln{l}', 1)
        add(f'lam{l}', 4)
    add('gfin', 8)
    add('pfix', 64)
    add('pfixc', 64)
    add('selL', 4)
    add('selR', 4)
    return off, n


COLS, NCOLS = _cols_layout()


def _colvec(v):
    v = np.asarray(v, np.float32)
    return np.ascontiguousarray(v.reshape(-1, 128).T)


class Ring:
    def __init__(self, mk, name, n, shape, dt):
        self.bufs = [(mk(f"{name}{i}", shape, dt), Tk(f"{name}{i}")) for i in range(n)]
        self.i = 0

    def next(self):
        b = self.bufs[self.i % len(self.bufs)]
        self.i += 1
        return b


def build(stage='all', debug=()):
    nc = bass.Bass("TRN2", target_bir_lowering=False)
    sch = Sched()
    top = ExitStack()

    def dram_in(name, shape, dt=F32):
        return nc.dram_tensor(name, list(shape), dt, kind="ExternalInput").ap()

    def dram_out(name, shape, dt=F32):
        return nc.dram_tensor(name, list(shape), dt, kind="ExternalOutput").ap()

    uniq = [0]

    def mk_sb(stack):
        def f(name, shape, dt):
            uniq[0] += 1
            return stack.enter_context(nc.sbuf_tensor(f"sb{uniq[0]}_{name}", list(shape), dt))
        return f
    sb = mk_sb(top)

    def MM(out, lhsT, rhs, start, stop, reads, writes):
        return sch.op('pe', lambda e: e.matmul(out, lhsT=lhsT, rhs=rhs, start=start, stop=stop),
                      reads=reads, writes=writes)

    def ACT(out, in_, func, reads, writes, scale=None, bias=None, pw=()):
        kw = {}
        if scale is not None:
            kw['scale'] = scale
        if bias is not None:
            kw['bias'] = bias
        return sch.op('act', lambda e: e.activation(out=out, in_=in_, func=func, **kw),
                      reads=reads, writes=writes, pwrites=pw)

    def TT(out, in0, in1, op, reads, writes, eng='dve', pw=()):
        return sch.op(eng, lambda e: e.tensor_tensor(out=out, in0=in0, in1=in1, op=op),
                      reads=reads, writes=writes, pwrites=pw)

    def TS(out, in0, s1, s2, op0, op1, reads, writes, eng='dve', pw=()):
        if op1 is None:
            return sch.op(eng, lambda e: e.tensor_scalar(out=out, in0=in0, scalar1=s1, scalar2=None, op0=op0),
                          reads=reads, writes=writes, pwrites=pw)
        return sch.op(eng, lambda e: e.tensor_scalar(out=out, in0=in0, scalar1=s1, scalar2=s2, op0=op0, op1=op1),
                      reads=reads, writes=writes, pwrites=pw)

    def STT(out, in0, scalar, in1, op0, op1, reads, writes, pw=()):
        return sch.op('dve', lambda e: e.scalar_tensor_tensor(out=out, in0=in0, scalar=scalar, in1=in1,
                                                              op0=op0, op1=op1),
                      reads=reads, writes=writes, pwrites=pw)

    def CP(out, in_, reads, writes, eng='dve', pw=()):
        if eng == 'act':
            return sch.op('act', lambda e: e.copy(out=out, in_=in_), reads=reads, writes=writes, pwrites=pw)
        return sch.op(eng, lambda e: e.tensor_copy(out=out, in_=in_), reads=reads, writes=writes, pwrites=pw)

    def RSTD(out, ps_in, inv_n, reads, t_out):
        ACT(out, ps_in, AF.Sqrt, reads, [t_out], scale=inv_n, bias=EPS)
        sch.op('dve', lambda e: e.reciprocal(out=out, in_=out), reads=[t_out], writes=[t_out])

    def RECIP(out, in_, reads, writes):
        return sch.op('dve', lambda e: e.reciprocal(out=out, in_=in_), reads=reads, writes=writes)

    def barrier():
        last = {e: len(sch.ops[e]) - 1 for e in ENGS}
        dl = {}
        for q in ENGS:
            n = sch.dma_count[q]
            for sl in range(min(n, sch.NDMA)):
                uses = (n - 1 - sl) // sch.NDMA + 1
                dl[('d', q, sl)] = 16 * uses
        for e in ENGS:
            o = Op(e, lambda en: en.nop(), len(sch.ops[e]))
            deps = dict(dl)
            for e2 in ENGS:
                if e2 != e and last[e2] >= 0:
                    k = last[e2]
                    while k >= 0 and sch.ops[e2][k].dma is not None:
                        k -= 1
                    if k >= 0:
                        deps[('e', e2)] = k
            sch._apply_waits(o, deps, set(deps.keys()))
            sch.ops[e].append(o)

    A_ = stage == 'A'
    B_ = stage == 'B'
    C_ = stage == 'C'
    ALL = stage == 'all'

    xT = dram_in("xT", [D, TOK])
    ctxT = dram_in("ctxT", [D, CTX])
    cols_d = dram_in("cols", [128, NCOLS])
    consts_d = dram_in("consts", [128, 384], BF16)
    constf_d = dram_in("constf", [128, 256], F32)
    ropeC_d = dram_in("ropeC", [128, TOK], BF16)
    ropeS_d = dram_in("ropeS", [128, TOK], BF16)
    w_mod = dram_in("w_mod", [DEPTH, D, 6 * D])
    w_in = dram_in("w_in", [DEPTH, D, IN_W])
    w_uq = dram_in("w_uq_p", [DEPTH, 384, 768])
    w_ukv = dram_in("w_ukv_p", [DEPTH, 256, 1024])
    pool_w = dram_in("pool_w", [DEPTH, 4, 128, 128])
    w_branch = dram_in("w_branch", [DEPTH, 3, 512, D])
    w_out = dram_in("w_out", [DEPTH, D, D])
    if B_ or ALL:
        ffn_wg = dram_in("ffn_wg", [D, D_FF])
        ffn_wu = dram_in("ffn_wu", [D, D_FF])
        ffn_wd = dram_in("ffn_wd", [D_FF, D])
    if C_ or ALL:
        router_d = dram_in("router", [D, NEXP])
        moe_wg = dram_in("moe_wg", [NEXP, D, D_FFE])
        moe_wu = dram_in("moe_wu", [NEXP, D, D_FFE])
        moe_wd = dram_in("moe_wd", [NEXP, D_FFE, D])

    kvl, kvc, kvg, halo = {}, {}, {}, {}
    t_kvl, t_kvc, t_kvg = {}, {}, {}
    if A_:
        kvl[0] = dram_out("kvl0", [ROWS, TOK], BF16)
        kvc[0] = dram_out("kvc0", [ROWS, CTX], BF16)
    if B_:
        kvg[0] = dram_in("kvg0", [GROWS, TOK], BF16)
        kvc[0] = dram_in("kvc0", [ROWS, CTX], BF16)
        halo[0] = dram_in("halo0", [128, 4, 16], BF16)
        kvl[1] = dram_out("kvl1", [ROWS, TOK], BF16)
        kvc[1] = dram_out("kvc1", [ROWS, CTX], BF16)
    if ALL:
        for l_ in range(DEPTH):
            kvl[l_] = nc.dram_tensor(f"kvl{l_}", [ROWS, TOK], BF16, kind="Internal").ap()
            kvc[l_] = nc.dram_tensor(f"kvc{l_}", [ROWS, CTX], BF16, kind="Internal").ap()
            kvg[l_] = nc.dram_tensor(f"kvg{l_}", [GROWS, TOK], BF16, kind="Internal").ap()
    if C_:
        kvg[1] = dram_in("kvg1", [GROWS, TOK], BF16)
        kvc[1] = dram_in("kvc1", [ROWS, CTX], BF16)
        halo[1] = dram_in("halo1", [128, 4, 16], BF16)
    u_out = u_in = uc_out = uc_in = None
    if A_ or B_:
        u_out = dram_out("u_out", [128, 4 * (TOK + 16)], BF16)
    if A_:
        uc_out = dram_out("uc_out", [128, 4 * (CTX + 16)], BF16)
    if B_ or C_:
        u_in = dram_in("u_in", [128, 4 * (TOK + 16)], BF16)
    if B_:
        uc_in = dram_in("uc_in", [128, 4 * (CTX + 16)], BF16)
    for l in range(DEPTH):
        t_kvl[l] = Tk(f'kvl{l}')
        t_kvc[l] = Tk(f'kvc{l}')
        t_kvg[l] = [Tk(f'kvg{l}_{c}') for c in range(NCH + 1)]

    psb = [top.enter_context(nc.psum_tensor(f"psb{i}", [128, 512], F32)) for i in range(8)]
    t_ps = [Tk(f'ps{i}') for i in range(8)]

    x_sb = sb("x_sb", [128, KC, TOK], F32)
    xc_sb = sb("xc_sb", [128, KC, CTX], F32)
    t_x = [[Tk(f'x{kc}_{b}') for b in range(4)] for kc in range(KC)]
    t_xc = [Tk(f'xc{kc}') for kc in range(KC)]
    cols = sb("cols_sb", [128, NCOLS], F32)
    t_cols = Tk('cols')
    consts = sb("consts_sb", [128, 384], BF16)
    constf = sb("constf_sb", [128, 256], F32)
    t_consts = Tk('consts')
    ones_bf = consts[:, 0:128]
    ident_bf = consts[:, 128:256]
    perm_bf = consts[:, 256:384]
    ones_f = constf[:, 0:128]
    ident_f = constf[:, 128:256]
    mod_sb = sb("mod_sb", [128, DEPTH, 48, 2], F32)
    modA = sb("modA_sb", [128, DEPTH, 2, 8, 2], F32)
    t_mod = Tk('mod')
    misc = sb("misc_sb", [128, 16], F32)
    t_misc = Tk('misc')
    u_sb = sb("u_sb", [128, 4, TOK + 16], BF16)
    uc_sb = sb("uc_sb", [128, 4, CTX + 16], BF16)
    t_u = Tk('u')
    t_uc = Tk('uc')

    def colap(name, j=0, w=1):
        o, _ = COLS[name]
        return cols[:, o + j:o + j + w]

    sch.dma('sp', cols[:], cols_d[:], writes=[t_cols])
    sch.dma('sp', consts[:], consts_d[:], writes=[t_consts])
    sch.dma('sp', constf[:], constf_d[:], pwrites=[t_consts])
    for kc in range(KC):
        sch.dma('sp', x_sb[:, kc, :], xT[kc * 128:(kc + 1) * 128, :], writes=t_x[kc])
    if not C_:
        for kc in range(KC):
            sch.dma('sp', xc_sb[:, kc, :], ctxT[kc * 128:(kc + 1) * 128, :], writes=[t_xc[kc]])
    if u_in is not None:
        sch.dma('sp', u_sb[:].rearrange("p g t -> p (g t)"), u_in[:], writes=[t_u])
    if uc_in is not None:
        sch.dma('sp', uc_sb[:].rearrange("p g t -> p (g t)"), uc_in[:], writes=[t_uc])

    def mods_stage(pairs, with_misc):
        with ExitStack() as sc:
            lsb = mk_sb(sc)
            silu_c = lsb("silu_c", [128, KC, 2], BF16)
            t_silu = Tk('silu')
            o_c, _ = COLS['c']
            o_cc, _ = COLS['cctx']
            ACT(silu_c[:, :, 0], cols[:, o_c:o_c + 8], AF.Silu, [t_cols], [t_silu])
            ACT(silu_c[:, :, 1], cols[:, o_cc:o_cc + 8], AF.Silu, [t_cols], [], pw=[t_silu])
            wm = [lsb(f"wm{i}", [128, KC, 1024], BF16) for i in range(2)]
            t_wm = [Tk('wm0'), Tk('wm1')]
            t_psmod = t_ps[7]
            ps_mod = psb[7][:, 0:192]
            psv = ps_mod.rearrange("p (l m j) -> p l m j", l=DEPTH, j=2)
            for it, (l, part) in enumerate(pairs):
                wv = w_mod[l].rearrange("(kc p) n -> p kc n", p=128)
                buf = it % 2
                sch.dma('pool', wm[buf][:], wv[:, :, part * 1024:(part + 1) * 1024], writes=[t_wm[buf]])
                for mc in range(8):
                    c0 = (l * 48 + part * 8 + mc) * 2
                    for kc in range(KC):
                        MM(ps_mod[:, c0:c0 + 2], wm[buf][:, kc, mc * 128:(mc + 1) * 128], silu_c[:, kc, :],
                           kc == 0, kc == KC - 1, [t_wm[buf], t_silu], [t_psmod])
            for (l, part) in pairs:
                ob, _ = COLS[f'bmod{l}']
                for j in range(2):
                    TT(mod_sb[:, l, part * 8:(part + 1) * 8, j], psv[:, l, part * 8:(part + 1) * 8, j],
                       cols[:, ob + part * 8:ob + (part + 1) * 8], ALU.add, [t_psmod, t_cols], [], pw=[t_mod])
            for (l, part) in pairs:
                if part not in (1, 4):
                    continue
                which = 0 if part == 1 else 1
                gname = f'gmix{l}' if which == 0 else f'gffn{l}'
                og, _ = COLS[gname]
                for j in range(2):
                    STT(modA[:, l, which, :, j], mod_sb[:, l, part * 8:(part + 1) * 8, j], 1.0,
                        cols[:, og:og + 8], ALU.add, ALU.mult, [t_mod, t_cols], [], pw=[t_mod])
            if with_misc:
                lamt = lsb("lamt", [128, 4], F32)
                t_lamt = Tk('lamt')
                for l in range(DEPTH):
                    ol, _ = COLS[f'lam{l}']
                    lv = cols[:, ol:ol + 4].rearrange("p (a b) -> p a b", b=2)
                    TT(lamt[:, 0:2], lv[:, :, 0], lv[:, :, 1], ALU.mult, [t_cols], [t_lamt])
                    MM(psb[6][:, 0:2], ones_f, lamt[:, 0:2], True, True, [t_lamt, t_consts], [t_ps[6]])
                    ACT(lamt[:, 2:4], psb[6][:, 0:2], AF.Exp, [t_ps[6]], [], pw=[t_lamt])
                    STT(misc[:, 4 * l:4 * l + 1], lamt[:, 3:4], -LAM_INIT[l], lamt[:, 2:3], ALU.add, ALU.subtract,
                        [t_lamt], [], pw=[t_misc])
                    osub, _ = COLS[f'subln{l}']
                    TS(misc[:, 4 * l + 1:4 * l + 2], cols[:, osub:osub + 1], 1.0 - LAM_INIT[l], None, ALU.mult, None,
                       [t_cols], [], pw=[t_misc])
        barrier()

    MODS_FIRST = [(0, 0), (0, 1), (0, 2)]
    MODS_REST = [(0, 3), (0, 4), (0, 5)] + [(1, p_) for p_ in range(6)]
    mods_stage(MODS_FIRST, True)
    if not ALL:
        mods_stage(MODS_REST, False)

    def modcol(l, part, kc, j):
        return mod_sb[:, l, part * 8 + kc, j:j + 1]

    def norm_mod(R, xsrc, t_xs, ntok, l, which, j, h_out, t_h, f32cb=None, bank=7):
        for kc in range(KC):
            sq, t_sq = R['bf'].next()
            ACT(sq[:, :ntok], xsrc(kc), AF.Square, t_xs[kc], [t_sq])
            MM(psb[bank][:, :ntok], ones_bf, sq[:, :ntok], kc == 0, kc == KC - 1, [t_sq, t_consts], [t_ps[bank]])
        rstd, t_rstd = R['rstd'].next()
        RSTD(rstd[:, :ntok], psb[bank][:, :ntok], 1.0 / D, [t_ps[bank]], t_rstd)
        shpart = 0 if which == 0 else 3
        for kc in range(KC):
            tmp, t_tmp = R['f32'].next()
            TT(tmp[:, :ntok], xsrc(kc), rstd[:, :ntok], ALU.mult, list(t_xs[kc]) + [t_rstd], [t_tmp])
            if h_out is not None:
                ACT(h_out[:, kc, :ntok], tmp[:, :ntok], AF.Identity, [t_tmp, t_mod], [] if kc else [t_h],
                    scale=modA[:, l, which, kc, j:j + 1], bias=modcol(l, shpart, kc, j), pw=[t_h] if kc else [])
            if f32cb is not None:
                f32cb(kc, tmp, t_tmp)

    def rope(R, z_bf, t_z, ntok, tok0, out_ap, t_out, ropeC, ropeS, t_rope, bank, pw=False):
        MM(psb[bank][:, :ntok], perm_bf, z_bf, True, True, [t_z, t_consts], [t_ps[bank]])
        t1, t_t1 = R['f32'].next()
        TT(t1[:, :ntok], z_bf, ropeC[:, tok0:tok0 + ntok], ALU.mult, [t_z, t_rope], [t_t1])
        t2, t_t2 = R['f32'].next()
        TT(t2[:, :ntok], psb[bank][:, :ntok], ropeS[:, tok0:tok0 + ntok], ALU.mult, [t_ps[bank], t_rope], [t_t2])
        if isinstance(out_ap, tuple):
            TT(out_ap[0], t1[0:64, :ntok], t2[0:64, :ntok], ALU.add, [t_t1, t_t2], [], pw=[t_out])
            TT(out_ap[1], t1[64:128, :ntok], t2[64:128, :ntok], ALU.add, [t_t1, t_t2], [], pw=[t_out])
            return
        TT(out_ap, t1[:, :ntok], t2[:, :ntok], ALU.add, [t_t1, t_t2], [] if pw else [t_out],
           pw=[t_out] if pw else [])

    def wload(dst, src, tk, first=True):
        sch.dma('pool', dst, src, writes=[tk] if first else [], pwrites=[] if first else [tk])

    def winv(l):
        return w_in[l].rearrange("(kc p) n -> p kc n", p=128)

    def phase1(l, with_ctx_u):
        with ExitStack() as sc:
            lsb = mk_sb(sc)
            R = {'bf': Ring(lsb, "p1bf", 4, [128, 512], BF16), 'f32': Ring(lsb, "p1f", 4, [128, 512], F32),
                 'rstd': Ring(lsb, "p1r", 2, [128, 512], F32)}
            wp = lsb("wP1", [128, KC, 1920], BF16)
            t_wp = Tk('wP1')
            wkv = lsb("wukv", [128, 2, 1024], BF16)
            t_wkv = Tk('wukv')
            ropeC = lsb("ropeC", [128, TOK], BF16)
            ropeS = lsb("ropeS", [128, TOK], BF16)
            t_rope = Tk('rope')
            sch.dma('sp', ropeC[:], ropeC_d[:], writes=[t_rope])
            sch.dma('sp', ropeS[:], ropeS_d[:], pwrites=[t_rope])
            wv = winv(l)
            first = True
            for (d0, s0, n) in ((0, C_DAK, 512), (512, C_DAV, 512), (1024, C_KVD, 256), (1280, C_KR, 64),
                                (1344, C_KR, 64), (1408, C_POOL, 512)):
                wload(wp[:, :, d0:d0 + n], wv[:, :, s0:s0 + n], t_wp, first)
                first = False
            wload(wkv[:], w_ukv[l].rearrange("(kc p) n -> p kc n", p=128), t_wkv)
            hring = [(lsb(f"p1h{i}", [128, KC, 512], BF16), Tk(f'p1h{i}')) for i in range(2)]
            kst = lsb("kst", [128, 9, 512], BF16)
            t_kst = Tk('kst')
            vst = lsb("vst", [128, 4, 1024], BF16)
            t_vst = Tk('vst')
            kvn = lsb("kvn", [128, 2, 512], BF16)
            t_kvn = Tk('kvn')
            kvf = lsb("kvf", [128, 2, 512], F32)
            t_kvf = Tk('kvf')
            hst = lsb("hst", [128, 4, 16], BF16)
            t_hst = Tk('hst')
            og, _ = COLS[f'gkv{l}']
            bankrr = [0]

            def nb():
                b = bankrr[0] % 6
                bankrr[0] += 1
                return b

            blocks = [('l', b) for b in range(4)] + [('c', 0)]
            for bi, (kind, b) in enumerate(blocks):
                isc = kind == 'c'
                ntok = CTX if isc else 512
                tok0 = 0 if isc else b * 512
                j = 1 if isc else 0
                if isc:
                    xsrc = lambda kc: xc_sb[:, kc, :]
                    t_xs = [[t_xc[kc]] for kc in range(KC)]
                else:
                    xsrc = lambda kc, tok0=tok0: x_sb[:, kc, tok0:tok0 + 512]
                    t_xs = [[t_x[kc][b]] for kc in range(KC)]
                h, t_h = hring[bi % 2]
                norm_mod(R, xsrc, t_xs, ntok, l, 0, j, h, t_h, bank=7)
                for ci in range(5):
                    c0 = ci * 128 if ci < 4 else 1280
                    bk = nb()
                    for kc in range(KC):
                        MM(psb[bk][:, :ntok], wp[:, kc, c0:c0 + 128], h[:, kc, :ntok], kc == 0, kc == KC - 1,
                           [t_wp, t_h], [t_ps[bk]])
                    dst = kst[:, ci if ci < 4 else 8, :ntok]
                    if isc:
                        CP(dst, psb[bk][:, :ntok], [t_ps[bk]], [], pw=[t_kst])
                    else:
                        z, t_z = R['bf'].next()
                        CP(z[:, :ntok], psb[bk][:, :ntok], [t_ps[bk]], [t_z], eng='act')
                        rope(R, z[:, :ntok], t_z, ntok, tok0, dst, t_kst, ropeC, ropeS, t_rope, nb(), pw=True)
                bks = []
                for ci in range(2):
                    bk = nb()
                    bks.append(bk)
                    for kc in range(KC):
                        MM(psb[bk][:, :ntok], wp[:, kc, 1024 + ci * 128:1024 + (ci + 1) * 128], h[:, kc, :ntok],
                           kc == 0, kc == KC - 1, [t_wp, t_h], [t_ps[bk]])
                    CP(kvf[:, ci, :ntok], psb[bk][:, :ntok], [t_ps[bk]], [], pw=[t_kvf])
                bk = nb()
                for ci in range(2):
                    sq, t_sq = R['bf'].next()
                    ACT(sq[:, :ntok], kvf[:, ci, :ntok], AF.Square, [t_kvf], [t_sq])
                    MM(psb[bk][:, :ntok], ones_bf, sq[:, :ntok], ci == 0, ci == 1, [t_sq, t_consts], [t_ps[bk]])
                rstd, t_rstd = R['rstd'].next()
                RSTD(rstd[:, :ntok], psb[bk][:, :ntok], 1.0 / 256, [t_ps[bk]], t_rstd)
                for ci in range(2):
                    STT(kvn[:, ci, :ntok], kvf[:, ci, :ntok], cols[:, og + ci:og + ci + 1], rstd[:, :ntok],
                        ALU.mult, ALU.mult, [t_kvf, t_rstd, t_cols], [], pw=[t_kvn])
                for hh in range(4):
                    bk = nb()
                    for kc in range(2):
                        MM(psb[bk][:, :ntok], wkv[:, kc, hh * 128:(hh + 1) * 128], kvn[:, kc, :ntok],
                           kc == 0, kc == 1, [t_wkv, t_kvn], [t_ps[bk]])
                    CP(kst[:, 4 + hh, :ntok], psb[bk][:, :ntok], [t_ps[bk]], [], pw=[t_kst])
                for ti in range(ntok // 128):
                    bk = nb()
                    for kc in range(KC):
                        MM(psb[bk][:, :], h[:, kc, ti * 128:(ti + 1) * 128], wp[:, kc, 512:1024],
                           kc == 0, kc == KC - 1, [t_wp, t_h], [t_ps[bk]])
                    CP(vst[:, ti, 0:512], psb[bk][:, :], [t_ps[bk]], [], eng='act', pw=[t_vst])
                    bk = nb()
                    for kc in range(2):
                        MM(psb[bk][:, :], kvn[:, kc, ti * 128:(ti + 1) * 128], wkv[:, kc, 512:1024],
                           kc == 0, kc == 1, [t_wkv, t_kvn], [t_ps[bk]])
                    CP(vst[:, ti, 512:1024], psb[bk][:, :], [t_ps[bk]], [], pw=[t_vst])
                if (not isc) or with_ctx_u:
                    ud = uc_sb if isc else u_sb
                    tu = t_uc if isc else t_u
                    for gi in range(4):
                        bk = nb()
                        for kc in range(KC):
                            MM(psb[bk][:, :ntok], wp[:, kc, 1408 + gi * 128:1408 + (gi + 1) * 128], h[:, kc, :ntok],
                               kc == 0, kc == KC - 1, [t_wp, t_h], [t_ps[bk]])
                        CP(ud[:, gi, 8 + tok0:8 + tok0 + ntok], psb[bk][:, :ntok], [t_ps[bk]], [], eng='act', pw=[tu])
                dk = kvc[l] if isc else kvl[l]
                tdk = t_kvc[l] if isc else t_kvl[l]
                sch.dma('sp', dk[R_KDA:R_KDA + 512, tok0:tok0 + ntok].rearrange("(c p) t -> p c t", p=128),
                        kst[:, 0:4, :ntok], reads=[t_kst], pwrites=[tdk])
                sch.dma('sp', dk[R_KN:R_KN + 512, tok0:tok0 + ntok].rearrange("(c p) t -> p c t", p=128),
                        kst[:, 4:8, :ntok], reads=[t_kst], pwrites=[tdk])
                sch.dma('sp', dk[R_KR:R_KR + 128, tok0:tok0 + ntok], kst[:, 8, :ntok], reads=[t_kst], pwrites=[tdk])
                for (r0, c0) in ((R_VDA, 0), (R_VM, 512)):
                    if isc:
                        vview = dk[r0:r0 + 512, :].rearrange("(t a) c -> t (a c)", a=2)
                        sch.dma('sp', vview[tok0:tok0 + ntok, :].rearrange("(i p) f -> p i f", p=128),
                                vst[:, 0:ntok // 128, c0:c0 + 512], reads=[t_vst], pwrites=[tdk])
                    else:
                        grp, i0 = b // 2, (b % 2) * 4
                        for hh in range(4):
                            rb = r0 + (hh * 2 + grp) * 64
                            blk = dk[rb:rb + 64, :].rearrange("r (a q) -> (r a) q", a=2).rearrange(
                                "p (i f) -> p i f", f=128)
                            sch.dma('sp', blk[:, i0:i0 + 4, :], vst[:, 0:4, c0 + hh * 128:c0 + (hh + 1) * 128],
                                    reads=[t_vst], pwrites=[tdk])
            if 'h' in debug:
                dh = dram_out("dbg_h", [128, KC * 512], BF16)
                sch.dma('sp', dh[:], hring[1][0][:].rearrange("p k t -> p (k t)"), reads=[hring[1][1]])
            if u_out is not None:
                sch.dma('sp', u_out[:], u_sb[:].rearrange("p g t -> p (g t)"), reads=[t_u])
            if uc_out is not None and with_ctx_u:
                sch.dma('sp', uc_out[:], uc_sb[:].rearrange("p g t -> p (g t)"), reads=[t_uc])
            if l in kvl:
                CP(hst[:, :, 0:8], u_sb[:, :, 8:16], [t_u], [t_hst], eng='pool')
                CP(hst[:, :, 8:16], u_sb[:, :, TOK:TOK + 8], [t_u], [], eng='pool', pw=[t_hst])
                sch.dma('sp', kvl[l][R_U:R_U + 4, :].rearrange("g (p t) -> p g t", t=16), hst[:],
                        reads=[t_hst], pwrites=[t_kvl[l]])
        barrier()

    def phase2(l, do_ctx):
        with ExitStack() as sc:
            lsb = mk_sb(sc)
            R = {"bf": Ring(lsb, "p2bf", 3, [128, 512], BF16), "f32": Ring(lsb, "p2f", 3, [128, 512], F32),
                 'rstd': Ring(lsb, "p2r", 1, [128, 512], F32)}
            wsl = Ring(lsb, "wsl", 3, [128, 2048], BF16)
            h = lsb("p2h", [128, KC, 512], BF16)
            t_h = Tk('p2h')
            ropeC = lsb("ropeCq", [128, 512], BF16)
            ropeS = lsb("ropeSq", [128, 512], BF16)
            t_rope = Tk('ropeq')
            qdaA = lsb("qdaA", [128, 4, 512], BF16)
            qdaB = lsb("qdaB", [128, 4, 512], BF16)
            qmr = lsb("qmr", [128, 4, 512], BF16)
            t_qda = Tk('qda')
            sch.op('dve', lambda e: e.memset(qdaA[64:128, :, :], 0.0), pwrites=[t_qda])
            sch.op('dve', lambda e: e.memset(qdaB[0:64, :, :], 0.0), pwrites=[t_qda])
            qm = lsb("qm", [128, 4, 512], BF16)
            t_qm = Tk('qm')
            for hh_ in range(4):
                if hh_ % 2 == 0:
                    sch.op('dve', lambda e, hh_=hh_: e.memset(qmr[64:128, hh_, :], 0.0), pwrites=[t_qm])
                else:
                    sch.op('dve', lambda e, hh_=hh_: e.memset(qmr[0:64, hh_, :], 0.0), pwrites=[t_qm])
            qn = lsb("qn", [128, 3, 512], BF16)
            t_qn = Tk('qn')
            o_da = lsb("o_da", [128, 4, 512], BF16)
            o_mla = lsb("o_mla", [128, 4, 512], BF16)
            o_pool = lsb("o_pool", [128, 4, 512], BF16)
            t_oda, t_omla, t_opool = Tk('oda'), Tk('omla'), Tk('opool')
            da_a = lsb("da_a", [128, 4, 512], BF16)
            t_daa = Tk('daa')
            merged = lsb("merged", [128, KC, 512], BF16)
            t_merged = Tk('merged')
            pring = Ring(lsb, "pT", 4, [128, 512], BF16)
            fA, fB, fC = (lsb(n_, [128, 512], F32) for n_ in ("fA", "fB", "fC"))
            t_fA, t_fB, t_fC = Tk('fA'), Tk('fB'), Tk('fC')
            slots = []
            for i in range(3):
                slots.append(dict(K1=lsb(f"kK1_{i}", [128, 1024], BF16), tK1=Tk(f'kK1_{i}'),
                                  K2=lsb(f"kK2_{i}", [128, 1024], BF16), tK2=Tk(f'kK2_{i}'),
                                  V=lsb(f"kV_{i}", [128, 8, 128], BF16), tV=Tk(f'kV_{i}')))
            pa = lsb("pa", [128, 528], F32)
            pb_ = lsb("pb", [128, 528], F32)
            t_pa, t_pb = Tk('pa'), Tk('pb')
            dbf = lsb("dbf", [128, 512], BF16)
            t_dbf = Tk('dbf')
            wv = winv(l)
            ogq, _ = COLS[f'gq{l}']
            ops_, _ = COLS[f'pscale{l}']
            neglam = misc[:, 4 * l:4 * l + 1]
            sublnS = misc[:, 4 * l + 1:4 * l + 2]

            if ALL:
                hall = lsb("hall", [128, 4, 4, 16], BF16)
                t_hall = Tk('hall')
                hb = NCH * 512
                for r in range(4):
                    sch.dma('sp', hall[:, r, :, :], kvg[l][hb + r * 8:hb + r * 8 + 4, :].rearrange("g (p t) -> p g t", t=16)[:, :, 0:16],
                            reads=[t_kvg[l][NCH]], writes=[t_hall] if r == 0 else [], pwrites=[] if r == 0 else [t_hall])
                oL, _ = COLS['selL']
                oR, _ = COLS['selR']
                for r in range(4):
                    if r == 0:
                        TS(u_sb[:, :, 0:8], hall[:, r, :, 8:16], cols[:, oL + r:oL + r + 1], None, ALU.mult, None,
                           [t_hall, t_cols], [], pw=[t_u])
                        TS(u_sb[:, :, TOK + 8:TOK + 16], hall[:, r, :, 0:8], cols[:, oR + r:oR + r + 1], None, ALU.mult, None,
                           [t_hall, t_cols], [], pw=[t_u])
                    else:
                        STT(u_sb[:, :, 0:8], hall[:, r, :, 8:16], cols[:, oL + r:oL + r + 1], u_sb[:, :, 0:8],
                            ALU.mult, ALU.add, [t_hall, t_cols, t_u], [], pw=[t_u])
                        STT(u_sb[:, :, TOK + 8:TOK + 16], hall[:, r, :, 0:8], cols[:, oR + r:oR + r + 1],
                            u_sb[:, :, TOK + 8:TOK + 16], ALU.mult, ALU.add, [t_hall, t_cols, t_u], [], pw=[t_u])
            if l in halo:
                sch.dma('sp', u_sb[:, :, 0:8], halo[l][:, :, 0:8], pwrites=[t_u])
                sch.dma('sp', u_sb[:, :, TOK + 8:TOK + 16], halo[l][:, :, 8:16], pwrites=[t_u])
            if do_ctx:
                sch.op('dve', lambda e: e.memset(uc_sb[:, :, 0:8], 0.0), pwrites=[t_uc])
                sch.op('dve', lambda e: e.memset(uc_sb[:, :, CTX + 8:CTX + 16], 0.0), pwrites=[t_uc])

            def load_group(kind, hh, g, slot):
                if kind == 'da':
                    rk, rv = R_KDA, R_VDA
                else:
                    rk, rv = R_KN, R_VM
                if g[0] == 'c':
                    src, tsrc = kvc[l], [t_kvc[l]]
                    vv = src[rv:rv + 512, :].rearrange("(t a) c -> t (a c)", a=2)
                    sch.dma('sp', slot['K1'][:, :CTX], src[rk + hh * 128:rk + (hh + 1) * 128, :],
                            reads=tsrc, writes=[slot['tK1']])
                    if kind == 'mla':
                        sch.dma('sp', slot['K2'][:, :CTX], src[R_KR:R_KR + 128, :], reads=tsrc, writes=[slot['tK2']])
                    sch.dma('sp', slot['V'][:, :CTX // 128, :],
                            vv[:, hh * 128:(hh + 1) * 128].rearrange("(i p) f -> p i f", p=128),
                            reads=tsrc, writes=[slot['tV']])
                    return
                _, r, hf = g
                src = kvg[l]
                col0 = hf * 1024

                def rows(row0):
                    c = row0 // 128
                    return src[c * 512 + r * 128:c * 512 + (r + 1) * 128, :], t_kvg[l][c]
                ap, tk = rows(rk + hh * 128)
                sch.dma('sp', slot['K1'][:, :1024], ap[:, col0:col0 + 1024], reads=[tk], writes=[slot['tK1']])
                if kind == 'mla':
                    ap, tk = rows(R_KR)
                    sch.dma('sp', slot['K2'][:, :1024], ap[:, col0:col0 + 1024], reads=[tk], writes=[slot['tK2']])
                rb = rv + (hh * 2 + hf) * 64
                c = rb // 128
                o_ = c * 512 + r * 128 + (rb % 128)
                blk = src[o_:o_ + 64, :].rearrange("r (a q) -> (r a) q", a=2).rearrange("p (i f) -> p i f", f=128)
                sch.dma('sp', slot['V'][:, :, :], blk, reads=[t_kvg[l][c]], writes=[slot['tV']])

            def attention(nq, groups):
                loads = [(kind, hh, g) for kind in ('da', 'mla') for hh in range(4) for g in groups]
                issued = [0]

                def ensure(n):
                    while issued[0] < min(n, len(loads)):
                        k_, h_, g_ = loads[issued[0]]
                        load_group(k_, h_, g_, slots[issued[0] % 3])
                        issued[0] += 1
                NS = len(slots)
                ensure(NS)
                idx = 0
                for kind in ('da', 'mla'):
                    for hh in range(4):
                        tiles = []
                        for g in groups:
                            nt = 2 if g[0] == 'c' else 8
                            tiles += [(idx, tt) for tt in range(nt)]
                            idx += 1
                        n = len(tiles)
                        if kind == 'da':
                            def S1(i):
                                gi, tt = tiles[i]
                                sl = slots[gi % 3]
                                bk = 0 if i % 2 == 0 else 6
                                MM(psb[bk][:, :nq], sl['K1'][:, tt * 128:(tt + 1) * 128], qdaA[:, hh, :nq],
                                   True, True, [sl['tK1'], t_qda], [t_ps[bk]])

                            def S2(i):
                                gi, tt = tiles[i]
                                sl = slots[gi % 3]
                                bk = 1 if i % 2 == 0 else 7
                                MM(psb[bk][:, :nq], sl['K1'][:, tt * 128:(tt + 1) * 128], qdaB[:, hh, :nq],
                                   True, True, [sl['tK1'], t_qda], [t_ps[bk]])
                            S1(0)
                            S2(0)
                            for i in range(n):
                                gi, tt = tiles[i]
                                sl = slots[gi % 3]
                                p1, t_p1 = pring.next()
                                p2, t_p2 = pring.next()
                                b1, b2 = (0, 1) if i % 2 == 0 else (6, 7)
                                ACT(p1[:, :nq], psb[b1][:, :nq], AF.Exp, [t_ps[b1]], [t_p1], scale=0.125)
                                ACT(p2[:, :nq], psb[b2][:, :nq], AF.Exp, [t_ps[b2]], [t_p2], scale=0.125)
                                if i + 1 < n:
                                    S1(i + 1)
                                    S2(i + 1)
                                MM(psb[2][:, :nq], sl['V'][:, tt, :], p1[:, :nq], i == 0, i == n - 1,
                                   [sl['tV'], t_p1], [t_ps[2]])
                                MM(psb[3][:, :nq], sl['V'][:, tt, :], p2[:, :nq], i == 0, i == n - 1,
                                   [sl['tV'], t_p2], [t_ps[3]])
                                if i == 0:
                                    CP(pa[:, :nq], p1[:, :nq], [t_p1], [t_pa])
                                    CP(pb_[:, :nq], p2[:, :nq], [t_p2], [t_pb])
                                else:
                                    TT(pa[:, :nq], pa[:, :nq], p1[:, :nq], ALU.add, [t_pa, t_p1], [], pw=[t_pa])
                                    TT(pb_[:, :nq], pb_[:, :nq], p2[:, :nq], ALU.add, [t_pb, t_p2], [], pw=[t_pb])
                                if i == n - 1 or tiles[i + 1][0] != gi:
                                    ensure(gi + NS + 1)
                            MM(psb[4][:, :nq], ones_f, pa[:, :nq], True, True, [t_pa, t_consts], [t_ps[4]])
                            MM(psb[5][:, :nq], ones_f, pb_[:, :nq], True, True, [t_pb, t_consts], [t_ps[5]])
                            RECIP(fA[:, :nq], psb[4][:, :nq], [t_ps[4]], [t_fA])
                            TT(fB[:, :nq], psb[2][:, :nq], fA[:, :nq], ALU.mult, [t_ps[2], t_fA], [t_fB])
                            RECIP(fA[:, :nq], psb[5][:, :nq], [t_ps[5]], [t_fA])
                            TT(fC[:, :nq], psb[3][:, :nq], fA[:, :nq], ALU.mult, [t_ps[3], t_fA], [t_fC])
                            STT(da_a[:, hh, :nq], fC[:, :nq], neglam, fB[:, :nq], ALU.mult, ALU.add,
                                [t_fC, t_fB, t_misc], [], pw=[t_daa])
                        else:
                            pbase = 64 * (hh % 2)

                            def SM(i):
                                gi, tt = tiles[i]
                                sl = slots[gi % 3]
                                bk = i % 2
                                MM(psb[bk][:, :nq], sl['K1'][:, tt * 128:(tt + 1) * 128], qm[:, hh, :nq],
                                   True, False, [sl['tK1'], t_qm], [t_ps[bk]])
                                MM(psb[bk][:, :nq], sl['K2'][:, tt * 128:(tt + 1) * 128],
                                   qmr[:, hh, :nq], False, True, [sl['tK2'], t_qm], [t_ps[bk]])
                            SM(0)
                            for i in range(n):
                                gi, tt = tiles[i]
                                sl = slots[gi % 3]
                                if i + 1 < n:
                                    SM(i + 1)
                                p1, t_p1 = pring.next()
                                ACT(p1[:, :nq], psb[i % 2][:, :nq], AF.Exp, [t_ps[i % 2]], [t_p1], scale=192.0 ** -0.5)
                                MM(psb[2][:, :nq], sl['V'][:, tt, :], p1[:, :nq], i == 0, i == n - 1,
                                   [sl['tV'], t_p1], [t_ps[2]])
                                if i == 0:
                                    CP(pa[:, :nq], p1[:, :nq], [t_p1], [t_pa])
                                else:
                                    TT(pa[:, :nq], pa[:, :nq], p1[:, :nq], ALU.add, [t_pa, t_p1], [], pw=[t_pa])
                                if i == n - 1 or tiles[i + 1][0] != gi:
                                    ensure(gi + NS + 1)
                            MM(psb[4][:, :nq], ones_f, pa[:, :nq], True, True, [t_pa, t_consts], [t_ps[4]])
                            RECIP(fA[:, :nq], psb[4][:, :nq], [t_ps[4]], [t_fA])
                            TT(o_mla[:, hh, :nq], psb[2][:, :nq], fA[:, :nq], ALU.mult, [t_ps[2], t_fA], [],
                               pw=[t_omla])
                for hh in range(4):
                    sq, t_sq = R['bf'].next()
                    TT(sq[:, :nq], da_a[:, hh, :nq], da_a[:, hh, :nq], ALU.mult, [t_daa], [t_sq])
                    MM(psb[6][:, :nq], ones_bf, sq[:, :nq], True, True, [t_sq, t_consts], [t_ps[6]])
                    rstd, t_rstd = R['rstd'].next()
                    RSTD(rstd[:, :nq], psb[6][:, :nq], 1.0 / 128, [t_ps[6]], t_rstd)
                    STT(o_da[:, hh, :nq], da_a[:, hh, :nq], sublnS, rstd[:, :nq], ALU.mult, ALU.mult,
                        [t_daa, t_rstd, t_misc], [], pw=[t_oda])

            blocks = ([('c', 0)] if do_ctx else []) + [('l', b) for b in range(4)]
            bankrr = [0]

            def nb():
                b = bankrr[0] % 6
                bankrr[0] += 1
                return b

            for (kind, b) in blocks:
                isc = kind == 'c'
                nq = CTX if isc else 512
                tok0 = 0 if isc else b * 512
                j = 1 if isc else 0
                if isc:
                    xsrc = lambda kc: xc_sb[:, kc, :]
                    xdst = lambda kc: xc_sb[:, kc, :]
                    t_xs = [[t_xc[kc]] for kc in range(KC)]
                else:
                    xsrc = lambda kc, tok0=tok0: x_sb[:, kc, tok0:tok0 + 512]
                    xdst = xsrc
                    t_xs = [[t_x[kc][b]] for kc in range(KC)]
                norm_mod(R, xsrc, t_xs, nq, l, 0, j, h, t_h, bank=7)
                if not isc:
                    sch.dma('sp', ropeC[:], ropeC_d[:, tok0:tok0 + 512], writes=[t_rope])
                    sch.dma('sp', ropeS[:], ropeS_d[:, tok0:tok0 + 512], pwrites=[t_rope])
                for half in range(2):
                    ws, t_ws = wsl.next()
                    wq = ws[:, :].rearrange("p (k n) -> p k n", k=KC)
                    wload(wq, wv[:, :, C_DAQ + half * 256:C_DAQ + (half + 1) * 256], t_ws)
                    for dd in range(2):
                        hh = half * 2 + dd
                        bk = nb()
                        for kc in range(KC):
                            MM(psb[bk][:, :nq], wq[:, kc, dd * 128:(dd + 1) * 128], h[:, kc, :nq], kc == 0, kc == KC - 1,
                               [t_ws, t_h], [t_ps[bk]])
                        if isc:
                            CP(qdaA[0:64, hh, :nq], psb[bk][0:64, :nq], [t_ps[bk]], [], pw=[t_qda])
                            CP(qdaB[64:128, hh, :nq], psb[bk][64:128, :nq], [t_ps[bk]], [], pw=[t_qda])
                        else:
                            z, t_z = R['bf'].next()
                            CP(z[:, :nq], psb[bk][:, :nq], [t_ps[bk]], [t_z], eng='act')
                            rope(R, z[:, :nq], t_z, nq, 0, (qdaA[0:64, hh, :nq], qdaB[64:128, hh, :nq]), t_qda,
                                 ropeC, ropeS, t_rope, nb(), pw=True)
                qbanks = []
                for (c0, ncol) in ((0, 256), (256, 128)):
                    ws, t_ws = wsl.next()
                    wq = ws[:, 0:KC * ncol].rearrange("p (k n) -> p k n", k=KC)
                    wload(wq, wv[:, :, C_QD + c0:C_QD + c0 + ncol], t_ws)
                    for dd in range(ncol // 128):
                        bk = nb()
                        qbanks.append(bk)
                        for kc in range(KC):
                            MM(psb[bk][:, :nq], wq[:, kc, dd * 128:(dd + 1) * 128], h[:, kc, :nq], kc == 0, kc == KC - 1,
                               [t_ws, t_h], [t_ps[bk]])
                for ci, bk in enumerate(qbanks):
                    sq, t_sq = R['bf'].next()
                    ACT(sq[:, :nq], psb[bk][:, :nq], AF.Square, [t_ps[bk]], [t_sq])
                    MM(psb[6][:, :nq], ones_bf, sq[:, :nq], ci == 0, ci == 2, [t_sq, t_consts], [t_ps[6]])
                rstd, t_rstd = R['rstd'].next()
                RSTD(rstd[:, :nq], psb[6][:, :nq], 1.0 / 384, [t_ps[6]], t_rstd)
                for ci, bk in enumerate(qbanks):
                    STT(qn[:, ci, :nq], psb[bk][:, :nq], cols[:, ogq + ci:ogq + ci + 1], rstd[:, :nq],
                        ALU.mult, ALU.mult, [t_ps[bk], t_rstd, t_cols], [], pw=[t_qn])
                wuv = w_uq[l].rearrange("(kc p) n -> p kc n", p=128)
                ws, t_ws = wsl.next()
                wun = ws[:, 0:3 * 512].rearrange("p (k n) -> p k n", k=3)
                wload(wun, wuv[:, :, 0:512], t_ws)
                ws2, t_ws2 = wsl.next()
                wur = ws2[:, 0:3 * 256].rearrange("p (k n) -> p k n", k=3)
                wload(wur, wuv[:, :, 512:768], t_ws2)
                for hh in range(4):
                    bk = nb()
                    for kc in range(3):
                        MM(psb[bk][:, :nq], wun[:, kc, hh * 128:(hh + 1) * 128], qn[:, kc, :nq], kc == 0, kc == 2,
                           [t_ws, t_qn], [t_ps[bk]])
                    CP(qm[:, hh, :nq], psb[bk][:, :nq], [t_ps[bk]], [], pw=[t_qm])
                for rc in range(2):
                    bk = nb()
                    for kc in range(3):
                        MM(psb[bk][:, :nq], wur[:, kc, rc * 128:(rc + 1) * 128], qn[:, kc, :nq], kc == 0, kc == 2,
                           [t_ws2, t_qn], [t_ps[bk]])
                    if isc:
                        CP(qmr[0:64, 2 * rc, :nq], psb[bk][0:64, :nq], [t_ps[bk]], [], pw=[t_qm])
                        CP(qmr[64:128, 2 * rc + 1, :nq], psb[bk][64:128, :nq], [t_ps[bk]], [], pw=[t_qm])
                    else:
                        z, t_z = R['bf'].next()
                        CP(z[:, :nq], psb[bk][:, :nq], [t_ps[bk]], [t_z], eng='act')
                        rope(R, z[:, :nq], t_z, nq, 0, (qmr[0:64, 2 * rc, :nq], qmr[64:128, 2 * rc + 1, :nq]), t_qm,
                             ropeC, ropeS, t_rope, nb(), pw=True)
                groups = [('c',)] + ([] if isc else [('l', r, hf) for r in range(4) for hf in range(2)])
                attention(nq, groups)
                usrc = uc_sb if isc else u_sb
                tus = t_uc if isc else t_u
                E = nq + 16
                ws, t_ws = wsl.next()
                wpl = ws[:, 0:512].rearrange("p (g d) -> p g d", g=4)
                wload(wpl, pool_w[l].rearrange("g c d -> c g d"), t_ws)
                fixname = 'pfixc' if isc else 'pfix'
                ofx, _ = COLS[fixname]
                for gi, w in enumerate(POOL_WINDOWS):
                    ue = usrc[:, gi, tok0:tok0 + E]
                    TT(pa[:, 1:E], ue[:, 0:E - 1], ue[:, 1:E], ALU.add, [tus], [t_pa])
                    cur, tcur = pa, t_pa
                    if w >= 4:
                        TT(pb_[:, 2:E - 1], pa[:, 1:E - 2], pa[:, 3:E], ALU.add, [t_pa], [t_pb])
                        cur, tcur = pb_, t_pb
                    if w >= 8:
                        TT(pa[:, 4:E - 3], pb_[:, 2:E - 5], pb_[:, 6:E - 1], ALU.add, [t_pb], [t_pa])
                        cur, tcur = pa, t_pa
                    if w >= 16:
                        TT(pb_[:, 8:E - 7], pa[:, 4:E - 11], pa[:, 12:E - 3], ALU.add, [t_pa], [t_pb])
                        cur, tcur = pb_, t_pb
                    if isc or b == 0:
                        TT(cur[:, 8:16], cur[:, 8:16], cols[:, ofx + gi * 16:ofx + gi * 16 + 8], ALU.mult,
                           [tcur, t_cols], [], pw=[tcur])
                    if isc or b == 3:
                        TT(cur[:, nq:nq + 8], cur[:, nq:nq + 8], cols[:, ofx + gi * 16 + 8:ofx + gi * 16 + 16], ALU.mult,
                           [tcur, t_cols], [], pw=[tcur])
                    STT(dbf[:, :nq], cur[:, 8:8 + nq], 1.0 / w, ue[:, 8:8 + nq], ALU.mult, ALU.subtract,
                        [tcur, tus], [t_dbf])
                    bk = nb()
                    MM(psb[bk][:, :nq], wpl[:, gi, :], dbf[:, :nq], True, True, [t_ws, t_dbf], [t_ps[bk]])
                    TS(o_pool[:, gi, :nq], psb[bk][:, :nq], cols[:, ops_ + gi:ops_ + gi + 1], None, ALU.mult, None,
                       [t_ps[bk], t_cols], [], pw=[t_opool])
                wbv = w_branch[l].rearrange("n (cc p) d -> p n cc d", p=128)
                for n_, (o_n, t_on) in enumerate(((o_da, t_oda), (o_mla, t_omla), (o_pool, t_opool))):
                    for half in range(2):
                        ws, t_wb = wsl.next()
                        wb = ws[:, :].rearrange("p (c d) -> p c d", c=4)
                        wload(wb, wbv[:, n_, :, half * 512:(half + 1) * 512], t_wb)
                        for quarter in range(2):
                            dpair = half * 2 + quarter
                            ws2, t_wg = wsl.next()
                            wg = ws2[:, :].rearrange("p (k n) -> p k n", k=KC)
                            wload(wg, wv[:, :, C_GATE + n_ * 1024 + dpair * 256:C_GATE + n_ * 1024 + (dpair + 1) * 256], t_wg)
                            for dd in range(2):
                                dch = dpair * 2 + dd
                                bg = nb()
                                for kc in range(KC):
                                    MM(psb[bg][:, :nq], wg[:, kc, dd * 128:(dd + 1) * 128], h[:, kc, :nq],
                                       kc == 0, kc == KC - 1, [t_wg, t_h], [t_ps[bg]])
                                sig, t_sig = R['bf'].next()
                                ACT(sig[:, :nq], psb[bg][:, :nq], AF.Sigmoid, [t_ps[bg]], [t_sig])
                                bp = nb()
                                for cc in range(4):
                                    MM(psb[bp][:, :nq], wb[:, cc, (dch % 4) * 128:(dch % 4 + 1) * 128], o_n[:, cc, :nq],
                                       cc == 0, cc == 3, [t_wb, t_on], [t_ps[bp]])
                                if n_ == 0:
                                    TT(merged[:, dch, :nq], sig[:, :nq], psb[bp][:, :nq], ALU.mult, [t_sig, t_ps[bp]], [],
                                       pw=[t_merged])
                                else:
                                    tmp, t_tmp = R['f32'].next()
                                    TT(tmp[:, :nq], sig[:, :nq], psb[bp][:, :nq], ALU.mult, [t_sig, t_ps[bp]], [t_tmp])
                                    TT(merged[:, dch, :nq], merged[:, dch, :nq], tmp[:, :nq], ALU.add, [t_merged, t_tmp], [],
                                       pw=[t_merged])
                wov = w_out[l].rearrange("(kc p) n -> p kc n", p=128)
                for q4 in range(4):
                    ws, t_wo = wsl.next()
                    wo = ws[:, :].rearrange("p (k n) -> p k n", k=KC)
                    wload(wo, wov[:, :, q4 * 256:(q4 + 1) * 256], t_wo)
                    for dd in range(2):
                        dco = q4 * 2 + dd
                        bk = nb()
                        for kc in range(KC):
                            MM(psb[bk][:, :nq], wo[:, kc, dd * 128:(dd + 1) * 128], merged[:, kc, :nq],
                               kc == 0, kc == KC - 1, [t_wo, t_merged], [t_ps[bk]])
                        STT(xdst(dco), psb[bk][:, :nq], modcol(l, 2, dco, j), xsrc(dco), ALU.mult, ALU.add,
                            [t_ps[bk], t_mod] + t_xs[dco], [], pw=t_xs[dco])
                if 'mix' in debug and (not isc) and b == 0:
                    dmg = dram_out("dbg_merged", [128, KC * 512], BF16)
                    sch.dma('sp', dmg[:], merged[:].rearrange("p k t -> p (k t)"), reads=[t_merged])
                    dod = dram_out("dbg_o", [128, 3 * 4 * 512], BF16)
                    sch.dma('sp', dod[:, 0:2048], o_da[:].rearrange("p k t -> p (k t)"), reads=[t_oda])
                    sch.dma('sp', dod[:, 2048:4096], o_mla[:].rearrange("p k t -> p (k t)"), reads=[t_omla])
                    sch.dma('sp', dod[:, 4096:6144], o_pool[:].rearrange("p k t -> p (k t)"), reads=[t_opool])
        barrier()

    def phase3(l, do_ctx):
        moe = (l % 2 == 1)
        with ExitStack() as sc:
            lsb = mk_sb(sc)
            R = {'bf': Ring(lsb, "p3bf", 3, [128, 512], BF16), 'f32': Ring(lsb, "p3f", 4, [128, 512], F32),
                 'rstd': Ring(lsb, "p3r", 2, [128, 512], F32)}
            NT = TOK + (CTX if do_ctx else 0)
            h2 = lsb("h2", [128, KC, NT], BF16)
            t_h2 = Tk('h2')
            wsl = Ring(lsb, "w3", 5, [128, 4096], BF16)
            actb = [(lsb(f"actb{i}", [128, 4, 512], BF16), Tk(f'actb{i}')) for i in range(2)]
            blocks = [('l', b) for b in range(4)] + ([('c', 0)] if do_ctx else [])
            if moe:
                router_sb = lsb("router", [128, KC, NEXP], F32)
                t_router = Tk('router')
                sch.dma('sp', router_sb[:], router_d.rearrange("(kc p) e -> p kc e", p=128), writes=[t_router])
                gates = lsb("gates", [128, 16, NEXP], F32)
                gtmp = lsb("gtmp", [128, 16, NEXP], F32)
                gm = lsb("gm", [128, 16], F32)
                t_gates = Tk('gates')
                gbc = [(lsb(f"gbc{i}", [128, TOK], BF16), Tk(f'gbc{i}')) for i in range(2)]
                gmat = [(lsb(f"gmat{i}", [128, 128], F32), Tk(f'gmat{i}')) for i in range(2)]
                lgT = lsb("lgT", [32, 512], F32)
                t_lgT = Tk('lgT')
                sch.op('dve', lambda e: e.memset(lgT[:], 0.0), writes=[t_lgT])
            for (kind, b) in blocks:
                isc = kind == 'c'
                ntok = CTX if isc else 512
                j = 1 if isc else 0
                off = TOK if isc else b * 512
                if isc:
                    xsrc = lambda kc: xc_sb[:, kc, :]
                    t_xs = [[t_xc[kc]] for kc in range(KC)]
                else:
                    xsrc = lambda kc, off=off: x_sb[:, kc, off:off + 512]
                    t_xs = [[t_x[kc][b]] for kc in range(KC)]
                cb = None
                if moe:
                    def cb(kc, tmp, t_tmp, b=b, j=j):
                        hf, t_hf = R['f32'].next()
                        ACT(hf[:, :512], tmp[:, :512], AF.Identity, [t_tmp, t_mod], [t_hf],
                            scale=modA[:, l, 1, kc, j:j + 1], bias=modcol(l, 3, kc, j))
                        MM(psb[6][0:NEXP, :], router_sb[:, kc, :], hf[:, :512], kc == 0, kc == KC - 1,
                           [t_hf, t_router], [t_ps[6]])
                        if kc == KC - 1:
                            CP(lgT[0:NEXP, :], psb[6][0:NEXP, :], [t_ps[6]], [], pw=[t_lgT])
                            for ti in range(4):
                                c0 = (b * 4 + ti) * NEXP
                                MM(psb[5][:, c0:c0 + NEXP], lgT[0:32, ti * 128:(ti + 1) * 128],
                                   ident_f[0:32, 0:NEXP], True, True, [t_lgT, t_consts], [t_ps[5]])
                norm_mod(R, xsrc, t_xs, ntok, l, 1, j, h2[:, :, off:off + ntok], t_h2, f32cb=cb, bank=7)
            if moe:
                lg = psb[5][:, 0:16 * NEXP].rearrange("p (t e) -> p t e", e=NEXP)
                CP(gates[:], lg, [t_ps[5]], [t_gates])
                sch.op('dve', lambda e: e.tensor_reduce(out=gm[:], in_=gates[:], axis=mybir.AxisListType.X, op=ALU.max),
                       reads=[t_gates], pwrites=[t_gates])
                gmb = gm[:, :, None].to_broadcast([128, 16, NEXP])
                TT(gtmp[:], gates[:], gmb, ALU.is_equal, [t_gates], [], pw=[t_gates])
                STT(gtmp[:], gtmp[:], -BIG, gates[:], ALU.mult, ALU.add, [t_gates], [], pw=[t_gates])
                gm2 = lsb("gm2", [128, 16], F32)
                sch.op('dve', lambda e: e.tensor_reduce(out=gm2[:], in_=gtmp[:], axis=mybir.AxisListType.X, op=ALU.max),
                       reads=[t_gates], pwrites=[t_gates])
                gm2b = gm2[:, :, None].to_broadcast([128, 16, NEXP])
                TT(gtmp[:], gates[:], gm2b, ALU.is_ge, [t_gates], [], pw=[t_gates])
                TT(gates[:], gates[:], gmb, ALU.subtract, [t_gates], [], pw=[t_gates])
                ACT(gates[:], gates[:], AF.Exp, [t_gates], [], pw=[t_gates])
                TT(gates[:], gates[:], gtmp[:], ALU.mult, [t_gates], [], pw=[t_gates])
                sch.op('dve', lambda e: e.tensor_reduce(out=gm[:], in_=gates[:], axis=mybir.AxisListType.X, op=ALU.add),
                       reads=[t_gates], pwrites=[t_gates])
                sch.op('dve', lambda e: e.reciprocal(out=gm[:], in_=gm[:]), reads=[t_gates], pwrites=[t_gates])
                TT(gates[:], gates[:], gmb, ALU.mult, [t_gates], [], pw=[t_gates])
                if 'gates' in debug:
                    dg = dram_out("dbg_gates", [128, 16 * NEXP])
                    sch.dma('sp', dg[:], gates[:].rearrange("p t e -> p (t e)"), reads=[t_gates])

            def ffn_expert(wgv, wuv, wdv, dff, gb, t_gb):
                nfg = (dff + 511) // 512
                bi = 0
                for fg in range(nfg):
                    nf = min(512, dff - fg * 512)
                    nfc = nf // 128
                    ws, t_wg = wsl.next()
                    wg = ws[:, 0:KC * nf].rearrange("p (k n) -> p k n", k=KC)
                    wload(wg, wgv[:, :, fg * 512:fg * 512 + nf], t_wg)
                    ws, t_wu = wsl.next()
                    wu = ws[:, 0:KC * nf].rearrange("p (k n) -> p k n", k=KC)
                    wload(wu, wuv[:, :, fg * 512:fg * 512 + nf], t_wu)
                    ws, t_wd = wsl.next()
                    wd = ws[:, 0:nfc * 1024].rearrange("p (c n) -> p c n", c=nfc)
                    wload(wd, wdv[:, fg * 4:fg * 4 + nfc, :], t_wd)
                    for (kind, b) in blocks:
                        isc = kind == 'c'
                        ntok = CTX if isc else 512
                        j = 1 if isc else 0
                        off = TOK if isc else b * 512
                        t_xs = [[t_xc[kc]] for kc in range(KC)] if isc else [[t_x[kc][b]] for kc in range(KC)]
                        xs = (lambda kc: xc_sb[:, kc, :]) if isc else (lambda kc, off=off: x_sb[:, kc, off:off + 512])
                        ab, t_ab = actb[bi % 2]
                        bi += 1
                        for fc in range(nfc):
                            pg, pu = (0, 1) if fc % 2 == 0 else (2, 3)
                            for kc in range(KC):
                                MM(psb[pg][:, :ntok], wg[:, kc, fc * 128:(fc + 1) * 128], h2[:, kc, off:off + ntok],
                                   kc == 0, kc == KC - 1, [t_wg, t_h2], [t_ps[pg]])
                            for kc in range(KC):
                                MM(psb[pu][:, :ntok], wu[:, kc, fc * 128:(fc + 1) * 128], h2[:, kc, off:off + ntok],
                                   kc == 0, kc == KC - 1, [t_wu, t_h2], [t_ps[pu]])
                            sg, t_sg = R['f32'].next()
                            ACT(sg[:, :ntok], psb[pg][:, :ntok], AF.Silu, [t_ps[pg]], [t_sg])
                            if gb is None:
                                TT(ab[:, fc, :ntok], sg[:, :ntok], psb[pu][:, :ntok], ALU.mult, [t_sg, t_ps[pu]], [],
                                   pw=[t_ab])
                            else:
                                TT(sg[:, :ntok], sg[:, :ntok], psb[pu][:, :ntok], ALU.mult, [t_sg, t_ps[pu]], [t_sg])
                                TT(ab[:, fc, :ntok], sg[:, :ntok], gb[:, off:off + ntok], ALU.mult, [t_sg, t_gb], [],
                                   pw=[t_ab])
                        for dch in range(KC):
                            py = 4 + (dch % 2)
                            for fc in range(nfc):
                                MM(psb[py][:, :ntok], wd[:, fc, dch * 128:(dch + 1) * 128], ab[:, fc, :ntok],
                                   fc == 0, fc == nfc - 1, [t_wd, t_ab], [t_ps[py]])
                            STT(xs(dch), psb[py][:, :ntok], modcol(l, 5, dch, j), xs(dch), ALU.mult, ALU.add,
                                [t_ps[py], t_mod] + t_xs[dch], [], pw=t_xs[dch])

            if not moe:
                ffn_expert(ffn_wg.rearrange("(kc p) n -> p kc n", p=128), ffn_wu.rearrange("(kc p) n -> p kc n", p=128),
                           ffn_wd.rearrange("(c p) n -> p c n", p=128), D_FF, None, None)
            else:
                for ex in range(NEXP):
                    gb, t_gb = gbc[ex % 2]
                    for ti in range(16):
                        gmt, t_gmt = gmat[ti % 2]
                        TS(gmt[:], ones_f, gates[:, ti, ex:ex + 1], None, ALU.mult, None, [t_consts, t_gates], [t_gmt])
                        bk = 6 + (ti // 4) % 2
                        MM(psb[bk][:, (ti % 4) * 128:(ti % 4 + 1) * 128], gmt[:], ident_f, True, True,
                           [t_gmt, t_consts], [t_ps[bk]])
                        if ti % 4 == 3:
                            CP(gb[:, (ti // 4) * 512:(ti // 4 + 1) * 512], psb[bk][:, :], [t_ps[bk]], [],
                               pw=[t_gb])
                    ffn_expert(moe_wg[ex].rearrange("(kc p) n -> p kc n", p=128),
                               moe_wu[ex].rearrange("(kc p) n -> p kc n", p=128),
                               moe_wd[ex].rearrange("(c p) n -> p c n", p=128), D_FFE, gb, t_gb)
        barrier()

    def final_norm():
        with ExitStack() as sc:
            lsb = mk_sb(sc)
            R = {'bf': Ring(lsb, "fnbf", 3, [128, 512], BF16), 'f32': Ring(lsb, "fnf", 4, [128, 512], F32),
                 'rstd': Ring(lsb, "fnr", 2, [128, 512], F32)}
            ogf, _ = COLS['gfin']
            for b in range(4):
                off = b * 512
                for kc in range(KC):
                    sq, t_sq = R['bf'].next()
                    ACT(sq[:], x_sb[:, kc, off:off + 512], AF.Square, [t_x[kc][b]], [t_sq])
                    MM(psb[7][:], ones_bf, sq[:], kc == 0, kc == KC - 1, [t_sq, t_consts], [t_ps[7]])
                rstd, t_rstd = R['rstd'].next()
                RSTD(rstd[:], psb[7][:], 1.0 / D, [t_ps[7]], t_rstd)
                for kc in range(KC):
                    STT(x_sb[:, kc, off:off + 512], x_sb[:, kc, off:off + 512], cols[:, ogf + kc:ogf + kc + 1], rstd[:],
                        ALU.mult, ALU.mult, [t_x[kc][b], t_rstd, t_cols], [], pw=[t_x[kc][b]])
        barrier()

    def gather(l):
        for c in range(NCH + 1):
            if c < NCH:
                src = kvl[l][c * 128:(c + 1) * 128, :]
                dst = kvg[l][c * 512:(c + 1) * 512, :]
            else:
                src = kvl[l][R_U:R_U + 8, :]
                dst = kvg[l][NCH * 512:NCH * 512 + 32, :]
            sch.coll((lambda e, src=src, dst=dst: e.collective_compute(
                "AllGather", ALU.bypass, replica_groups=[[0, 1, 2, 3], [4, 5, 6, 7]], ins=[src], outs=[dst])),
                reads=[t_kvl[l]], writes=[t_kvg[l][c]])

    if A_:
        phase1(0, True)
    if B_:
        phase2(0, True)
        phase3(0, True)
        phase1(1, False)
        x1T = dram_out("x1T", [D, TOK])
        t_o1 = Tk('o1')
        for kc in range(KC):
            sch.dma('sp', x1T[kc * 128:(kc + 1) * 128, :], x_sb[:, kc, :], reads=t_x[kc], pwrites=[t_o1])
    if ALL:
        phase1(0, True)
        gather(0)
        mods_stage(MODS_REST, False)
        phase2(0, True)
        phase3(0, True)
        phase1(1, False)
        gather(1)
        phase2(1, False)
        phase3(1, False)
        final_norm()
    if C_:
        phase2(1, False)
        if 'xa' in debug:
            dxa = dram_out("dbg_xa", [D, TOK])
            for kc in range(KC):
                sch.dma('sp', dxa[kc * 128:(kc + 1) * 128, :], x_sb[:, kc, :], reads=t_x[kc])
            barrier()
        phase3(1, False)
        final_norm()

    if 'mod' in debug:
        dmod = dram_out("dbg_mod", [128, DEPTH * 96])
        sch.dma('sp', dmod[:], mod_sb[:].rearrange("p l m j -> p (l m j)"), reads=[t_mod])
        dmisc = dram_out("dbg_misc", [128, 16])
        sch.dma('sp', dmisc[:], misc[:], reads=[t_misc])
    if 'u' in debug:
        du = dram_out("dbg_u", [128, 4 * (TOK + 16)], BF16)
        sch.dma('sp', du[:], u_sb[:].rearrange("p g t -> p (g t)"), reads=[t_u])
    if 'x' in debug:
        dx = dram_out("dbg_x", [D, TOK])
        for kc in range(KC):
            sch.dma('sp', dx[kc * 128:(kc + 1) * 128, :], x_sb[:, kc, :], reads=t_x[kc])
        dxc = dram_out("dbg_xc", [D, CTX])
        for kc in range(KC):
            sch.dma('sp', dxc[kc * 128:(kc + 1) * 128, :], xc_sb[:, kc, :], reads=[t_xc[kc]])

    t_out = Tk('out')
    if C_ or ALL:
        outT = dram_out("outT", [D, TOK])
        for kc in range(KC):
            sch.dma('sp', outT[kc * 128:(kc + 1) * 128, :], x_sb[:, kc, :], reads=t_x[kc], pwrites=[t_out])
    barrier()
    sch.emit(nc, top)
    top.close()
    return nc


def _rope_tables():
    n_freq = 16
    inv = np.exp(-math.log(10000.0) * np.arange(n_freq, dtype=np.float32) * np.float32(2.0 / 32)).astype(np.float32)
    t = np.arange(S)
    row = (t // 64).astype(np.float32)
    colp = (t % 64).astype(np.float32)
    ar = row[:, None] * inv[None, :]
    ac = colp[:, None] * inv[None, :]
    C = np.zeros((128, S), np.float32)
    Sg = np.zeros((128, S), np.float32)
    for p in range(128):
        d = p % 64
        ang = ar if d < 32 else ac
        i = d % 16
        sign = -1.0 if (d % 32) < 16 else 1.0
        C[p] = np.cos(ang[:, i])
        Sg[p] = sign * np.sin(ang[:, i])
    return C, Sg


def _pool_fix(L, t_start, n):
    f = np.ones((4, 16), np.float32)
    for g, w in enumerate(POOL_WINDOWS):
        for k in range(16):
            t = t_start + k if k < 8 else t_start + n - 16 + k
            lo = min(max(t - w // 2, 0), L)
            hi = min(max(t - w // 2 + w, 0), L)
            f[g, k] = w / float(hi - lo)
    return f


def host_common(inp):
    cols = np.zeros((128, NCOLS), np.float32)

    def put(name, arr):
        o, w = COLS[name]
        assert arr.shape == (128, w), (name, arr.shape, w)
        cols[:, o:o + w] = arr
    put('cctx', _colvec(inp['c_ctx']))
    for l in range(DEPTH):
        put(f'bmod{l}', _colvec(inp['b_mod'][l]))
        put(f'gmix{l}', _colvec(inp['g_mix'][l]))
        put(f'gffn{l}', _colvec(inp['g_ffn'][l]))
        put(f'gq{l}', _colvec(inp['mla_gq'][l]))
        put(f'gkv{l}', _colvec(inp['mla_gkv'][l]))
        put(f'pscale{l}', _colvec(inp['pool_scale'][l]))
        put(f'subln{l}', _colvec(inp['da_subln'][l]))
        lam = np.zeros((128, 4), np.float32)
        lam[:64, :] = np.asarray(inp['da_lambda'][l], np.float32).T
        lam[64:, :] = 0.0
        put(f'lam{l}', lam)
    put('gfin', _colvec(inp['g_final']))
    put('pfixc', np.broadcast_to(_pool_fix(CTX, 0, CTX).reshape(1, 64), (128, 64)).copy())
    consts = np.zeros((128, 384), np.float32)
    consts[:, 0:128] = 1.0
    consts[:, 128:256] = np.eye(128, dtype=np.float32)
    for m in range(128):
        partner = (m & ~31) | ((m & 31) ^ 16)
        consts[partner, 256 + m] = 1.0
    constf = np.ascontiguousarray(consts[:, 0:256])
    consts = consts.astype(ml_dtypes.bfloat16)
    C, Sg = _rope_tables()
    uq_perm = np.concatenate([np.arange(h * 192, h * 192 + 128) for h in range(4)] +
                             [np.arange(h * 192 + 128, h * 192 + 192) for h in range(4)])
    ukv_perm = np.concatenate([np.arange(h * 256, h * 256 + 128) for h in range(4)] +
                              [np.arange(h * 256 + 128, h * 256 + 256) for h in range(4)])
    f32 = lambda a: np.ascontiguousarray(np.asarray(a, np.float32))
    shared = {
        "consts": consts, "constf": constf,
        "w_mod": f32(inp['w_mod']), "w_in": f32(inp['w_in']),
        "w_uq_p": f32(np.asarray(inp['w_uq'])[:, :, uq_perm]),
        "w_ukv_p": f32(np.asarray(inp['w_ukv'])[:, :, ukv_perm]),
        "pool_w": f32(inp['pool_w']), "w_branch": f32(inp['w_branch']), "w_out": f32(inp['w_out']),
    }
    percore = []
    for core in range(NCORE):
        b = core // 4
        t0 = (core % 4) * TOK
        cc = cols.copy()
        o, w = COLS['c']
        cc[:, o:o + w] = _colvec(inp['c'][b])
        o, w = COLS['pfix']
        cc[:, o:o + w] = np.broadcast_to(_pool_fix(S, t0, TOK).reshape(1, 64), (128, 64))
        s_ = core % 4
        o, w = COLS['selL']
        if s_ > 0:
            cc[:, o + s_ - 1] = 1.0
        o, w = COLS['selR']
        if s_ < 3:
            cc[:, o + s_ + 1] = 1.0
        percore.append({
            "cols": cc,
            "ropeC": np.ascontiguousarray(C[:, t0:t0 + TOK]).astype(ml_dtypes.bfloat16),
            "ropeS": np.ascontiguousarray(Sg[:, t0:t0 + TOK]).astype(ml_dtypes.bfloat16),
        })
    return shared, percore


def _gather_kv(res, name_l, name_c):
    per = []
    for core in range(NCORE):
        b, s = core // 4, core % 4
        sh = [np.asarray(res[b * 4 + r][name_l]) for r in range(4)]
        kvg = np.concatenate([sh[r][c * 128:(c + 1) * 128] for c in range(NCH) for r in range(4)] +
                             [sh[r][R_U:R_U + 8] for r in range(4)], axis=0)
        hl = np.zeros((128, 4, 16), ml_dtypes.bfloat16)
        if s > 0:
            nb_ = np.asarray(res[core - 1][name_l])[R_U:R_U + 4].reshape(4, 128, 16)
            hl[:, :, 0:8] = nb_[:, :, 8:16].transpose(1, 0, 2)
        if s < 3:
            nb_ = np.asarray(res[core + 1][name_l])[R_U:R_U + 4].reshape(4, 128, 16)
            hl[:, :, 8:16] = nb_[:, :, 0:8].transpose(1, 0, 2)
        per.append((np.ascontiguousarray(kvg), np.asarray(res[core][name_c]), hl))
    return per


def stage_maps(inputs, shared, percore, stage, prev=None):
    f32 = lambda a: np.ascontiguousarray(np.asarray(a, np.float32))
    x = np.asarray(inputs['x'], np.float32)
    ctx = np.asarray(inputs['ctx'], np.float32)
    extra = {}
    if stage in ('B', 'all'):
        extra.update(ffn_wg=f32(inputs['ffn_w_gate'][0]), ffn_wu=f32(inputs['ffn_w_up'][0]),
                     ffn_wd=f32(inputs['ffn_w_down'][0]))
    if stage in ('C', 'all'):
        extra.update(router=f32(inputs['moe_router'][0]), moe_wg=f32(inputs['moe_w_gate'][0]),
                     moe_wu=f32(inputs['moe_w_up'][0]), moe_wd=f32(inputs['moe_w_down'][0]))
    maps = []
    for core in range(NCORE):
        b = core // 4
        t0 = (core % 4) * TOK
        m = dict(shared)
        m.update(percore[core])
        m.update(extra)
        m["ctxT"] = np.ascontiguousarray(ctx[b].T)
        if stage == 'C':
            m["xT"] = np.ascontiguousarray(prev['x1T'][core])
        else:
            m["xT"] = np.ascontiguousarray(x[b, t0:t0 + TOK, :].T)
        if stage == 'B':
            m["kvg0"], m["kvc0"], m["halo0"] = prev['kv'][core]
            m["u_in"] = prev['u'][core]
            m["uc_in"] = prev['uc'][core]
        if stage == 'C':
            m["kvg1"], m["kvc1"], m["halo1"] = prev['kv'][core]
            m["u_in"] = prev['u'][core]
        maps.append(m)
    return maps


def kernel_unfused(**inputs):
    shared, percore = host_common(inputs)
    cores = list(range(NCORE))
    ra = run_bass_kernel_spmd(build('A'), stage_maps(inputs, shared, percore, 'A'), core_ids=cores).results
    kv0 = _gather_kv(ra, 'kvl0', 'kvc0')
    pa = dict(kv=kv0, u=[np.asarray(ra[c]['u_out']) for c in cores], uc=[np.asarray(ra[c]['uc_out']) for c in cores])
    rb = run_bass_kernel_spmd(build('B'), stage_maps(inputs, shared, percore, 'B', pa), core_ids=cores).results
    kv1 = _gather_kv(rb, 'kvl1', 'kvc1')
    pb = dict(kv=kv1, x1T=[np.asarray(rb[c]['x1T']) for c in cores], u=[np.asarray(rb[c]['u_out']) for c in cores])
    rc = run_bass_kernel_spmd(build('C'), stage_maps(inputs, shared, percore, 'C', pb), core_ids=cores).results
    out = np.zeros((2, S, D), np.float32)
    for core in cores:
        b = core // 4
        t0 = (core % 4) * TOK
        out[b, t0:t0 + TOK, :] = np.asarray(rc[core]["outT"]).T
    return out


def kernel(**inputs):
    shared, percore = host_common(inputs)
    cores = list(range(NCORE))
    res = run_bass_kernel_spmd(build('all'), stage_maps(inputs, shared, percore, 'all'), core_ids=cores).results
    out = np.zeros((2, S, D), np.float32)
    for core in cores:
        b = core // 4
        t0 = (core % 4) * TOK
        out[b, t0:t0 + TOK, :] = np.asarray(res[core]["outT"]).T
    return out
```

```python
import math
from contextlib import ExitStack
import numpy as np
import ml_dtypes
import concourse.bass as bass
import concourse.mybir as mybir
from concourse.bass_utils import run_bass_kernel_spmd

F32 = mybir.dt.float32
BF16 = mybir.dt.bfloat16
AF = mybir.ActivationFunctionType
ALU = mybir.AluOpType

D = 1024
KC = 8
S = 8192
NCORE = 8
TOK = 2048
CTX = 256
NKEY = S + CTX
DEPTH = 2
IN_W = 5824
D_FF = 2816
NEXP = 8
D_FFE = 3584
EPS = 1e-6

ENGS = ('pe', 'act', 'dve', 'pool', 'sp')


class Tk:
    __slots__ = ('name', 'w', 'r')

    def __init__(self, name):
        self.name = name
        self.w = {}
        self.r = {}


class Op:
    __slots__ = ('eng', 'fn', 'waits', 'idx', 'signal', 'dma', 'signo')

    def __init__(self, eng, fn, idx):
        self.eng = eng
        self.fn = fn
        self.waits = []
        self.idx = idx
        self.signal = False
        self.dma = None
        self.signo = None


class Sched:
    NDMA = 12
    EPOCH = 12000

    def __init__(self):
        self.ops = {e: [] for e in ENGS}
        self.seen = {e: {} for e in ENGS}
        self.dma_count = {e: 0 for e in ENGS}
        self.dma_slot_val = {}

    def _deps(self, reads, writes):
        deps = {}

        def add(d):
            for k, v in d.items():
                if deps.get(k, -1) < v:
                    deps[k] = v
        for t in reads:
            add(t.w)
        for t in writes:
            add(t.w)
            add(t.r)
        return deps

    def _apply_waits(self, op, deps, raw_keys):
        eng = op.eng
        seen = self.seen[eng]
        for k, v in deps.items():
            if k[0] == 'e' and k[1] == eng and k not in raw_keys:
                continue
            if seen.get(k, -1) >= v:
                continue
            seen[k] = v
            op.waits.append((k, v))
            if k[0] == 'e':
                self.ops[k[1]][v].signal = True

    def op(self, eng, fn, reads=(), writes=(), pwrites=()):
        o = Op(eng, fn, len(self.ops[eng]))
        deps = self._deps(reads, list(writes) + list(pwrites))
        raw = set()
        for t in reads:
            raw.update(t.w.keys())
        self._apply_waits(o, deps, raw)
        self.ops[eng].append(o)
        key = ('e', eng)
        for t in writes:
            t.w = {key: o.idx}
            t.r = {}
        for t in pwrites:
            t.w[key] = o.idx
        for t in reads:
            t.r[key] = o.idx
        return o

    def dma(self, q, out, in_, reads=(), writes=(), pwrites=()):
        n = self.dma_count[q]
        self.dma_count[q] += 1
        slot = n % self.NDMA
        key = ('d', q, slot)
        val = 16 * (n // self.NDMA + 1)
        o = Op(q, lambda e: e.dma_start(out=out, in_=in_), len(self.ops[q]))
        o.dma = (key, val)
        deps = self._deps(reads, list(writes) + list(pwrites))
        if val > 16:
            deps[key] = val - 16
        raw = set()
        for t in reads:
            raw.update(t.w.keys())
        raw.add(key)
        self._apply_waits(o, deps, raw)
        self.ops[q].append(o)
        for t in writes:
            t.w = {key: val}
            t.r = {}
        for t in pwrites:
            t.w[key] = val
        for t in reads:
            t.r[key] = val
        return o

    def coll(self, fn, reads=(), writes=(), pwrites=()):
        q = 'pool'
        n = self.ncoll = getattr(self, 'ncoll', 0) + 1
        key = ('c', n)
        o = Op(q, fn, len(self.ops[q]))
        o.dma = (key, 1)
        deps = self._deps(reads, list(writes) + list(pwrites))
        raw = set()
        for t in reads:
            raw.update(t.w.keys())
        self._apply_waits(o, deps, raw)
        self.ops[q].append(o)
        for t in writes:
            t.w = {key: 1}
            t.r = {}
        for t in pwrites:
            t.w[key] = 1
        for t in reads:
            t.r[key] = 1
        return o

    def emit(self, nc, stack):
        esems = {}
        for e in ENGS:
            k = 0
            for o in self.ops[e]:
                if o.dma is None and o.signal:
                    o.signo = k
                    k += 1
            nep = (k + self.EPOCH - 1) // self.EPOCH
            esems[e] = [stack.enter_context(nc.semaphore(f"s_{e}_{i}")) for i in range(nep)]
        dsems = {}
        for e in ENGS:
            for sl in range(min(self.NDMA, self.dma_count[e])):
                dsems[('d', e, sl)] = stack.enter_context(nc.semaphore(f"d_{e}_{sl}"))
        for i in range(1, getattr(self, 'ncoll', 0) + 1):
            dsems[('c', i)] = stack.enter_context(nc.semaphore(f"c_{i}"))
        ops = self.ops
        EP = self.EPOCH

        def run(ename, eng):
            for o in ops[ename]:
                for k, v in o.waits:
                    if k[0] in ('d', 'c'):
                        eng.wait_ge(dsems[k], v)
                    else:
                        sn = ops[k[1]][v].signo
                        eng.wait_ge(esems[k[1]][sn // EP], sn % EP + 1)
                ins = o.fn(eng)
                if o.dma is not None:
                    ins.then_inc(dsems[o.dma[0]], 1 if o.dma[0][0] == 'c' else 16)
                elif o.signal:
                    ins.then_inc(esems[ename][o.signo // EP], 1)

        with nc.Block() as block:
            @block.tensor
            def _(e):
                run('pe', e)

            @block.scalar
            def _(e):
                run('act', e)

            @block.vector
            def _(e):
                run('dve', e)

            @block.gpsimd
            def _(e):
                run('pool', e)

            @block.sync
            def _(e):
                run('sp', e)


R_KDA, R_KN, R_KR, R_VDA, R_VM, R_U, ROWS = 0, 512, 1024, 1152, 1664, 2176, 2184
NCH = 17
GROWS = NCH * 512 + 32
C_DAQ, C_DAK, C_DAV, C_QD, C_KVD, C_KR, C_POOL, C_GATE = 0, 512, 1024, 1536, 1920, 2176, 2240, 2752
LAM_INIT = [0.8 - 0.6 * math.exp(-0.3 * l) for l in range(DEPTH)]
POOL_WINDOWS = (2, 4, 8, 16)
BIG = 1.0e4


def _cols_layout():
    off = {}
    n = 0

    def add(name, w):
        nonlocal n
        off[name] = (n, w)
        n += w
    add('c', 8)
    add('cctx', 8)
    for l in range(DEPTH):
        add(f'bmod{l}', 48)
        add(f'gmix{l}', 8)
        add(f'gffn{l}', 8)
        add(f'gq{l}', 3)
        add(f'gkv{l}', 2)
        add(f'pscale{l}', 4)
        add(f'subln{l}', 1)
        add(f'lam{l}', 4)
    add('gfin', 8)
    add('pfix', 64)
    add('pfixc', 64)
    add('selL', 4)
    add('selR', 4)
    return off, n


COLS, NCOLS = _cols_layout()


def _colvec(v):
    v = np.asarray(v, np.float32)
    return np.ascontiguousarray(v.reshape(-1, 128).T)


class Ring:
    def __init__(self, mk, name, n, shape, dt):
        self.bufs = [(mk(f"{name}{i}", shape, dt), Tk(f"{name}{i}")) for i in range(n)]
        self.i = 0

    def next(self):
        b = self.bufs[self.i % len(self.bufs)]
        self.i += 1
        return b


def build(stage='all', debug=()):
    nc = bass.Bass("TRN2", target_bir_lowering=False)
    sch = Sched()
    top = ExitStack()

    def dram_in(name, shape, dt=F32):
        return nc.dram_tensor(name, list(shape), dt, kind="ExternalInput").ap()

    def dram_out(name, shape, dt=F32):
        return nc.dram_tensor(name, list(shape), dt, kind="ExternalOutput").ap()

    uniq = [0]

    def mk_sb(stack):
        def f(name, shape, dt):
            uniq[0] += 1
            return stack.enter_context(nc.sbuf_tensor(f"sb{uniq[0]}_{name}", list(shape), dt))
        return f
    sb = mk_sb(top)

    def MM(out, lhsT, rhs, start, stop, reads, writes):
        return sch.op('pe', lambda e: e.matmul(out, lhsT=lhsT, rhs=rhs, start=start, stop=stop),
                      reads=reads, writes=writes)

    def ACT(out, in_, func, reads, writes, scale=None, bias=None, pw=()):
        kw = {}
        if scale is not None:
            kw['scale'] = scale
        if bias is not None:
            kw['bias'] = bias
        return sch.op('act', lambda e: e.activation(out=out, in_=in_, func=func, **kw),
                      reads=reads, writes=writes, pwrites=pw)

    def TT(out, in0, in1, op, reads, writes, eng='dve', pw=()):
        return sch.op(eng, lambda e: e.tensor_tensor(out=out, in0=in0, in1=in1, op=op),
                      reads=reads, writes=writes, pwrites=pw)

    def TS(out, in0, s1, s2, op0, op1, reads, writes, eng='dve', pw=()):
        if op1 is None:
            return sch.op(eng, lambda e: e.tensor_scalar(out=out, in0=in0, scalar1=s1, scalar2=None, op0=op0),
                          reads=reads, writes=writes, pwrites=pw)
        return sch.op(eng, lambda e: e.tensor_scalar(out=out, in0=in0, scalar1=s1, scalar2=s2, op0=op0, op1=op1),
                      reads=reads, writes=writes, pwrites=pw)

    def STT(out, in0, scalar, in1, op0, op1, reads, writes, pw=()):
        return sch.op('dve', lambda e: e.scalar_tensor_tensor(out=out, in0=in0, scalar=scalar, in1=in1,
                                                              op0=op0, op1=op1),
                      reads=reads, writes=writes, pwrites=pw)

    def CP(out, in_, reads, writes, eng='dve', pw=()):
        if eng == 'act':
            return sch.op('act', lambda e: e.copy(out=out, in_=in_), reads=reads, writes=writes, pwrites=pw)
        return sch.op(eng, lambda e: e.tensor_copy(out=out, in_=in_), reads=reads, writes=writes, pwrites=pw)

    def RSTD(out, ps_in, inv_n, reads, t_out):
        ACT(out, ps_in, AF.Sqrt, reads, [t_out], scale=inv_n, bias=EPS)
        sch.op('dve', lambda e: e.reciprocal(out=out, in_=out), reads=[t_out], writes=[t_out])

    def RECIP(out, in_, reads, writes):
        return sch.op('dve', lambda e: e.reciprocal(out=out, in_=in_), reads=reads, writes=writes)

    def barrier():
        last = {e: len(sch.ops[e]) - 1 for e in ENGS}
        dl = {}
        for q in ENGS:
            n = sch.dma_count[q]
            for sl in range(min(n, sch.NDMA)):
                uses = (n - 1 - sl) // sch.NDMA + 1
                dl[('d', q, sl)] = 16 * uses
        for e in ENGS:
            o = Op(e, lambda en: en.nop(), len(sch.ops[e]))
            deps = dict(dl)
            for e2 in ENGS:
                if e2 != e and last[e2] >= 0:
                    k = last[e2]
                    while k >= 0 and sch.ops[e2][k].dma is not None:
                        k -= 1
                    if k >= 0:
                        deps[('e', e2)] = k
            sch._apply_waits(o, deps, set(deps.keys()))
            sch.ops[e].append(o)

    A_ = stage == 'A'
    B_ = stage == 'B'
    C_ = stage == 'C'
    ALL = stage == 'all'

    xT = dram_in("xT", [D, TOK])
    ctxT = dram_in("ctxT", [D, CTX])
    cols_d = dram_in("cols", [128, NCOLS])
    consts_d = dram_in("consts", [128, 384], BF16)
    constf_d = dram_in("constf", [128, 256], F32)
    ropeC_d = dram_in("ropeC", [128, TOK], BF16)
    ropeS_d = dram_in("ropeS", [128, TOK], BF16)
    w_mod = dram_in("w_mod", [DEPTH, D, 6 * D])
    w_in = dram_in("w_in", [DEPTH, D, IN_W])
    w_uq = dram_in("w_uq_p", [DEPTH, 384, 768])
    w_ukv = dram_in("w_ukv_p", [DEPTH, 256, 1024])
    pool_w = dram_in("pool_w", [DEPTH, 4, 128, 128])
    w_branch = dram_in("w_branch", [DEPTH, 3, 512, D])
    w_out = dram_in("w_out", [DEPTH, D, D])
    if B_ or ALL:
        ffn_wg = dram_in("ffn_wg", [D, D_FF])
        ffn_wu = dram_in("ffn_wu", [D, D_FF])
        ffn_wd = dram_in("ffn_wd", [D_FF, D])
    if C_ or ALL:
        router_d = dram_in("router", [D, NEXP])
        moe_wg = dram_in("moe_wg", [NEXP, D, D_FFE])
        moe_wu = dram_in("moe_wu", [NEXP, D, D_FFE])
        moe_wd = dram_in("moe_wd", [NEXP, D_FFE, D])

    kvl, kvc, kvg, halo = {}, {}, {}, {}
    t_kvl, t_kvc, t_kvg = {}, {}, {}
    if A_:
        kvl[0] = dram_out("kvl0", [ROWS, TOK], BF16)
        kvc[0] = dram_out("kvc0", [ROWS, CTX], BF16)
    if B_:
        kvg[0] = dram_in("kvg0", [GROWS, TOK], BF16)
        kvc[0] = dram_in("kvc0", [ROWS, CTX], BF16)
        halo[0] = dram_in("halo0", [128, 4, 16], BF16)
        kvl[1] = dram_out("kvl1", [ROWS, TOK], BF16)
        kvc[1] = dram_out("kvc1", [ROWS, CTX], BF16)
    if ALL:
        for l_ in range(DEPTH):
            kvl[l_] = nc.dram_tensor(f"kvl{l_}", [ROWS, TOK], BF16, kind="Internal").ap()
            kvc[l_] = nc.dram_tensor(f"kvc{l_}", [ROWS, CTX], BF16, kind="Internal").ap()
            kvg[l_] = nc.dram_tensor(f"kvg{l_}", [GROWS, TOK], BF16, kind="Internal").ap()
    if C_:
        kvg[1] = dram_in("kvg1", [GROWS, TOK], BF16)
        kvc[1] = dram_in("kvc1", [ROWS, CTX], BF16)
        halo[1] = dram_in("halo1", [128, 4, 16], BF16)
    u_out = u_in = uc_out = uc_in = None
    if A_ or B_:
        u_out = dram_out("u_out", [128, 4 * (TOK + 16)], BF16)
    if A_:
        uc_out = dram_out("uc_out", [128, 4 * (CTX + 16)], BF16)
    if B_ or C_:
        u_in = dram_in("u_in", [128, 4 * (TOK + 16)], BF16)
    if B_:
        uc_in = dram_in("uc_in", [128, 4 * (CTX + 16)], BF16)
    for l in range(DEPTH):
        t_kvl[l] = Tk(f'kvl{l}')
        t_kvc[l] = Tk(f'kvc{l}')
        t_kvg[l] = [Tk(f'kvg{l}_{c}') for c in range(NCH + 1)]

    psb = [top.enter_context(nc.psum_tensor(f"psb{i}", [128, 512], F32)) for i in range(8)]
    t_ps = [Tk(f'ps{i}') for i in range(8)]

    x_sb = sb("x_sb", [128, KC, TOK], F32)
    xc_sb = sb("xc_sb", [128, KC, CTX], F32)
    t_x = [[Tk(f'x{kc}_{b}') for b in range(4)] for kc in range(KC)]
    t_xc = [Tk(f'xc{kc}') for kc in range(KC)]
    cols = sb("cols_sb", [128, NCOLS], F32)
    t_cols = Tk('cols')
    consts = sb("consts_sb", [128, 384], BF16)
    constf = sb("constf_sb", [128, 256], F32)
    t_consts = Tk('consts')
    ones_bf = consts[:, 0:128]
    ident_bf = consts[:, 128:256]
    perm_bf = consts[:, 256:384]
    ones_f = constf[:, 0:128]
    ident_f = constf[:, 128:256]
    mod_sb = sb("mod_sb", [128, DEPTH, 48, 2], F32)
    modA = sb("modA_sb", [128, DEPTH, 2, 8, 2], F32)
    t_mod = Tk('mod')
    misc = sb("misc_sb", [128, 16], F32)
    t_misc = Tk('misc')
    u_sb = sb("u_sb", [128, 4, TOK + 16], BF16)
    uc_sb = sb("uc_sb", [128, 4, CTX + 16], BF16)
    t_u = Tk('u')
    t_uc = Tk('uc')

    def colap(name, j=0, w=1):
        o, _ = COLS[name]
        return cols[:, o + j:o + j + w]

    sch.dma('sp', cols[:], cols_d[:], writes=[t_cols])
    sch.dma('sp', consts[:], consts_d[:], writes=[t_consts])
    sch.dma('sp', constf[:], constf_d[:], pwrites=[t_consts])
    for kc in range(KC):
        sch.dma('sp', x_sb[:, kc, :], xT[kc * 128:(kc + 1) * 128, :], writes=t_x[kc])
    if not C_:
        for kc in range(KC):
            sch.dma('sp', xc_sb[:, kc, :], ctxT[kc * 128:(kc + 1) * 128, :], writes=[t_xc[kc]])
    if u_in is not None:
        sch.dma('sp', u_sb[:].rearrange("p g t -> p (g t)"), u_in[:], writes=[t_u])
    if uc_in is not None:
        sch.dma('sp', uc_sb[:].rearrange("p g t -> p (g t)"), uc_in[:], writes=[t_uc])

    def mods_stage(pairs, with_misc):
        with ExitStack() as sc:
            lsb = mk_sb(sc)
            silu_c = lsb("silu_c", [128, KC, 2], BF16)
            t_silu = Tk('silu')
            o_c, _ = COLS['c']
            o_cc, _ = COLS['cctx']
            ACT(silu_c[:, :, 0], cols[:, o_c:o_c + 8], AF.Silu, [t_cols], [t_silu])
            ACT(silu_c[:, :, 1], cols[:, o_cc:o_cc + 8], AF.Silu, [t_cols], [], pw=[t_silu])
            wm = [lsb(f"wm{i}", [128, KC, 1024], BF16) for i in range(2)]
            t_wm = [Tk('wm0'), Tk('wm1')]
            t_psmod = t_ps[7]
            ps_mod = psb[7][:, 0:192]
            psv = ps_mod.rearrange("p (l m j) -> p l m j", l=DEPTH, j=2)
            for it, (l, part) in enumerate(pairs):
                wv = w_mod[l].rearrange("(kc p) n -> p kc n", p=128)
                buf = it % 2
                sch.dma('pool', wm[buf][:], wv[:, :, part * 1024:(part + 1) * 1024], writes=[t_wm[buf]])
                for mc in range(8):
                    c0 = (l * 48 + part * 8 + mc) * 2
                    for kc in range(KC):
                        MM(ps_mod[:, c0:c0 + 2], wm[buf][:, kc, mc * 128:(mc + 1) * 128], silu_c[:, kc, :],
                           kc == 0, kc == KC - 1, [t_wm[buf], t_silu], [t_psmod])
            for (l, part) in pairs:
                ob, _ = COLS[f'bmod{l}']
                for j in range(2):
                    TT(mod_sb[:, l, part * 8:(part + 1) * 8, j], psv[:, l, part * 8:(part + 1) * 8, j],
                       cols[:, ob + part * 8:ob + (part + 1) * 8], ALU.add, [t_psmod, t_cols], [], pw=[t_mod])
            for (l, part) in pairs:
                if part not in (1, 4):
                    continue
                which = 0 if part == 1 else 1
                gname = f'gmix{l}' if which == 0 else f'gffn{l}'
                og, _ = COLS[gname]
                for j in range(2):
                    STT(modA[:, l, which, :, j], mod_sb[:, l, part * 8:(part + 1) * 8, j], 1.0,
                        cols[:, og:og + 8], ALU.add, ALU.mult, [t_mod, t_cols], [], pw=[t_mod])
            if with_misc:
                lamt = lsb("lamt", [128, 4], F32)
                t_lamt = Tk('lamt')
                for l in range(DEPTH):
                    ol, _ = COLS[f'lam{l}']
                    lv = cols[:, ol:ol + 4].rearrange("p (a b) -> p a b", b=2)
                    TT(lamt[:, 0:2], lv[:, :, 0], lv[:, :, 1], ALU.mult, [t_cols], [t_lamt])
                    MM(psb[6][:, 0:2], ones_f, lamt[:, 0:2], True, True, [t_lamt, t_consts], [t_ps[6]])
                    ACT(lamt[:, 2:4], psb[6][:, 0:2], AF.Exp, [t_ps[6]], [], pw=[t_lamt])
                    STT(misc[:, 4 * l:4 * l + 1], lamt[:, 3:4], -LAM_INIT[l], lamt[:, 2:3], ALU.add, ALU.subtract,
                        [t_lamt], [], pw=[t_misc])
                    osub, _ = COLS[f'subln{l}']
                    TS(misc[:, 4 * l + 1:4 * l + 2], cols[:, osub:osub + 1], 1.0 - LAM_INIT[l], None, ALU.mult, None,
                       [t_cols], [], pw=[t_misc])
        barrier()

    MODS_FIRST = [(0, 0), (0, 1), (0, 2)]
    MODS_REST = [(0, 3), (0, 4), (0, 5)] + [(1, p_) for p_ in range(6)]
    mods_stage(MODS_FIRST, True)
    if not ALL:
        mods_stage(MODS_REST, False)

    def modcol(l, part, kc, j):
        return mod_sb[:, l, part * 8 + kc, j:j + 1]

    def norm_mod(R, xsrc, t_xs, ntok, l, which, j, h_out, t_h, f32cb=None, bank=7):
        for kc in range(KC):
            sq, t_sq = R['bf'].next()
            ACT(sq[:, :ntok], xsrc(kc), AF.Square, t_xs[kc], [t_sq])
            MM(psb[bank][:, :ntok], ones_bf, sq[:, :ntok], kc == 0, kc == KC - 1, [t_sq, t_consts], [t_ps[bank]])
        rstd, t_rstd = R['rstd'].next()
        RSTD(rstd[:, :ntok], psb[bank][:, :ntok], 1.0 / D, [t_ps[bank]], t_rstd)
        shpart = 0 if which == 0 else 3
        for kc in range(KC):
            tmp, t_tmp = R['f32'].next()
            TT(tmp[:, :ntok], xsrc(kc), rstd[:, :ntok], ALU.mult, list(t_xs[kc]) + [t_rstd], [t_tmp])
            if h_out is not None:
                ACT(h_out[:, kc, :ntok], tmp[:, :ntok], AF.Identity, [t_tmp, t_mod], [] if kc else [t_h],
                    scale=modA[:, l, which, kc, j:j + 1], bias=modcol(l, shpart, kc, j), pw=[t_h] if kc else [])
            if f32cb is not None:
                f32cb(kc, tmp, t_tmp)

    def rope(R, z_bf, t_z, ntok, tok0, out_ap, t_out, ropeC, ropeS, t_rope, bank, pw=False):
        MM(psb[bank][:, :ntok], perm_bf, z_bf, True, True, [t_z, t_consts], [t_ps[bank]])
        t1, t_t1 = R['f32'].next()
        TT(t1[:, :ntok], z_bf, ropeC[:, tok0:tok0 + ntok], ALU.mult, [t_z, t_rope], [t_t1])
        t2, t_t2 = R['f32'].next()
        TT(t2[:, :ntok], psb[bank][:, :ntok], ropeS[:, tok0:tok0 + ntok], ALU.mult, [t_ps[bank], t_rope], [t_t2])
        if isinstance(out_ap, tuple):
            TT(out_ap[0], t1[0:64, :ntok], t2[0:64, :ntok], ALU.add, [t_t1, t_t2], [], pw=[t_out])
            TT(out_ap[1], t1[64:128, :ntok], t2[64:128, :ntok], ALU.add, [t_t1, t_t2], [], pw=[t_out])
            return
        TT(out_ap, t1[:, :ntok], t2[:, :ntok], ALU.add, [t_t1, t_t2], [] if pw else [t_out],
           pw=[t_out] if pw else [])

    def wload(dst, src, tk, first=True):
        sch.dma('pool', dst, src, writes=[tk] if first else [], pwrites=[] if first else [tk])

    def winv(l):
        return w_in[l].rearrange("(kc p) n -> p kc n", p=128)

    def phase1(l, with_ctx_u):
        with ExitStack() as sc:
            lsb = mk_sb(sc)
            R = {'bf': Ring(lsb, "p1bf", 4, [128, 512], BF16), 'f32': Ring(lsb, "p1f", 4, [128, 512], F32),
                 'rstd': Ring(lsb, "p1r", 2, [128, 512], F32)}
            wp = lsb("wP1", [128, KC, 1920], BF16)
            t_wp = Tk('wP1')
            wkv = lsb("wukv", [128, 2, 1024], BF16)
            t_wkv = Tk('wukv')
            ropeC = lsb("ropeC", [128, TOK], BF16)
            ropeS = lsb("ropeS", [128, TOK], BF16)
            t_rope = Tk('rope')
            sch.dma('sp', ropeC[:], ropeC_d[:], writes=[t_rope])
            sch.dma('sp', ropeS[:], ropeS_d[:], pwrites=[t_rope])
            wv = winv(l)
            first = True
            for (d0, s0, n) in ((0, C_DAK, 512), (512, C_DAV, 512), (1024, C_KVD, 256), (1280, C_KR, 64),
                                (1344, C_KR, 64), (1408, C_POOL, 512)):
                wload(wp[:, :, d0:d0 + n], wv[:, :, s0:s0 + n], t_wp, first)
                first = False
            wload(wkv[:], w_ukv[l].rearrange("(kc p) n -> p kc n", p=128), t_wkv)
            hring = [(lsb(f"p1h{i}", [128, KC, 512], BF16), Tk(f'p1h{i}')) for i in range(2)]
            kst = lsb("kst", [128, 9, 512], BF16)
            t_kst = Tk('kst')
            vst = lsb("vst", [128, 4, 1024], BF16)
            t_vst = Tk('vst')
            kvn = lsb("kvn", [128, 2, 512], BF16)
            t_kvn = Tk('kvn')
            kvf = lsb("kvf", [128, 2, 512], F32)
            t_kvf = Tk('kvf')
            hst = lsb("hst", [128, 4, 16], BF16)
            t_hst = Tk('hst')
            og, _ = COLS[f'gkv{l}']
            bankrr = [0]

            def nb():
                b = bankrr[0] % 6
                bankrr[0] += 1
                return b

            blocks = [('l', b) for b in range(4)] + [('c', 0)]
            for bi, (kind, b) in enumerate(blocks):
                isc = kind == 'c'
                ntok = CTX if isc else 512
                tok0 = 0 if isc else b * 512
                j = 1 if isc else 0
                if isc:
                    xsrc = lambda kc: xc_sb[:, kc, :]
                    t_xs = [[t_xc[kc]] for kc in range(KC)]
                else:
                    xsrc = lambda kc, tok0=tok0: x_sb[:, kc, tok0:tok0 + 512]
                    t_xs = [[t_x[kc][b]] for kc in range(KC)]
                h, t_h = hring[bi % 2]
                norm_mod(R, xsrc, t_xs, ntok, l, 0, j, h, t_h, bank=7)
                for ci in range(5):
                    c0 = ci * 128 if ci < 4 else 1280
                    bk = nb()
                    for kc in range(KC):
                        MM(psb[bk][:, :ntok], wp[:, kc, c0:c0 + 128], h[:, kc, :ntok], kc == 0, kc == KC - 1,
                           [t_wp, t_h], [t_ps[bk]])
                    dst = kst[:, ci if ci < 4 else 8, :ntok]
                    if isc:
                        CP(dst, psb[bk][:, :ntok], [t_ps[bk]], [], pw=[t_kst])
                    else:
                        z, t_z = R['bf'].next()
                        CP(z[:, :ntok], psb[bk][:, :ntok], [t_ps[bk]], [t_z], eng='act')
                        rope(R, z[:, :ntok], t_z, ntok, tok0, dst, t_kst, ropeC, ropeS, t_rope, nb(), pw=True)
                bks = []
                for ci in range(2):
                    bk = nb()
                    bks.append(bk)
                    for kc in range(KC):
                        MM(psb[bk][:, :ntok], wp[:, kc, 1024 + ci * 128:1024 + (ci + 1) * 128], h[:, kc, :ntok],
                           kc == 0, kc == KC - 1, [t_wp, t_h], [t_ps[bk]])
                    CP(kvf[:, ci, :ntok], psb[bk][:, :ntok], [t_ps[bk]], [], pw=[t_kvf])
                bk = nb()
                for ci in range(2):
                    sq, t_sq = R['bf'].next()
                    ACT(sq[:, :ntok], kvf[:, ci, :ntok], AF.Square, [t_kvf], [t_sq])
                    MM(psb[bk][:, :ntok], ones_bf, sq[:, :ntok], ci == 0, ci == 1, [t_sq, t_consts], [t_ps[bk]])
                rstd, t_rstd = R['rstd'].next()
                RSTD(rstd[:, :ntok], psb[bk][:, :ntok], 1.0 / 256, [t_ps[bk]], t_rstd)
                for ci in range(2):
                    STT(kvn[:, ci, :ntok], kvf[:, ci, :ntok], cols[:, og + ci:og + ci + 1], rstd[:, :ntok],
                        ALU.mult, ALU.mult, [t_kvf, t_rstd, t_cols], [], pw=[t_kvn])
                for hh in range(4):
                    bk = nb()
                    for kc in range(2):
                        MM(psb[bk][:, :ntok], wkv[:, kc, hh * 128:(hh + 1) * 128], kvn[:, kc, :ntok],
                           kc == 0, kc == 1, [t_wkv, t_kvn], [t_ps[bk]])
                    CP(kst[:, 4 + hh, :ntok], psb[bk][:, :ntok], [t_ps[bk]], [], pw=[t_kst])
                for ti in range(ntok // 128):
                    bk = nb()
                    for kc in range(KC):
                        MM(psb[bk][:, :], h[:, kc, ti * 128:(ti + 1) * 128], wp[:, kc, 512:1024],
                           kc == 0, kc == KC - 1, [t_wp, t_h], [t_ps[bk]])
                    CP(vst[:, ti, 0:512], psb[bk][:, :], [t_ps[bk]], [], eng='act', pw=[t_vst])
                    bk = nb()
                    for kc in range(2):
                        MM(psb[bk][:, :], kvn[:, kc, ti * 128:(ti + 1) * 128], wkv[:, kc, 512:1024],
                           kc == 0, kc == 1, [t_wkv, t_kvn], [t_ps[bk]])
                    CP(vst[:, ti, 512:1024], psb[bk][:, :], [t_ps[bk]], [], pw=[t_vst])
                if (not isc) or with_ctx_u:
                    ud = uc_sb if isc else u_sb
                    tu = t_uc if isc else t_u
                    for gi in range(4):
                        bk = nb()
                        for kc in range(KC):
                            MM(psb[bk][:, :ntok], wp[:, kc, 1408 + gi * 128:1408 + (gi + 1) * 128], h[:, kc, :ntok],
                               kc == 0, kc == KC - 1, [t_wp, t_h], [t_ps[bk]])
                        CP(ud[:, gi, 8 + tok0:8 + tok0 + ntok], psb[bk][:, :ntok], [t_ps[bk]], [], eng='act', pw=[tu])
                dk = kvc[l] if isc else kvl[l]
                tdk = t_kvc[l] if isc else t_kvl[l]
                sch.dma('sp', dk[R_KDA:R_KDA + 512, tok0:tok0 + ntok].rearrange("(c p) t -> p c t", p=128),
                        kst[:, 0:4, :ntok], reads=[t_kst], pwrites=[tdk])
                sch.dma('sp', dk[R_KN:R_KN + 512, tok0:tok0 + ntok].rearrange("(c p) t -> p c t", p=128),
                        kst[:, 4:8, :ntok], reads=[t_kst], pwrites=[tdk])
                sch.dma('sp', dk[R_KR:R_KR + 128, tok0:tok0 + ntok], kst[:, 8, :ntok], reads=[t_kst], pwrites=[tdk])
                for (r0, c0) in ((R_VDA, 0), (R_VM, 512)):
                    if isc:
                        vview = dk[r0:r0 + 512, :].rearrange("(t a) c -> t (a c)", a=2)
                        sch.dma('sp', vview[tok0:tok0 + ntok, :].rearrange("(i p) f -> p i f", p=128),
                                vst[:, 0:ntok // 128, c0:c0 + 512], reads=[t_vst], pwrites=[tdk])
                    else:
                        grp, i0 = b // 2, (b % 2) * 4
                        for hh in range(4):
                            rb = r0 + (hh * 2 + grp) * 64
                            blk = dk[rb:rb + 64, :].rearrange("r (a q) -> (r a) q", a=2).rearrange(
                                "p (i f) -> p i f", f=128)
                            sch.dma('sp', blk[:, i0:i0 + 4, :], vst[:, 0:4, c0 + hh * 128:c0 + (hh + 1) * 128],
                                    reads=[t_vst], pwrites=[tdk])
            if 'h' in debug:
                dh = dram_out("dbg_h", [128, KC * 512], BF16)
                sch.dma('sp', dh[:], hring[1][0][:].rearrange("p k t -> p (k t)"), reads=[hring[1][1]])
            if u_out is not None:
                sch.dma('sp', u_out[:], u_sb[:].rearrange("p g t -> p (g t)"), reads=[t_u])
            if uc_out is not None and with_ctx_u:
                sch.dma('sp', uc_out[:], uc_sb[:].rearrange("p g t -> p (g t)"), reads=[t_uc])
            if l in kvl:
                CP(hst[:, :, 0:8], u_sb[:, :, 8:16], [t_u], [t_hst], eng='pool')
                CP(hst[:, :, 8:16], u_sb[:, :, TOK:TOK + 8], [t_u], [], eng='pool', pw=[t_hst])
                sch.dma('sp', kvl[l][R_U:R_U + 4, :].rearrange("g (p t) -> p g t", t=16), hst[:],
                        reads=[t_hst], pwrites=[t_kvl[l]])
        barrier()

    def phase2(l, do_ctx):
        with ExitStack() as sc:
            lsb = mk_sb(sc)
            R = {"bf": Ring(lsb, "p2bf", 3, [128, 512], BF16), "f32": Ring(lsb, "p2f", 3, [128, 512], F32),
                 'rstd': Ring(lsb, "p2r", 2, [128, 512], F32)}
            wsl = Ring(lsb, "wsl", 4, [128, 2048], BF16)
            h = lsb("p2h", [128, KC, 512], BF16)
            t_h = Tk('p2h')
            ropeC = lsb("ropeCq", [128, 512], BF16)
            ropeS = lsb("ropeSq", [128, 512], BF16)
            t_rope = Tk('ropeq')
            qdaA = lsb("qdaA", [128, 4, 512], BF16)
            qdaB = lsb("qdaB", [128, 4, 512], BF16)
            qmr = lsb("qmr", [128, 4, 512], BF16)
            t_qda = Tk('qda')
            sch.op('dve', lambda e: e.memset(qdaA[64:128, :, :], 0.0), pwrites=[t_qda])
            sch.op('dve', lambda e: e.memset(qdaB[0:64, :, :], 0.0), pwrites=[t_qda])
            qm = lsb("qm", [128, 4, 512], BF16)
            t_qm = Tk('qm')
            for hh_ in range(4):
                if hh_ % 2 == 0:
                    sch.op('dve', lambda e, hh_=hh_: e.memset(qmr[64:128, hh_, :], 0.0), pwrites=[t_qm])
                else:
                    sch.op('dve', lambda e, hh_=hh_: e.memset(qmr[0:64, hh_, :], 0.0), pwrites=[t_qm])
            qn = lsb("qn", [128, 3, 512], BF16)
            t_qn = Tk('qn')
            o_da = lsb("o_da", [128, 4, 512], BF16)
            o_mla = lsb("o_mla", [128, 4, 512], BF16)
            o_pool = lsb("o_pool", [128, 4, 512], BF16)
            t_oda, t_omla, t_opool = Tk('oda'), Tk('omla'), Tk('opool')
            da_a = lsb("da_a", [128, 4, 512], BF16)
            t_daa = Tk('daa')
            merged = lsb("merged", [128, KC, 512], BF16)
            t_merged = Tk('merged')
            pring = Ring(lsb, "pT", 4, [128, 512], BF16)
            fA, fB, fC = (lsb(n_, [128, 512], F32) for n_ in ("fA", "fB", "fC"))
            t_fA, t_fB, t_fC = Tk('fA'), Tk('fB'), Tk('fC')
            slots = []
            for i in range(2):
                slots.append(dict(K1=lsb(f"kK1_{i}", [128, 1024], BF16), tK1=Tk(f'kK1_{i}'),
                                  K2=lsb(f"kK2_{i}", [128, 1024], BF16), tK2=Tk(f'kK2_{i}'),
                                  V=lsb(f"kV_{i}", [128, 8, 128], BF16), tV=Tk(f'kV_{i}')))
            pa = lsb("pa", [128, 528], F32)
            pb_ = lsb("pb", [128, 528], F32)
            t_pa, t_pb = Tk('pa'), Tk('pb')
            dbf = lsb("dbf", [128, 512], BF16)
            t_dbf = Tk('dbf')
            wv = winv(l)
            ogq, _ = COLS[f'gq{l}']
            ops_, _ = COLS[f'pscale{l}']
            neglam = misc[:, 4 * l:4 * l + 1]
            sublnS = misc[:, 4 * l + 1:4 * l + 2]

            if ALL:
                hall = lsb("hall", [128, 4, 4, 16], BF16)
                t_hall = Tk('hall')
                hb = NCH * 512
                for r in range(4):
                    sch.dma('sp', hall[:, r, :, :], kvg[l][hb + r * 8:hb + r * 8 + 4, :].rearrange("g (p t) -> p g t", t=16)[:, :, 0:16],
                            reads=[t_kvg[l][NCH]], writes=[t_hall] if r == 0 else [], pwrites=[] if r == 0 else [t_hall])
                oL, _ = COLS['selL']
                oR, _ = COLS['selR']
                for r in range(4):
                    if r == 0:
                        TS(u_sb[:, :, 0:8], hall[:, r, :, 8:16], cols[:, oL + r:oL + r + 1], None, ALU.mult, None,
                           [t_hall, t_cols], [], pw=[t_u])
                        TS(u_sb[:, :, TOK + 8:TOK + 16], hall[:, r, :, 0:8], cols[:, oR + r:oR + r + 1], None, ALU.mult, None,
                           [t_hall, t_cols], [], pw=[t_u])
                    else:
                        STT(u_sb[:, :, 0:8], hall[:, r, :, 8:16], cols[:, oL + r:oL + r + 1], u_sb[:, :, 0:8],
                            ALU.mult, ALU.add, [t_hall, t_cols, t_u], [], pw=[t_u])
                        STT(u_sb[:, :, TOK + 8:TOK + 16], hall[:, r, :, 0:8], cols[:, oR + r:oR + r + 1],
                            u_sb[:, :, TOK + 8:TOK + 16], ALU.mult, ALU.add, [t_hall, t_cols, t_u], [], pw=[t_u])
            if l in halo:
                sch.dma('sp', u_sb[:, :, 0:8], halo[l][:, :, 0:8], pwrites=[t_u])
                sch.dma('sp', u_sb[:, :, TOK + 8:TOK + 16], halo[l][:, :, 8:16], pwrites=[t_u])
            if do_ctx:
                sch.op('dve', lambda e: e.memset(uc_sb[:, :, 0:8], 0.0), pwrites=[t_uc])
                sch.op('dve', lambda e: e.memset(uc_sb[:, :, CTX + 8:CTX + 16], 0.0), pwrites=[t_uc])

            def load_group(kind, hh, g, slot):
                if kind == 'da':
                    rk, rv = R_KDA, R_VDA
                else:
                    rk, rv = R_KN, R_VM
                if g[0] == 'c':
                    src, tsrc = kvc[l], [t_kvc[l]]
                    vv = src[rv:rv + 512, :].rearrange("(t a) c -> t (a c)", a=2)
                    sch.dma('sp', slot['K1'][:, :CTX], src[rk + hh * 128:rk + (hh + 1) * 128, :],
                            reads=tsrc, writes=[slot['tK1']])
                    if kind == 'mla':
                        sch.dma('sp', slot['K2'][:, :CTX], src[R_KR:R_KR + 128, :], reads=tsrc, writes=[slot['tK2']])
                    sch.dma('sp', slot['V'][:, :CTX // 128, :],
                            vv[:, hh * 128:(hh + 1) * 128].rearrange("(i p) f -> p i f", p=128),
                            reads=tsrc, writes=[slot['tV']])
                    return
                _, r, hf = g
                src = kvg[l]
                col0 = hf * 1024

                def rows(row0):
                    c = row0 // 128
                    return src[c * 512 + r * 128:c * 512 + (r + 1) * 128, :], t_kvg[l][c]
                ap, tk = rows(rk + hh * 128)
                sch.dma('sp', slot['K1'][:, :1024], ap[:, col0:col0 + 1024], reads=[tk], writes=[slot['tK1']])
                if kind == 'mla':
                    ap, tk = rows(R_KR)
                    sch.dma('sp', slot['K2'][:, :1024], ap[:, col0:col0 + 1024], reads=[tk], writes=[slot['tK2']])
                rb = rv + (hh * 2 + hf) * 64
                c = rb // 128
                o_ = c * 512 + r * 128 + (rb % 128)
                blk = src[o_:o_ + 64, :].rearrange("r (a q) -> (r a) q", a=2).rearrange("p (i f) -> p i f", f=128)
                sch.dma('sp', slot['V'][:, :, :], blk, reads=[t_kvg[l][c]], writes=[slot['tV']])

            def attention(nq, groups):
                loads = [(kind, hh, g) for kind in ('da', 'mla') for hh in range(4) for g in groups]
                issued = [0]

                def ensure(n):
                    while issued[0] < min(n, len(loads)):
                        k_, h_, g_ = loads[issued[0]]
                        load_group(k_, h_, g_, slots[issued[0] % 2])
                        issued[0] += 1
                NS = len(slots)
                ensure(NS)
                idx = 0
                for kind in ('da', 'mla'):
                    for hh in range(4):
                        tiles = []
                        for g in groups:
                            nt = 2 if g[0] == 'c' else 8
                            tiles += [(idx, tt) for tt in range(nt)]
                            idx += 1
                        n = len(tiles)
                        if kind == 'da':
                            def S1(i):
                                gi, tt = tiles[i]
                                sl = slots[gi % 2]
                                bk = 0 if i % 2 == 0 else 6
                                MM(psb[bk][:, :nq], sl['K1'][:, tt * 128:(tt + 1) * 128], qdaA[:, hh, :nq],
                                   True, True, [sl['tK1'], t_qda], [t_ps[bk]])

                            def S2(i):
                                gi, tt = tiles[i]
                                sl = slots[gi % 2]
                                bk = 1 if i % 2 == 0 else 7
                                MM(psb[bk][:, :nq], sl['K1'][:, tt * 128:(tt + 1) * 128], qdaB[:, hh, :nq],
                                   True, True, [sl['tK1'], t_qda], [t_ps[bk]])
                            S1(0)
                            S2(0)
                            for i in range(n):
                                gi, tt = tiles[i]
                                sl = slots[gi % 2]
                                p1, t_p1 = pring.next()
                                p2, t_p2 = pring.next()
                                b1, b2 = (0, 1) if i % 2 == 0 else (6, 7)
                                ACT(p1[:, :nq], psb[b1][:, :nq], AF.Exp, [t_ps[b1]], [t_p1], scale=0.125)
                                ACT(p2[:, :nq], psb[b2][:, :nq], AF.Exp, [t_ps[b2]], [t_p2], scale=0.125)
                                if i + 1 < n:
                                    S1(i + 1)
                                    S2(i + 1)
                                MM(psb[2][:, :nq], sl['V'][:, tt, :], p1[:, :nq], i == 0, i == n - 1,
                                   [sl['tV'], t_p1], [t_ps[2]])
                                MM(psb[3][:, :nq], sl['V'][:, tt, :], p2[:, :nq], i == 0, i == n - 1,
                                   [sl['tV'], t_p2], [t_ps[3]])
                                if i == 0:
                                    CP(pa[:, :nq], p1[:, :nq], [t_p1], [t_pa])
                                    CP(pb_[:, :nq], p2[:, :nq], [t_p2], [t_pb])
                                else:
                                    TT(pa[:, :nq], pa[:, :nq], p1[:, :nq], ALU.add, [t_pa, t_p1], [], pw=[t_pa])
                                    TT(pb_[:, :nq], pb_[:, :nq], p2[:, :nq], ALU.add, [t_pb, t_p2], [], pw=[t_pb])
                                if i == n - 1 or tiles[i + 1][0] != gi:
                                    ensure(gi + NS + 1)
                            MM(psb[4][:, :nq], ones_f, pa[:, :nq], True, True, [t_pa, t_consts], [t_ps[4]])
                            MM(psb[5][:, :nq], ones_f, pb_[:, :nq], True, True, [t_pb, t_consts], [t_ps[5]])
                            RECIP(fA[:, :nq], psb[4][:, :nq], [t_ps[4]], [t_fA])
                            TT(fB[:, :nq], psb[2][:, :nq], fA[:, :nq], ALU.mult, [t_ps[2], t_fA], [t_fB])
                            RECIP(fA[:, :nq], psb[5][:, :nq], [t_ps[5]], [t_fA])
                            TT(fC[:, :nq], psb[3][:, :nq], fA[:, :nq], ALU.mult, [t_ps[3], t_fA], [t_fC])
                            STT(da_a[:, hh, :nq], fC[:, :nq], neglam, fB[:, :nq], ALU.mult, ALU.add,
                                [t_fC, t_fB, t_misc], [], pw=[t_daa])
                        else:
                            pbase = 64 * (hh % 2)

                            def SM(i):
                                gi, tt = tiles[i]
                                sl = slots[gi % 2]
                                bk = i % 2
                                MM(psb[bk][:, :nq], sl['K1'][:, tt * 128:(tt + 1) * 128], qm[:, hh, :nq],
                                   True, False, [sl['tK1'], t_qm], [t_ps[bk]])
                                MM(psb[bk][:, :nq], sl['K2'][:, tt * 128:(tt + 1) * 128],
                                   qmr[:, hh, :nq], False, True, [sl['tK2'], t_qm], [t_ps[bk]])
                            SM(0)
                            for i in range(n):
                                gi, tt = tiles[i]
                                sl = slots[gi % 2]
                                if i + 1 < n:
                                    SM(i + 1)
                                p1, t_p1 = pring.next()
                                ACT(p1[:, :nq], psb[i % 2][:, :nq], AF.Exp, [t_ps[i % 2]], [t_p1], scale=192.0 ** -0.5)
                                MM(psb[2][:, :nq], sl['V'][:, tt, :], p1[:, :nq], i == 0, i == n - 1,
                                   [sl['tV'], t_p1], [t_ps[2]])
                                if i == 0:
                                    CP(pa[:, :nq], p1[:, :nq], [t_p1], [t_pa])
                                else:
                                    TT(pa[:, :nq], pa[:, :nq], p1[:, :nq], ALU.add, [t_pa, t_p1], [], pw=[t_pa])
                                if i == n - 1 or tiles[i + 1][0] != gi:
                                    ensure(gi + NS + 1)
                            MM(psb[4][:, :nq], ones_f, pa[:, :nq], True, True, [t_pa, t_consts], [t_ps[4]])
                            RECIP(fA[:, :nq], psb[4][:, :nq], [t_ps[4]], [t_fA])
                            TT(o_mla[:, hh, :nq], psb[2][:, :nq], fA[:, :nq], ALU.mult, [t_ps[2], t_fA], [],
                               pw=[t_omla])
                for hh in range(4):
                    sq, t_sq = R['bf'].next()
                    TT(sq[:, :nq], da_a[:, hh, :nq], da_a[:, hh, :nq], ALU.mult, [t_daa], [t_sq])
                    MM(psb[6][:, :nq], ones_bf, sq[:, :nq], True, True, [t_sq, t_consts], [t_ps[6]])
                    rstd, t_rstd = R['rstd'].next()
                    RSTD(rstd[:, :nq], psb[6][:, :nq], 1.0 / 128, [t_ps[6]], t_rstd)
                    STT(o_da[:, hh, :nq], da_a[:, hh, :nq], sublnS, rstd[:, :nq], ALU.mult, ALU.mult,
                        [t_daa, t_rstd, t_misc], [], pw=[t_oda])

            blocks = ([('c', 0)] if do_ctx else []) + [('l', b) for b in range(4)]
            bankrr = [0]

            def nb():
                b = bankrr[0] % 6
                bankrr[0] += 1
                return b

            for (kind, b) in blocks:
                isc = kind == 'c'
                nq = CTX if isc else 512
                tok0 = 0 if isc else b * 512
                j = 1 if isc else 0
                if isc:
                    xsrc = lambda kc: xc_sb[:, kc, :]
                    xdst = lambda kc: xc_sb[:, kc, :]
                    t_xs = [[t_xc[kc]] for kc in range(KC)]
                else:
                    xsrc = lambda kc, tok0=tok0: x_sb[:, kc, tok0:tok0 + 512]
                    xdst = xsrc
                    t_xs = [[t_x[kc][b]] for kc in range(KC)]
                norm_mod(R, xsrc, t_xs, nq, l, 0, j, h, t_h, bank=7)
                if not isc:
                    sch.dma('sp', ropeC[:], ropeC_d[:, tok0:tok0 + 512], writes=[t_rope])
                    sch.dma('sp', ropeS[:], ropeS_d[:, tok0:tok0 + 512], pwrites=[t_rope])
                for half in range(2):
                    ws, t_ws = wsl.next()
                    wq = ws[:, :].rearrange("p (k n) -> p k n", k=KC)
                    wload(wq, wv[:, :, C_DAQ + half * 256:C_DAQ + (half + 1) * 256], t_ws)
                    for dd in range(2):
                        hh = half * 2 + dd
                        bk = nb()
                        for kc in range(KC):
                            MM(psb[bk][:, :nq], wq[:, kc, dd * 128:(dd + 1) * 128], h[:, kc, :nq], kc == 0, kc == KC - 1,
                               [t_ws, t_h], [t_ps[bk]])
                        if isc:
                            CP(qdaA[0:64, hh, :nq], psb[bk][0:64, :nq], [t_ps[bk]], [], pw=[t_qda])
                            CP(qdaB[64:128, hh, :nq], psb[bk][64:128, :nq], [t_ps[bk]], [], pw=[t_qda])
                        else:
                            z, t_z = R['bf'].next()
                            CP(z[:, :nq], psb[bk][:, :nq], [t_ps[bk]], [t_z], eng='act')
                            rope(R, z[:, :nq], t_z, nq, 0, (qdaA[0:64, hh, :nq], qdaB[64:128, hh, :nq]), t_qda,
                                 ropeC, ropeS, t_rope, nb(), pw=True)
                qbanks = []
                for (c0, ncol) in ((0, 256), (256, 128)):
                    ws, t_ws = wsl.next()
                    wq = ws[:, 0:KC * ncol].rearrange("p (k n) -> p k n", k=KC)
                    wload(wq, wv[:, :, C_QD + c0:C_QD + c0 + ncol], t_ws)
                    for dd in range(ncol // 128):
                        bk = nb()
                        qbanks.append(bk)
                        for kc in range(KC):
                            MM(psb[bk][:, :nq], wq[:, kc, dd * 128:(dd + 1) * 128], h[:, kc, :nq], kc == 0, kc == KC - 1,
                               [t_ws, t_h], [t_ps[bk]])
                for ci, bk in enumerate(qbanks):
                    sq, t_sq = R['bf'].next()
                    ACT(sq[:, :nq], psb[bk][:, :nq], AF.Square, [t_ps[bk]], [t_sq])
                    MM(psb[6][:, :nq], ones_bf, sq[:, :nq], ci == 0, ci == 2, [t_sq, t_consts], [t_ps[6]])
                rstd, t_rstd = R['rstd'].next()
                RSTD(rstd[:, :nq], psb[6][:, :nq], 1.0 / 384, [t_ps[6]], t_rstd)
                for ci, bk in enumerate(qbanks):
                    STT(qn[:, ci, :nq], psb[bk][:, :nq], cols[:, ogq + ci:ogq + ci + 1], rstd[:, :nq],
                        ALU.mult, ALU.mult, [t_ps[bk], t_rstd, t_cols], [], pw=[t_qn])
                wuv = w_uq[l].rearrange("(kc p) n -> p kc n", p=128)
                ws, t_ws = wsl.next()
                wun = ws[:, 0:3 * 512].rearrange("p (k n) -> p k n", k=3)
                wload(wun, wuv[:, :, 0:512], t_ws)
                ws2, t_ws2 = wsl.next()
                wur = ws2[:, 0:3 * 256].rearrange("p (k n) -> p k n", k=3)
                wload(wur, wuv[:, :, 512:768], t_ws2)
                for hh in range(4):
                    bk = nb()
                    for kc in range(3):
                        MM(psb[bk][:, :nq], wun[:, kc, hh * 128:(hh + 1) * 128], qn[:, kc, :nq], kc == 0, kc == 2,
                           [t_ws, t_qn], [t_ps[bk]])
                    CP(qm[:, hh, :nq], psb[bk][:, :nq], [t_ps[bk]], [], pw=[t_qm])
                for rc in range(2):
                    bk = nb()
                    for kc in range(3):
                        MM(psb[bk][:, :nq], wur[:, kc, rc * 128:(rc + 1) * 128], qn[:, kc, :nq], kc == 0, kc == 2,
                           [t_ws2, t_qn], [t_ps[bk]])
                    if isc:
                        CP(qmr[0:64, 2 * rc, :nq], psb[bk][0:64, :nq], [t_ps[bk]], [], pw=[t_qm])
                        CP(qmr[64:128, 2 * rc + 1, :nq], psb[bk][64:128, :nq], [t_ps[bk]], [], pw=[t_qm])
                    else:
                        z, t_z = R['bf'].next()
                        CP(z[:, :nq], psb[bk][:, :nq], [t_ps[bk]], [t_z], eng='act')
                        rope(R, z[:, :nq], t_z, nq, 0, (qmr[0:64, 2 * rc, :nq], qmr[64:128, 2 * rc + 1, :nq]), t_qm,
                             ropeC, ropeS, t_rope, nb(), pw=True)
                groups = [('c',)] + ([] if isc else [('l', r, hf) for r in range(4) for hf in range(2)])
                attention(nq, groups)
                usrc = uc_sb if isc else u_sb
                tus = t_uc if isc else t_u
                E = nq + 16
                ws, t_ws = wsl.next()
                wpl = ws[:, 0:512].rearrange("p (g d) -> p g d", g=4)
                wload(wpl, pool_w[l].rearrange("g c d -> c g d"), t_ws)
                fixname = 'pfixc' if isc else 'pfix'
                ofx, _ = COLS[fixname]
                for gi, w in enumerate(POOL_WINDOWS):
                    ue = usrc[:, gi, tok0:tok0 + E]
                    TT(pa[:, 1:E], ue[:, 0:E - 1], ue[:, 1:E], ALU.add, [tus], [t_pa])
                    cur, tcur = pa, t_pa
                    if w >= 4:
                        TT(pb_[:, 2:E - 1], pa[:, 1:E - 2], pa[:, 3:E], ALU.add, [t_pa], [t_pb])
                        cur, tcur = pb_, t_pb
                    if w >= 8:
                        TT(pa[:, 4:E - 3], pb_[:, 2:E - 5], pb_[:, 6:E - 1], ALU.add, [t_pb], [t_pa])
                        cur, tcur = pa, t_pa
                    if w >= 16:
                        TT(pb_[:, 8:E - 7], pa[:, 4:E - 11], pa[:, 12:E - 3], ALU.add, [t_pa], [t_pb])
                        cur, tcur = pb_, t_pb
                    if isc or b == 0:
                        TT(cur[:, 8:16], cur[:, 8:16], cols[:, ofx + gi * 16:ofx + gi * 16 + 8], ALU.mult,
                           [tcur, t_cols], [], pw=[tcur])
                    if isc or b == 3:
                        TT(cur[:, nq:nq + 8], cur[:, nq:nq + 8], cols[:, ofx + gi * 16 + 8:ofx + gi * 16 + 16], ALU.mult,
                           [tcur, t_cols], [], pw=[tcur])
                    STT(dbf[:, :nq], cur[:, 8:8 + nq], 1.0 / w, ue[:, 8:8 + nq], ALU.mult, ALU.subtract,
                        [tcur, tus], [t_dbf])
                    bk = nb()
                    MM(psb[bk][:, :nq], wpl[:, gi, :], dbf[:, :nq], True, True, [t_ws, t_dbf], [t_ps[bk]])
                    TS(o_pool[:, gi, :nq], psb[bk][:, :nq], cols[:, ops_ + gi:ops_ + gi + 1], None, ALU.mult, None,
                       [t_ps[bk], t_cols], [], pw=[t_opool])
                wbv = w_branch[l].rearrange("n (cc p) d -> p n cc d", p=128)
                for n_, (o_n, t_on) in enumerate(((o_da, t_oda), (o_mla, t_omla), (o_pool, t_opool))):
                    for half in range(2):
                        ws, t_wb = wsl.next()
                        wb = ws[:, :].rearrange("p (c d) -> p c d", c=4)
                        wload(wb, wbv[:, n_, :, half * 512:(half + 1) * 512], t_wb)
                        for quarter in range(2):
                            dpair = half * 2 + quarter
                            ws2, t_wg = wsl.next()
                            wg = ws2[:, :].rearrange("p (k n) -> p k n", k=KC)
                            wload(wg, wv[:, :, C_GATE + n_ * 1024 + dpair * 256:C_GATE + n_ * 1024 + (dpair + 1) * 256], t_wg)
                            for dd in range(2):
                                dch = dpair * 2 + dd
                                bg = nb()
                                for kc in range(KC):
                                    MM(psb[bg][:, :nq], wg[:, kc, dd * 128:(dd + 1) * 128], h[:, kc, :nq],
                                       kc == 0, kc == KC - 1, [t_wg, t_h], [t_ps[bg]])
                                sig, t_sig = R['bf'].next()
                                ACT(sig[:, :nq], psb[bg][:, :nq], AF.Sigmoid, [t_ps[bg]], [t_sig])
                                bp = nb()
                                for cc in range(4):
                                    MM(psb[bp][:, :nq], wb[:, cc, (dch % 4) * 128:(dch % 4 + 1) * 128], o_n[:, cc, :nq],
                                       cc == 0, cc == 3, [t_wb, t_on], [t_ps[bp]])
                                if n_ == 0:
                                    TT(merged[:, dch, :nq], sig[:, :nq], psb[bp][:, :nq], ALU.mult, [t_sig, t_ps[bp]], [],
                                       pw=[t_merged])
                                else:
                                    tmp, t_tmp = R['f32'].next()
                                    TT(tmp[:, :nq], sig[:, :nq], psb[bp][:, :nq], ALU.mult, [t_sig, t_ps[bp]], [t_tmp])
                                    TT(merged[:, dch, :nq], merged[:, dch, :nq], tmp[:, :nq], ALU.add, [t_merged, t_tmp], [],
                                       pw=[t_merged])
                wov = w_out[l].rearrange("(kc p) n -> p kc n", p=128)
                for q4 in range(4):
                    ws, t_wo = wsl.next()
                    wo = ws[:, :].rearrange("p (k n) -> p k n", k=KC)
                    wload(wo, wov[:, :, q4 * 256:(q4 + 1) * 256], t_wo)
                    for dd in range(2):
                        dco = q4 * 2 + dd
                        bk = nb()
                        for kc in range(KC):
                            MM(psb[bk][:, :nq], wo[:, kc, dd * 128:(dd + 1) * 128], merged[:, kc, :nq],
                               kc == 0, kc == KC - 1, [t_wo, t_merged], [t_ps[bk]])
                        STT(xdst(dco), psb[bk][:, :nq], modcol(l, 2, dco, j), xsrc(dco), ALU.mult, ALU.add,
                            [t_ps[bk], t_mod] + t_xs[dco], [], pw=t_xs[dco])
                if 'mix' in debug and (not isc) and b == 0:
                    dmg = dram_out("dbg_merged", [128, KC * 512], BF16)
                    sch.dma('sp', dmg[:], merged[:].rearrange("p k t -> p (k t)"), reads=[t_merged])
                    dod = dram_out("dbg_o", [128, 3 * 4 * 512], BF16)
                    sch.dma('sp', dod[:, 0:2048], o_da[:].rearrange("p k t -> p (k t)"), reads=[t_oda])
                    sch.dma('sp', dod[:, 2048:4096], o_mla[:].rearrange("p k t -> p (k t)"), reads=[t_omla])
                    sch.dma('sp', dod[:, 4096:6144], o_pool[:].rearrange("p k t -> p (k t)"), reads=[t_opool])
        barrier()

    def phase3(l, do_ctx):
        moe = (l % 2 == 1)
        with ExitStack() as sc:
            lsb = mk_sb(sc)
            R = {'bf': Ring(lsb, "p3bf", 3, [128, 512], BF16), 'f32': Ring(lsb, "p3f", 4, [128, 512], F32),
                 'rstd': Ring(lsb, "p3r", 2, [128, 512], F32)}
            NT = TOK + (CTX if do_ctx else 0)
            h2 = lsb("h2", [128, KC, NT], BF16)
            t_h2 = Tk('h2')
            wsl = Ring(lsb, "w3", 5, [128, 4096], BF16)
            actb = [(lsb(f"actb{i}", [128, 4, 512], BF16), Tk(f'actb{i}')) for i in range(2)]
            blocks = [('l', b) for b in range(4)] + ([('c', 0)] if do_ctx else [])
            if moe:
                router_sb = lsb("router", [128, KC, NEXP], F32)
                t_router = Tk('router')
                sch.dma('sp', router_sb[:], router_d.rearrange("(kc p) e -> p kc e", p=128), writes=[t_router])
                gates = lsb("gates", [128, 16, NEXP], F32)
                gtmp = lsb("gtmp", [128, 16, NEXP], F32)
                gm = lsb("gm", [128, 16], F32)
                t_gates = Tk('gates')
                gbc = [(lsb(f"gbc{i}", [128, TOK], BF16), Tk(f'gbc{i}')) for i in range(2)]
                gmat = [(lsb(f"gmat{i}", [128, 128], F32), Tk(f'gmat{i}')) for i in range(2)]
                lgT = lsb("lgT", [32, 512], F32)
                t_lgT = Tk('lgT')
                sch.op('dve', lambda e: e.memset(lgT[:], 0.0), writes=[t_lgT])
            for (kind, b) in blocks:
                isc = kind == 'c'
                ntok = CTX if isc else 512
                j = 1 if isc else 0
                off = TOK if isc else b * 512
                if isc:
                    xsrc = lambda kc: xc_sb[:, kc, :]
                    t_xs = [[t_xc[kc]] for kc in range(KC)]
                else:
                    xsrc = lambda kc, off=off: x_sb[:, kc, off:off + 512]
                    t_xs = [[t_x[kc][b]] for kc in range(KC)]
                cb = None
                if moe:
                    def cb(kc, tmp, t_tmp, b=b, j=j):
                        hf, t_hf = R['f32'].next()
                        ACT(hf[:, :512], tmp[:, :512], AF.Identity, [t_tmp, t_mod], [t_hf],
                            scale=modA[:, l, 1, kc, j:j + 1], bias=modcol(l, 3, kc, j))
                        MM(psb[6][0:NEXP, :], router_sb[:, kc, :], hf[:, :512], kc == 0, kc == KC - 1,
                           [t_hf, t_router], [t_ps[6]])
                        if kc == KC - 1:
                            CP(lgT[0:NEXP, :], psb[6][0:NEXP, :], [t_ps[6]], [], pw=[t_lgT])
                            for ti in range(4):
                                c0 = (b * 4 + ti) * NEXP
                                MM(psb[5][:, c0:c0 + NEXP], lgT[0:32, ti * 128:(ti + 1) * 128],
                                   ident_f[0:32, 0:NEXP], True, True, [t_lgT, t_consts], [t_ps[5]])
                norm_mod(R, xsrc, t_xs, ntok, l, 1, j, h2[:, :, off:off + ntok], t_h2, f32cb=cb, bank=7)
            if moe:
                lg = psb[5][:, 0:16 * NEXP].rearrange("p (t e) -> p t e", e=NEXP)
                CP(gates[:], lg, [t_ps[5]], [t_gates])
                sch.op('dve', lambda e: e.tensor_reduce(out=gm[:], in_=gates[:], axis=mybir.AxisListType.X, op=ALU.max),
                       reads=[t_gates], pwrites=[t_gates])
                gmb = gm[:, :, None].to_broadcast([128, 16, NEXP])
                TT(gtmp[:], gates[:], gmb, ALU.is_equal, [t_gates], [], pw=[t_gates])
                STT(gtmp[:], gtmp[:], -BIG, gates[:], ALU.mult, ALU.add, [t_gates], [], pw=[t_gates])
                gm2 = lsb("gm2", [128, 16], F32)
                sch.op('dve', lambda e: e.tensor_reduce(out=gm2[:], in_=gtmp[:], axis=mybir.AxisListType.X, op=ALU.max),
                       reads=[t_gates], pwrites=[t_gates])
                gm2b = gm2[:, :, None].to_broadcast([128, 16, NEXP])
                TT(gtmp[:], gates[:], gm2b, ALU.is_ge, [t_gates], [], pw=[t_gates])
                TT(gates[:], gates[:], gmb, ALU.subtract, [t_gates], [], pw=[t_gates])
                ACT(gates[:], gates[:], AF.Exp, [t_gates], [], pw=[t_gates])
                TT(gates[:], gates[:], gtmp[:], ALU.mult, [t_gates], [], pw=[t_gates])
                sch.op('dve', lambda e: e.tensor_reduce(out=gm[:], in_=gates[:], axis=mybir.AxisListType.X, op=ALU.add),
                       reads=[t_gates], pwrites=[t_gates])
                sch.op('dve', lambda e: e.reciprocal(out=gm[:], in_=gm[:]), reads=[t_gates], pwrites=[t_gates])
                TT(gates[:], gates[:], gmb, ALU.mult, [t_gates], [], pw=[t_gates])
                if 'gates' in debug:
                    dg = dram_out("dbg_gates", [128, 16 * NEXP])
                    sch.dma('sp', dg[:], gates[:].rearrange("p t e -> p (t e)"), reads=[t_gates])

            def ffn_expert(wgv, wuv, wdv, dff, gb, t_gb):
                nfg = (dff + 511) // 512
                bi = 0
                for fg in range(nfg):
                    nf = min(512, dff - fg * 512)
                    nfc = nf // 128
                    ws, t_wg = wsl.next()
                    wg = ws[:, 0:KC * nf].rearrange("p (k n) -> p k n", k=KC)
                    wload(wg, wgv[:, :, fg * 512:fg * 512 + nf], t_wg)
                    ws, t_wu = wsl.next()
                    wu = ws[:, 0:KC * nf].rearrange("p (k n) -> p k n", k=KC)
                    wload(wu, wuv[:, :, fg * 512:fg * 512 + nf], t_wu)
                    ws, t_wd = wsl.next()
                    wd = ws[:, 0:nfc * 1024].rearrange("p (c n) -> p c n", c=nfc)
                    wload(wd, wdv[:, fg * 4:fg * 4 + nfc, :], t_wd)
                    for (kind, b) in blocks:
                        isc = kind == 'c'
                        ntok = CTX if isc else 512
                        j = 1 if isc else 0
                        off = TOK if isc else b * 512
                        t_xs = [[t_xc[kc]] for kc in range(KC)] if isc else [[t_x[kc][b]] for kc in range(KC)]
                        xs = (lambda kc: xc_sb[:, kc, :]) if isc else (lambda kc, off=off: x_sb[:, kc, off:off + 512])
                        ab, t_ab = actb[bi % 2]
                        bi += 1
                        for fc in range(nfc):
                            pg, pu = (0, 1) if fc % 2 == 0 else (2, 3)
                            for kc in range(KC):
                                MM(psb[pg][:, :ntok], wg[:, kc, fc * 128:(fc + 1) * 128], h2[:, kc, off:off + ntok],
                                   kc == 0, kc == KC - 1, [t_wg, t_h2], [t_ps[pg]])
                            for kc in range(KC):
                                MM(psb[pu][:, :ntok], wu[:, kc, fc * 128:(fc + 1) * 128], h2[:, kc, off:off + ntok],
                                   kc == 0, kc == KC - 1, [t_wu, t_h2], [t_ps[pu]])
                            sg, t_sg = R['f32'].next()
                            ACT(sg[:, :ntok], psb[pg][:, :ntok], AF.Silu, [t_ps[pg]], [t_sg])
                            if gb is None:
                                TT(ab[:, fc, :ntok], sg[:, :ntok], psb[pu][:, :ntok], ALU.mult, [t_sg, t_ps[pu]], [],
                                   pw=[t_ab])
                            else:
                                TT(sg[:, :ntok], sg[:, :ntok], psb[pu][:, :ntok], ALU.mult, [t_sg, t_ps[pu]], [t_sg])
                                TT(ab[:, fc, :ntok], sg[:, :ntok], gb[:, off:off + ntok], ALU.mult, [t_sg, t_gb], [],
                                   pw=[t_ab])
                        for dch in range(KC):
                            py = 4 + (dch % 2)
                            for fc in range(nfc):
                                MM(psb[py][:, :ntok], wd[:, fc, dch * 128:(dch + 1) * 128], ab[:, fc, :ntok],
                                   fc == 0, fc == nfc - 1, [t_wd, t_ab], [t_ps[py]])
                            STT(xs(dch), psb[py][:, :ntok], modcol(l, 5, dch, j), xs(dch), ALU.mult, ALU.add,
                                [t_ps[py], t_mod] + t_xs[dch], [], pw=t_xs[dch])

            if not moe:
                ffn_expert(ffn_wg.rearrange("(kc p) n -> p kc n", p=128), ffn_wu.rearrange("(kc p) n -> p kc n", p=128),
                           ffn_wd.rearrange("(c p) n -> p c n", p=128), D_FF, None, None)
            else:
                for ex in range(NEXP):
                    gb, t_gb = gbc[ex % 2]
                    for ti in range(16):
                        gmt, t_gmt = gmat[ti % 2]
                        TS(gmt[:], ones_f, gates[:, ti, ex:ex + 1], None, ALU.mult, None, [t_consts, t_gates], [t_gmt])
                        bk = 6 + (ti // 4) % 2
                        MM(psb[bk][:, (ti % 4) * 128:(ti % 4 + 1) * 128], gmt[:], ident_f, True, True,
                           [t_gmt, t_consts], [t_ps[bk]])
                        if ti % 4 == 3:
                            CP(gb[:, (ti // 4) * 512:(ti // 4 + 1) * 512], psb[bk][:, :], [t_ps[bk]], [],
                               pw=[t_gb])
                    ffn_expert(moe_wg[ex].rearrange("(kc p) n -> p kc n", p=128),
                               moe_wu[ex].rearrange("(kc p) n -> p kc n", p=128),
                               moe_wd[ex].rearrange("(c p) n -> p c n", p=128), D_FFE, gb, t_gb)
        barrier()

    def final_norm():
        with ExitStack() as sc:
            lsb = mk_sb(sc)
            R = {'bf': Ring(lsb, "fnbf", 3, [128, 512], BF16), 'f32': Ring(lsb, "fnf", 4, [128, 512], F32),
                 'rstd': Ring(lsb, "fnr", 2, [128, 512], F32)}
            ogf, _ = COLS['gfin']
            for b in range(4):
                off = b * 512
                for kc in range(KC):
                    sq, t_sq = R['bf'].next()
                    ACT(sq[:], x_sb[:, kc, off:off + 512], AF.Square, [t_x[kc][b]], [t_sq])
                    MM(psb[7][:], ones_bf, sq[:], kc == 0, kc == KC - 1, [t_sq, t_consts], [t_ps[7]])
                rstd, t_rstd = R['rstd'].next()
                RSTD(rstd[:], psb[7][:], 1.0 / D, [t_ps[7]], t_rstd)
                for kc in range(KC):
                    STT(x_sb[:, kc, off:off + 512], x_sb[:, kc, off:off + 512], cols[:, ogf + kc:ogf + kc + 1], rstd[:],
                        ALU.mult, ALU.mult, [t_x[kc][b], t_rstd, t_cols], [], pw=[t_x[kc][b]])
        barrier()

    def gather(l):
        for c in range(NCH + 1):
            if c < NCH:
                src = kvl[l][c * 128:(c + 1) * 128, :]
                dst = kvg[l][c * 512:(c + 1) * 512, :]
            else:
                src = kvl[l][R_U:R_U + 8, :]
                dst = kvg[l][NCH * 512:NCH * 512 + 32, :]
            sch.coll((lambda e, src=src, dst=dst: e.collective_compute(
                "AllGather", ALU.bypass, replica_groups=[[0, 1, 2, 3], [4, 5, 6, 7]], ins=[src], outs=[dst])),
                reads=[t_kvl[l]], writes=[t_kvg[l][c]])

    if A_:
        phase1(0, True)
    if B_:
        phase2(0, True)
        phase3(0, True)
        phase1(1, False)
        x1T = dram_out("x1T", [D, TOK])
        t_o1 = Tk('o1')
        for kc in range(KC):
            sch.dma('sp', x1T[kc * 128:(kc + 1) * 128, :], x_sb[:, kc, :], reads=t_x[kc], pwrites=[t_o1])
    if ALL:
        phase1(0, True)
        gather(0)
        mods_stage(MODS_REST, False)
        phase2(0, True)
        phase3(0, True)
        phase1(1, False)
        gather(1)
        phase2(1, False)
        phase3(1, False)
        final_norm()
    if C_:
        phase2(1, False)
        if 'xa' in debug:
            dxa = dram_out("dbg_xa", [D, TOK])
            for kc in range(KC):
                sch.dma('sp', dxa[kc * 128:(kc + 1) * 128, :], x_sb[:, kc, :], reads=t_x[kc])
            barrier()
        phase3(1, False)
        final_norm()

    if 'mod' in debug:
        dmod = dram_out("dbg_mod", [128, DEPTH * 96])
        sch.dma('sp', dmod[:], mod_sb[:].rearrange("p l m j -> p (l m j)"), reads=[t_mod])
        dmisc = dram_out("dbg_misc", [128, 16])
        sch.dma('sp', dmisc[:], misc[:], reads=[t_misc])
    if 'u' in debug:
        du = dram_out("dbg_u", [128, 4 * (TOK + 16)], BF16)
        sch.dma('sp', du[:], u_sb[:].rearrange("p g t -> p (g t)"), reads=[t_u])
    if 'x' in debug:
        dx = dram_out("dbg_x", [D, TOK])
        for kc in range(KC):
            sch.dma('sp', dx[kc * 128:(kc + 1) * 128, :], x_sb[:, kc, :], reads=t_x[kc])
        dxc = dram_out("dbg_xc", [D, CTX])
        for kc in range(KC):
            sch.dma('sp', dxc[kc * 128:(kc + 1) * 128, :], xc_sb[:, kc, :], reads=[t_xc[kc]])

    t_out = Tk('out')
    if C_ or ALL:
        outT = dram_out("outT", [D, TOK])
        for kc in range(KC):
            sch.dma('sp', outT[kc * 128:(kc + 1) * 128, :], x_sb[:, kc, :], reads=t_x[kc], pwrites=[t_out])
    barrier()
    sch.emit(nc, top)
    top.close()
    return nc


def _rope_tables():
    n_freq = 16
    inv = np.exp(-math.log(10000.0) * np.arange(n_freq, dtype=np.float32) * np.float32(2.0 / 32)).astype(np.float32)
    t = np.arange(S)
    row = (t // 64).astype(np.float32)
    colp = (t % 64).astype(np.float32)
    ar = row[:, None] * inv[None, :]
    ac = colp[:, None] * inv[None, :]
    C = np.zeros((128, S), np.float32)
    Sg = np.zeros((128, S), np.float32)
    for p in range(128):
        d = p % 64
        ang = ar if d < 32 else ac
        i = d % 16
        sign = -1.0 if (d % 32) < 16 else 1.0
        C[p] = np.cos(ang[:, i])
        Sg[p] = sign * np.sin(ang[:, i])
    return C, Sg


def _pool_fix(L, t_start, n):
    f = np.ones((4, 16), np.float32)
    for g, w in enumerate(POOL_WINDOWS):
        for k in range(16):
            t = t_start + k if k < 8 else t_start + n - 16 + k
            lo = min(max(t - w // 2, 0), L)
            hi = min(max(t - w // 2 + w, 0), L)
            f[g, k] = w / float(hi - lo)
    return f


def host_common(inp):
    cols = np.zeros((128, NCOLS), np.float32)

    def put(name, arr):
        o, w = COLS[name]
        assert arr.shape == (128, w), (name, arr.shape, w)
        cols[:, o:o + w] = arr
    put('cctx', _colvec(inp['c_ctx']))
    for l in range(DEPTH):
        put(f'bmod{l}', _colvec(inp['b_mod'][l]))
        put(f'gmix{l}', _colvec(inp['g_mix'][l]))
        put(f'gffn{l}', _colvec(inp['g_ffn'][l]))
        put(f'gq{l}', _colvec(inp['mla_gq'][l]))
        put(f'gkv{l}', _colvec(inp['mla_gkv'][l]))
        put(f'pscale{l}', _colvec(inp['pool_scale'][l]))
        put(f'subln{l}', _colvec(inp['da_subln'][l]))
        lam = np.zeros((128, 4), np.float32)
        lam[:64, :] = np.asarray(inp['da_lambda'][l], np.float32).T
        lam[64:, :] = 0.0
        put(f'lam{l}', lam)
    put('gfin', _colvec(inp['g_final']))
    put('pfixc', np.broadcast_to(_pool_fix(CTX, 0, CTX).reshape(1, 64), (128, 64)).copy())
    consts = np.zeros((128, 384), np.float32)
    consts[:, 0:128] = 1.0
    consts[:, 128:256] = np.eye(128, dtype=np.float32)
    for m in range(128):
        partner = (m & ~31) | ((m & 31) ^ 16)
        consts[partner, 256 + m] = 1.0
    constf = np.ascontiguousarray(consts[:, 0:256])
    consts = consts.astype(ml_dtypes.bfloat16)
    C, Sg = _rope_tables()
    uq_perm = np.concatenate([np.arange(h * 192, h * 192 + 128) for h in range(4)] +
                             [np.arange(h * 192 + 128, h * 192 + 192) for h in range(4)])
    ukv_perm = np.concatenate([np.arange(h * 256, h * 256 + 128) for h in range(4)] +
                              [np.arange(h * 256 + 128, h * 256 + 256) for h in range(4)])
    f32 = lambda a: np.ascontiguousarray(np.asarray(a, np.float32))
    shared = {
        "consts": consts, "constf": constf,
        "w_mod": f32(inp['w_mod']), "w_in": f32(inp['w_in']),
        "w_uq_p": f32(np.asarray(inp['w_uq'])[:, :, uq_perm]),
        "w_ukv_p": f32(np.asarray(inp['w_ukv'])[:, :, ukv_perm]),
        "pool_w": f32(inp['pool_w']), "w_branch": f32(inp['w_branch']), "w_out": f32(inp['w_out']),
    }
    percore = []
    for core in range(NCORE):
        b = core // 4
        t0 = (core % 4) * TOK
        cc = cols.copy()
        o, w = COLS['c']
        cc[:, o:o + w] = _colvec(inp['c'][b])
        o, w = COLS['pfix']
        cc[:, o:o + w] = np.broadcast_to(_pool_fix(S, t0, TOK).reshape(1, 64), (128, 64))
        s_ = core % 4
        o, w = COLS['selL']
        if s_ > 0:
            cc[:, o + s_ - 1] = 1.0
        o, w = COLS['selR']
        if s_ < 3:
            cc[:, o + s_ + 1] = 1.0
        percore.append({
            "cols": cc,
            "ropeC": np.ascontiguousarray(C[:, t0:t0 + TOK]).astype(ml_dtypes.bfloat16),
            "ropeS": np.ascontiguousarray(Sg[:, t0:t0 + TOK]).astype(ml_dtypes.bfloat16),
        })
    return shared, percore


def _gather_kv(res, name_l, name_c):
    per = []
    for core in range(NCORE):
        b, s = core // 4, core % 4
        sh = [np.asarray(res[b * 4 + r][name_l]) for r in range(4)]
        kvg = np.concatenate([sh[r][c * 128:(c + 1) * 128] for c in range(NCH) for r in range(4)] +
                             [sh[r][R_U:R_U + 8] for r in range(4)], axis=0)
        hl = np.zeros((128, 4, 16), ml_dtypes.bfloat16)
        if s > 0:
            nb_ = np.asarray(res[core - 1][name_l])[R_U:R_U + 4].reshape(4, 128, 16)
            hl[:, :, 0:8] = nb_[:, :, 8:16].transpose(1, 0, 2)
        if s < 3:
            nb_ = np.asarray(res[core + 1][name_l])[R_U:R_U + 4].reshape(4, 128, 16)
            hl[:, :, 8:16] = nb_[:, :, 0:8].transpose(1, 0, 2)
        per.append((np.ascontiguousarray(kvg), np.asarray(res[core][name_c]), hl))
    return per


def stage_maps(inputs, shared, percore, stage, prev=None):
    f32 = lambda a: np.ascontiguousarray(np.asarray(a, np.float32))
    x = np.asarray(inputs['x'], np.float32)
    ctx = np.asarray(inputs['ctx'], np.float32)
    extra = {}
    if stage in ('B', 'all'):
        extra.update(ffn_wg=f32(inputs['ffn_w_gate'][0]), ffn_wu=f32(inputs['ffn_w_up'][0]),
                     ffn_wd=f32(inputs['ffn_w_down'][0]))
    if stage in ('C', 'all'):
        extra.update(router=f32(inputs['moe_router'][0]), moe_wg=f32(inputs['moe_w_gate'][0]),
                     moe_wu=f32(inputs['moe_w_up'][0]), moe_wd=f32(inputs['moe_w_down'][0]))
    maps = []
    for core in range(NCORE):
        b = core // 4
        t0 = (core % 4) * TOK
        m = dict(shared)
        m.update(percore[core])
        m.update(extra)
        m["ctxT"] = np.ascontiguousarray(ctx[b].T)
        if stage == 'C':
            m["xT"] = np.ascontiguousarray(prev['x1T'][core])
        else:
            m["xT"] = np.ascontiguousarray(x[b, t0:t0 + TOK, :].T)
        if stage == 'B':
            m["kvg0"], m["kvc0"], m["halo0"] = prev['kv'][core]
            m["u_in"] = prev['u'][core]
            m["uc_in"] = prev['uc'][core]
        if stage == 'C':
            m["kvg1"], m["kvc1"], m["halo1"] = prev['kv'][core]
            m["u_in"] = prev['u'][core]
        maps.append(m)
    return maps


def kernel_unfused(**inputs):
    shared, percore = host_common(inputs)
    cores = list(range(NCORE))
    ra = run_bass_kernel_spmd(build('A'), stage_maps(inputs, shared, percore, 'A'), core_ids=cores).results
    kv0 = _gather_kv(ra, 'kvl0', 'kvc0')
    pa = dict(kv=kv0, u=[np.asarray(ra[c]['u_out']) for c in cores], uc=[np.asarray(ra[c]['uc_out']) for c in cores])
    rb = run_bass_kernel_spmd(build('B'), stage_maps(inputs, shared, percore, 'B', pa), core_ids=cores).results
    kv1 = _gather_kv(rb, 'kvl1', 'kvc1')
    pb = dict(kv=kv1, x1T=[np.asarray(rb[c]['x1T']) for c in cores], u=[np.asarray(rb[c]['u_out']) for c in cores])
    rc = run_bass_kernel_spmd(build('C'), stage_maps(inputs, shared, percore, 'C', pb), core_ids=cores).results
    out = np.zeros((2, S, D), np.float32)
    for core in cores:
        b = core // 4
        t0 = (core % 4) * TOK
        out[b, t0:t0 + TOK, :] = np.asarray(rc[core]["outT"]).T
    return out


def kernel(**inputs):
    shared, percore = host_common(inputs)
    cores = list(range(NCORE))
    res = run_bass_kernel_spmd(build('all'), stage_maps(inputs, shared, percore, 'all'), core_ids=cores).results
    out = np.zeros((2, S, D), np.float32)
    for core in cores:
        b = core // 4
        t0 = (core % 4) * TOK
        out[b, t0:t0 + TOK, :] = np.asarray(res[core]["outT"]).T
    return out
```
